# Optimizing a Trainium2 kernel written in Bass

```python
import math
import numpy as np
import jax
import jax.numpy as jnp
from jax import lax

D_MODEL = 1024
BATCH = 16
SEQ = 4096
DEPTH = 4

HEAD_DIM = 64
NSA_Q_HEADS = 8
NSA_KV_HEADS = 2
NSA_GROUP = NSA_Q_HEADS // NSA_KV_HEADS
NSA_WIDTH = NSA_Q_HEADS * HEAD_DIM
CMP_LEN = 32
CMP_STRIDE = 16
CMP_HIDDEN = 256
SEL_LEN = 64
SEL_TOPK = 16
WINDOW = 512
NSA_Q_BLOCK = 64
ROPE_THETA = 10000.0
HALF_DIM = HEAD_DIM // 2
FORCED_SCORE = 1e9
NEG_INF = -1e30
CONV_WIDTH = 256
CONV_LEN = 31
S5_WIDTH = 256
S5_GROUP_CH = 16
S5_GROUPS = S5_WIDTH // S5_GROUP_CH
S5_STATE = 64
DT_MIN = 1e-3
DT_MAX = 1e-1
MIX_WIDTH = NSA_WIDTH + CONV_WIDTH + S5_WIDTH
KV_WIDTH = 6 * NSA_KV_HEADS * HEAD_DIM
GATE_WIDTH = 3 * NSA_Q_HEADS
IN_WIDTH = NSA_WIDTH + KV_WIDTH + GATE_WIDTH + 2 * CONV_WIDTH + S5_WIDTH
SPLIT_POINTS = (NSA_WIDTH,
                NSA_WIDTH + KV_WIDTH,
                NSA_WIDTH + KV_WIDTH + GATE_WIDTH,
                NSA_WIDTH + KV_WIDTH + GATE_WIDTH + 2 * CONV_WIDTH)
D_FF = 2816
N_EXPERTS = 8
TOP_K = 2
D_FF_EXPERT = 3584
N_DENSE = (DEPTH + 1) // 2
N_MOE = DEPTH // 2
PLE_DIM = 256
POS_OFFSET_MAX = 4096
ALPHA = (2 * DEPTH) ** 0.25
BETA = (8 * DEPTH) ** -0.25
LN_EPS = 1e-5

kernel_name = "hybrid_nsa_conformer_s5_moe_deepnorm"


def layer_norm(x, g, b):
    xf = x.astype(jnp.float32)
    mu = xf.mean(-1, keepdims=True)
    var = jnp.square(xf - mu).mean(-1, keepdims=True)
    return ((xf - mu) * lax.rsqrt(var + LN_EPS) * g + b).astype(x.dtype)


def masked_softmax(s, mask):
    s = jnp.where(mask, s, NEG_INF)
    m = s.max(-1, keepdims=True)
    e = jnp.where(mask, jnp.exp(s - m), 0.0)
    return e / jnp.maximum(e.sum(-1, keepdims=True), 1e-30)


def rope_tables(positions):
    inv_freq = ROPE_THETA ** (-jnp.arange(0, HEAD_DIM, 2, dtype=jnp.float32) / HEAD_DIM)
    ang = positions[..., None].astype(jnp.float32) * inv_freq
    return jnp.cos(ang)[:, :, None, :], jnp.sin(ang)[:, :, None, :]


def apply_rope(t, cos, sin):
    tf = t.astype(jnp.float32)
    t1, t2 = tf[..., :HALF_DIM], tf[..., HALF_DIM:]
    return jnp.concatenate([t1 * cos - t2 * sin, t2 * cos + t1 * sin], -1).astype(t.dtype)


def nsa_attention(q, k_cmp, v_cmp, k_sel, v_sel, k_win, v_win, gates,
                  cmp_pe, cmp_w1, cmp_b1, cmp_w2, cmp_b2):
    B, L = q.shape[0], q.shape[1]
    scale = HEAD_DIM ** -0.5
    qg = q.reshape(B, L, NSA_KV_HEADS, NSA_GROUP, HEAD_DIM).transpose(0, 2, 3, 1, 4)
    gg = gates.reshape(B, L, NSA_KV_HEADS, NSA_GROUP, 3).transpose(0, 2, 3, 1, 4)
    to_kv = lambda t: t.transpose(0, 2, 1, 3)

    n_cmp = (L - CMP_LEN) // CMP_STRIDE + 1
    blk_idx = np.arange(n_cmp)[:, None] * CMP_STRIDE + np.arange(CMP_LEN)[None, :]

    def compress(t, j):
        blocks = t[:, :, blk_idx] + cmp_pe[j]
        flat = blocks.reshape(B, NSA_KV_HEADS, n_cmp, CMP_LEN * HEAD_DIM)
        return jax.nn.gelu(flat @ cmp_w1[j] + cmp_b1[j]) @ cmp_w2[j] + cmp_b2[j]

    kc = compress(to_kv(k_cmp), 0)
    vc = compress(to_kv(v_cmp), 1)
    cmp_end = jnp.asarray(np.arange(n_cmp) * CMP_STRIDE + CMP_LEN - 1)

    n_sel = L // SEL_LEN
    topk = min(SEL_TOPK, n_sel)
    c_start = np.arange(n_cmp)[:, None] * CMP_STRIDE
    s_start = np.arange(n_sel)[None, :] * SEL_LEN
    overlap = jnp.asarray(((c_start < s_start + SEL_LEN) & (c_start + CMP_LEN > s_start)).astype(np.float32))
    ks_blocks = to_kv(k_sel).reshape(B, NSA_KV_HEADS, n_sel, SEL_LEN, HEAD_DIM)
    vs_blocks = to_kv(v_sel).reshape(B, NSA_KV_HEADS, n_sel, SEL_LEN, HEAD_DIM)
    gather = jax.vmap(jax.vmap(lambda blk, ix: blk[ix]))
    sel_offsets = jnp.arange(SEL_LEN)
    blk_ids = jnp.arange(n_sel)

    pad = ((0, 0), (0, 0), (WINDOW, 0), (0, 0))
    kw = jnp.pad(to_kv(k_win), pad)
    vw = jnp.pad(to_kv(v_win), pad)

    def block_fn(c):
        q0 = c * NSA_Q_BLOCK
        qc = lax.dynamic_slice_in_dim(qg, q0, NSA_Q_BLOCK, axis=3)
        gc = lax.dynamic_slice_in_dim(gg, q0, NSA_Q_BLOCK, axis=3)
        t = q0 + jnp.arange(NSA_Q_BLOCK)

        s = jnp.einsum('bkgqd,bknd->bkgqn', qc, kc).astype(jnp.float32) * scale
        p_cmp = masked_softmax(s, cmp_end[None, :] <= t[:, None])
        o_cmp = jnp.einsum('bkgqn,bknd->bkgqd', p_cmp, vc)

        imp = jnp.einsum('bkgqn,ns->bkqs', p_cmp, overlap)
        qblk = t // SEL_LEN
        forced = (blk_ids[None, :] == 0) | (blk_ids[None, :] == qblk[:, None]) | (blk_ids[None, :] == qblk[:, None] - 1)
        score = jnp.where(forced, FORCED_SCORE, jnp.where(blk_ids[None, :] <= qblk[:, None], imp, NEG_INF))
        _, idx = lax.top_k(score, topk)
        ks = gather(ks_blocks, idx).reshape(B, NSA_KV_HEADS, NSA_Q_BLOCK, topk * SEL_LEN, HEAD_DIM)
        vs = gather(vs_blocks, idx).reshape(B, NSA_KV_HEADS, NSA_Q_BLOCK, topk * SEL_LEN, HEAD_DIM)
        pos = (idx[..., None] * SEL_LEN + sel_offsets).reshape(B, NSA_KV_HEADS, NSA_Q_BLOCK, topk * SEL_LEN)
        s = jnp.einsum('bkgqd,bkqsd->bkgqs', qc, ks).astype(jnp.float32) * scale
        p_sel = masked_softmax(s, (pos <= t[:, None])[:, :, None])
        o_sel = jnp.einsum('bkgqs,bkqsd->bkgqd', p_sel, vs)

        kwc = lax.dynamic_slice_in_dim(kw, q0, NSA_Q_BLOCK + WINDOW, axis=2)
        vwc = lax.dynamic_slice_in_dim(vw, q0, NSA_Q_BLOCK + WINDOW, axis=2)
        wpos = q0 - WINDOW + jnp.arange(NSA_Q_BLOCK + WINDOW)
        wmask = (wpos[None, :] <= t[:, None]) & (wpos[None, :] > t[:, None] - WINDOW) & (wpos[None, :] >= 0)
        s = jnp.einsum('bkgqd,bksd->bkgqs', qc, kwc).astype(jnp.float32) * scale
        p_win = masked_softmax(s, wmask)
        o_win = jnp.einsum('bkgqs,bksd->bkgqd', p_win, vwc)

        return (gc[..., 0:1] * o_cmp + gc[..., 1:2] * o_sel + gc[..., 2:3] * o_win).astype(q.dtype)

    out = lax.map(block_fn, jnp.arange(L // NSA_Q_BLOCK))
    return out.transpose(1, 0, 4, 2, 3, 5).reshape(B, L, NSA_WIDTH)


def conformer_conv(zc, conv_w, conv_b, ln_g, ln_b):
    u = zc[..., :CONV_WIDTH] * jax.nn.sigmoid(zc[..., CONV_WIDTH:])
    y = lax.conv_general_dilated(u, conv_w[:, None, :].astype(u.dtype), (1,), [(CONV_LEN - 1, 0)],
                                 dimension_numbers=('NWC', 'WIO', 'NWC'),
                                 feature_group_count=CONV_WIDTH) + conv_b
    return jax.nn.silu(layer_norm(y, ln_g, ln_b))


def _complex_linear_combine(e1, e2):
    a1r, a1i, b1r, b1i = e1
    a2r, a2i, b2r, b2i = e2
    return (a2r * a1r - a2i * a1i,
            a2r * a1i + a2i * a1r,
            a2r * b1r - a2i * b1i + b2r,
            a2r * b1i + a2i * b1r + b2i)


def s5_layer(u, a_re, a_im, log_dt, b_re, b_im, c_re, c_im, d_skip, glu_w, glu_b):
    B, L, _ = u.shape
    uf = u.astype(jnp.float32)
    ug = uf.reshape(B, L, S5_GROUPS, S5_GROUP_CH)
    a_re = a_re.astype(jnp.float32)
    a_im = a_im.astype(jnp.float32)
    dt = jnp.exp(log_dt.astype(jnp.float32))[:, None]
    mag = jnp.exp(a_re * dt)
    ab_re = mag * jnp.cos(a_im * dt)
    ab_im = mag * jnp.sin(a_im * dt)
    den = a_re * a_re + a_im * a_im
    coef_re = ((ab_re - 1.0) * a_re + ab_im * a_im) / den
    coef_im = (ab_im * a_re - (ab_re - 1.0) * a_im) / den
    b_re = b_re.astype(jnp.float32)
    b_im = b_im.astype(jnp.float32)
    bb_re = coef_re[..., None] * b_re - coef_im[..., None] * b_im
    bb_im = coef_re[..., None] * b_im + coef_im[..., None] * b_re
    bu_re = jnp.einsum('blgc,gnc->blgn', ug, bb_re)
    bu_im = jnp.einsum('blgc,gnc->blgn', ug, bb_im)
    shape = bu_re.shape
    elems = (jnp.broadcast_to(ab_re, shape), jnp.broadcast_to(ab_im, shape), bu_re, bu_im)
    _, _, h_re, h_im = lax.associative_scan(_complex_linear_combine, elems, axis=1)
    y = (jnp.einsum('blgn,gcn->blgc', h_re, c_re.astype(jnp.float32))
         - jnp.einsum('blgn,gcn->blgc', h_im, c_im.astype(jnp.float32)))
    y = y.reshape(B, L, S5_WIDTH) + d_skip * uf
    z = jax.nn.gelu(y)
    return (z * jax.nn.sigmoid(z @ glu_w + glu_b)).astype(u.dtype)


def hybrid_mixer(x, cos, sin, w_in, w_out, cmp_pe, cmp_w1, cmp_b1, cmp_w2, cmp_b2,
                 conv_w, conv_b, conv_ln_g, conv_ln_b,
                 s5_a_re, s5_a_im, s5_log_dt, s5_b_re, s5_b_im, s5_c_re, s5_c_im,
                 s5_d, s5_glu_w, s5_glu_b):
    B, L, _ = x.shape
    z = x @ w_in
    zq, zkv, zg, zc, zs = jnp.split(z, SPLIT_POINTS, axis=-1)
    q = apply_rope(zq.reshape(B, L, NSA_Q_HEADS, HEAD_DIM), cos, sin)
    kv = zkv.reshape(B, L, 6, NSA_KV_HEADS, HEAD_DIM)
    k_cmp = apply_rope(kv[:, :, 0], cos, sin)
    k_sel = apply_rope(kv[:, :, 2], cos, sin)
    k_win = apply_rope(kv[:, :, 4], cos, sin)
    gates = jax.nn.sigmoid(zg).reshape(B, L, NSA_Q_HEADS, 3)
    o_nsa = nsa_attention(q, k_cmp, kv[:, :, 1], k_sel, kv[:, :, 3], k_win, kv[:, :, 5], gates,
                          cmp_pe, cmp_w1, cmp_b1, cmp_w2, cmp_b2)
    o_conv = conformer_conv(zc, conv_w, conv_b, conv_ln_g, conv_ln_b)
    o_s5 = s5_layer(zs, s5_a_re, s5_a_im, s5_log_dt, s5_b_re, s5_b_im, s5_c_re, s5_c_im,
                    s5_d, s5_glu_w, s5_glu_b)
    return jnp.concatenate([o_nsa, o_conv.astype(o_nsa.dtype), o_s5.astype(o_nsa.dtype)], -1) @ w_out


def swiglu(x, w1, w3, w2):
    return (jax.nn.silu(x @ w1) * (x @ w3)) @ w2


def moe_swiglu(x, router, w1, w3, w2):
    logits = (x @ router).astype(jnp.float32)
    top_vals, top_idx = lax.top_k(logits, TOP_K)
    wts = jax.nn.softmax(top_vals, axis=-1)
    comb = jnp.einsum('blk,blke->ble', wts, jax.nn.one_hot(top_idx, N_EXPERTS, dtype=jnp.float32))
    y = jnp.zeros(x.shape, jnp.float32)
    for e in range(N_EXPERTS):
        y = y + comb[..., e:e + 1] * swiglu(x, w1[e], w3[e], w2[e])
    return y.astype(x.dtype)


def setup_inputs(seed: int = 0) -> dict:
    key = jax.random.key(seed)
    keys = iter(jax.random.split(key, 48))
    f32 = jnp.float32

    def nrm(shape, scale):
        return jax.random.normal(next(keys), shape, f32) * scale

    x = nrm((BATCH, SEQ, D_MODEL), 1.0)
    p = nrm((DEPTH, BATCH, SEQ, PLE_DIM), 1.0)
    offsets = jax.random.randint(next(keys), (BATCH, 1), 0, POS_OFFSET_MAX, dtype=jnp.int32)
    positions = offsets + jnp.arange(SEQ, dtype=jnp.int32)[None, :]
    w_in = nrm((DEPTH, D_MODEL, IN_WIDTH), D_MODEL ** -0.5)
    w_out = nrm((DEPTH, MIX_WIDTH, D_MODEL), MIX_WIDTH ** -0.5 * BETA)
    cmp_pe = nrm((DEPTH, 2, CMP_LEN, HEAD_DIM), 0.1)
    cmp_w1 = nrm((DEPTH, 2, CMP_LEN * HEAD_DIM, CMP_HIDDEN), (CMP_LEN * HEAD_DIM) ** -0.5)
    cmp_b1 = nrm((DEPTH, 2, CMP_HIDDEN), 0.02)
    cmp_w2 = nrm((DEPTH, 2, CMP_HIDDEN, HEAD_DIM), CMP_HIDDEN ** -0.5)
    cmp_b2 = nrm((DEPTH, 2, HEAD_DIM), 0.02)
    conv_w = nrm((DEPTH, CONV_LEN, CONV_WIDTH), CONV_LEN ** -0.5)
    conv_b = nrm((DEPTH, CONV_WIDTH), 0.02)
    conv_ln_g = 1.0 + nrm((DEPTH, CONV_WIDTH), 0.02)
    conv_ln_b = nrm((DEPTH, CONV_WIDTH), 0.02)
    s5_a_re = -0.5 + nrm((DEPTH, S5_GROUPS, S5_STATE), 0.01)
    s5_a_im = math.pi * jnp.arange(S5_STATE, dtype=f32) + nrm((DEPTH, S5_GROUPS, S5_STATE), 0.01)
    s5_log_dt = jax.random.uniform(next(keys), (DEPTH, S5_GROUPS), f32,
                                   math.log(DT_MIN), math.log(DT_MAX))
    s5_b_re = nrm((DEPTH, S5_GROUPS, S5_STATE, S5_GROUP_CH), (2 * S5_GROUP_CH) ** -0.5)
    s5_b_im = nrm((DEPTH, S5_GROUPS, S5_STATE, S5_GROUP_CH), (2 * S5_GROUP_CH) ** -0.5)
    s5_c_re = nrm((DEPTH, S5_GROUPS, S5_GROUP_CH, S5_STATE), S5_STATE ** -0.5)
    s5_c_im = nrm((DEPTH, S5_GROUPS, S5_GROUP_CH, S5_STATE), S5_STATE ** -0.5)
    s5_d = nrm((DEPTH, S5_WIDTH), 1.0)
    s5_glu_w = nrm((DEPTH, S5_WIDTH, S5_WIDTH), S5_WIDTH ** -0.5)
    s5_glu_b = nrm((DEPTH, S5_WIDTH), 0.02)
    ffn_w1 = nrm((N_DENSE, D_MODEL, D_FF), D_MODEL ** -0.5)
    ffn_w3 = nrm((N_DENSE, D_MODEL, D_FF), D_MODEL ** -0.5)
    ffn_w2 = nrm((N_DENSE, D_FF, D_MODEL), D_FF ** -0.5 * BETA)
    moe_router = nrm((N_MOE, D_MODEL, N_EXPERTS), D_MODEL ** -0.5)
    moe_w1 = nrm((N_MOE, N_EXPERTS, D_MODEL, D_FF_EXPERT), D_MODEL ** -0.5)
    moe_w3 = nrm((N_MOE, N_EXPERTS, D_MODEL, D_FF_EXPERT), D_MODEL ** -0.5)
    moe_w2 = nrm((N_MOE, N_EXPERTS, D_FF_EXPERT, D_MODEL), D_FF_EXPERT ** -0.5 * BETA)
    ple_gate_w = nrm((DEPTH, D_MODEL, D_MODEL), D_MODEL ** -0.5)
    ple_gate_b = nrm((DEPTH, D_MODEL), 0.02)
    ple_proj = nrm((DEPTH, PLE_DIM, D_MODEL), PLE_DIM ** -0.5 * BETA)
    ln_g = 1.0 + nrm((DEPTH, 3, D_MODEL), 0.02)
    ln_b = nrm((DEPTH, 3, D_MODEL), 0.02)
    return {"x": x, "p": p, "positions": positions, "w_in": w_in, "w_out": w_out,
            "cmp_pe": cmp_pe, "cmp_w1": cmp_w1, "cmp_b1": cmp_b1, "cmp_w2": cmp_w2, "cmp_b2": cmp_b2,
            "conv_w": conv_w, "conv_b": conv_b, "conv_ln_g": conv_ln_g, "conv_ln_b": conv_ln_b,
            "s5_a_re": s5_a_re, "s5_a_im": s5_a_im, "s5_log_dt": s5_log_dt,
            "s5_b_re": s5_b_re, "s5_b_im": s5_b_im, "s5_c_re": s5_c_re, "s5_c_im": s5_c_im,
            "s5_d": s5_d, "s5_glu_w": s5_glu_w, "s5_glu_b": s5_glu_b,
            "ffn_w1": ffn_w1, "ffn_w3": ffn_w3, "ffn_w2": ffn_w2,
            "moe_router": moe_router, "moe_w1": moe_w1, "moe_w3": moe_w3, "moe_w2": moe_w2,
            "ple_gate_w": ple_gate_w, "ple_gate_b": ple_gate_b, "ple_proj": ple_proj,
            "ln_g": ln_g, "ln_b": ln_b}


def reference(x, p, positions, w_in, w_out, cmp_pe, cmp_w1, cmp_b1, cmp_w2, cmp_b2,
              conv_w, conv_b, conv_ln_g, conv_ln_b,
              s5_a_re, s5_a_im, s5_log_dt, s5_b_re, s5_b_im, s5_c_re, s5_c_im,
              s5_d, s5_glu_w, s5_glu_b,
              ffn_w1, ffn_w3, ffn_w2, moe_router, moe_w1, moe_w3, moe_w2,
              ple_gate_w, ple_gate_b, ple_proj, ln_g, ln_b):
    cos, sin = rope_tables(positions)
    for i in range(DEPTH):
        h = hybrid_mixer(x, cos, sin, w_in[i], w_out[i],
                         cmp_pe[i], cmp_w1[i], cmp_b1[i], cmp_w2[i], cmp_b2[i],
                         conv_w[i], conv_b[i], conv_ln_g[i], conv_ln_b[i],
                         s5_a_re[i], s5_a_im[i], s5_log_dt[i], s5_b_re[i], s5_b_im[i],
                         s5_c_re[i], s5_c_im[i], s5_d[i], s5_glu_w[i], s5_glu_b[i])
        x = layer_norm(ALPHA * x + h.astype(x.dtype), ln_g[i, 0], ln_b[i, 0])
        j = i // 2
        if i % 2 == 0:
            f = swiglu(x, ffn_w1[j], ffn_w3[j], ffn_w2[j])
        else:
            f = moe_swiglu(x, moe_router[j], moe_w1[j], moe_w3[j], moe_w2[j])
        x = layer_norm(ALPHA * x + f.astype(x.dtype), ln_g[i, 1], ln_b[i, 1])
        e = (p[i] @ ple_proj[i]) * jax.nn.sigmoid(x @ ple_gate_w[i] + ple_gate_b[i])
        x = layer_norm(ALPHA * x + e.astype(x.dtype), ln_g[i, 2], ln_b[i, 2])
    return x
```

```python
import math
import os
from contextlib import ExitStack
import numpy as np
import ml_dtypes
import concourse.bass as bass
import concourse.mybir as mybir
from concourse.bass_utils import run_bass_kernel_spmd

F32 = mybir.dt.float32
BF16 = mybir.dt.bfloat16
I32 = mybir.dt.int32
AF = mybir.ActivationFunctionType
ALU = mybir.AluOpType

D = 1024
L = 4096
DEPTH = 4
NT = L // 512
ALPHA = (2 * DEPTH) ** 0.25
LN_EPS = 1e-5
INW = 2072
D_FF = 2816
D_FFE = 3584
NE = 8
BIG = 30000.0
TWO_PI = 2.0 * math.pi
NDSEM = 80


class Buf:
    def __init__(self, t):
        self.t = t
        self.w = {}
        self.wf = {}
        self.r = {}
        self.prev = {}
        self.dsem = None

    def __getitem__(self, idx):
        return self.t[idx]


def _merge(d, src):
    for s, v in src.items():
        if d.get(s, 0) < v:
            d[s] = v


class KB:
    def __init__(self, nc, es):
        self.nc = nc
        self.E = {'pe': nc.tensor, 'act': nc.scalar, 'dve': nc.vector, 'pool': nc.gpsimd, 'sp': nc.sync}
        self.sem = {}
        self.tot = {}
        for e in self.E:
            self._mksem('E_' + e, es)
        self.dfree = []
        for i in range(NDSEM):
            self._mksem('D%d' % i, es)
            self.dfree.append('D%d' % i)
        self.waited = {e: {} for e in self.E}
        self.sync_same = {'pe': False, 'act': True, 'dve': True, 'pool': True, 'sp': False}
        self.phase_bufs = []
        self.rr = 0
        self.log = None

    def _mksem(self, name, es):
        self.sem[name] = es.enter_context(self.nc.semaphore(name))
        self.tot[name] = 0

    def buf(self, es, name, shape, dt, psum=False):
        self.nbuf = getattr(self, 'nbuf', 0) + 1
        name = "%s_%d" % (name, self.nbuf)
        if psum:
            t = es.enter_context(self.nc.psum_tensor(name, shape, dt))
        else:
            t = es.enter_context(self.nc.sbuf_tensor(name, shape, dt))
        b = Buf(t)
        self.phase_bufs.append(b)
        return b

    def _wait(self, eng, deps):
        own = 'E_' + eng
        for s, v in deps.items():
            if s == own and not self.sync_same[eng]:
                continue
            if self.waited[eng].get(s, 0) >= v:
                continue
            self.E[eng].wait_ge(self.sem[s], v)
            self.waited[eng][s] = v
            if self.log is not None:
                self.log.append((eng, 'w', s, v))

    def _deps(self, R, W, PW):
        deps = {}
        for b in R:
            _merge(deps, b.w)
        for b in W:
            _merge(deps, b.w)
            _merge(deps, b.r)
            _merge(deps, b.prev)
        for b in PW:
            if b.r:
                p = {}
                _merge(p, b.r)
                _merge(p, b.w)
                b.prev = p
                b.w = {}
                b.wf = {}
                b.r = {}
            _merge(deps, b.prev)
            _merge(deps, b.wf)
        return deps

    def _post(self, tok, R, W, PW):
        for b in R:
            _merge(b.r, tok)
        for b in W:
            b.w = dict(tok)
            b.wf = dict(tok)
            b.r = {}
            b.prev = {}
        for b in PW:
            _merge(b.w, tok)

    def op(self, eng, fn, R=(), W=(), PW=()):
        deps = self._deps(R, W, PW)
        self._wait(eng, deps)
        ins = fn(self.E[eng])
        s = 'E_' + eng
        self.tot[s] += 1
        ins.then_inc(self.sem[s], 1)
        if self.log is not None:
            self.log.append((eng, 'i', s, 1))
        self._post({s: self.tot[s]}, R, W, PW)

    def dma(self, out, in_, sb, R=(), W=(), PW=(), q='sp', **kw):
        deps = self._deps(R, W, PW)
        self._wait(q, deps)
        if sb.dsem is None:
            sb.dsem = self.dfree.pop()
        ins = self.E[q].dma_start(out=out, in_=in_, **kw)
        s = sb.dsem
        self.tot[s] += 16
        ins.then_inc(self.sem[s], 16)
        if self.log is not None:
            self.log.append((q, 'i', s, 16))
        self._post({s: self.tot[s]}, R, W, PW)

    def barrier(self):
        if getattr(self, 'pre_barrier', None) is not None and not os.environ.get('NO_PREBAR'):
            self.pre_barrier()
        allt = dict(self.tot)
        for e in self.E:
            self._wait(e, allt)
        for b in self.phase_bufs:
            if b.dsem is not None:
                self.dfree.append(b.dsem)
                b.dsem = None
        self.phase_bufs = []

    def ew(self):
        self.rr += 1
        return ('dve', 'pool')[self.rr % 2]


def layer_norm_tm(kb, tt, g_b, b_b, out_ap, scr):
    st, mv, rs = scr['st'], scr['mv'], scr['rs']
    kb.op('dve', lambda e: e.bn_stats(out=st[:, 0:6], in_=tt[:, 0:512]), R=[tt], PW=[st])
    kb.op('dve', lambda e: e.bn_stats(out=st[:, 6:12], in_=tt[:, 512:1024]), R=[tt], PW=[st])
    kb.op('dve', lambda e: e.bn_aggr(out=mv[:, 0:2], in_=st[:, 0:12]), R=[st], W=[mv])
    kb.op('act', lambda e: e.activation(out=rs[:, 0:1], in_=mv[:, 1:2], func=AF.Sqrt, bias=scr['eps'][:, 0:1], scale=1.0), R=[mv, scr['eps']], W=[rs])
    kb.op('dve', lambda e: e.reciprocal(out=rs[:, 1:2], in_=rs[:, 0:1]), R=[rs], PW=[rs])
    kb.op('dve', lambda e: e.tensor_scalar(out=tt[:, :], in0=tt[:, :], scalar1=mv[:, 0:1], scalar2=rs[:, 1:2], op0=ALU.subtract, op1=ALU.mult), R=[tt, mv, rs], W=[tt])
    kb.op('pool', lambda e: e.tensor_tensor(out=tt[:, :], in0=tt[:, :], in1=g_b[:, :], op=ALU.mult), R=[tt, g_b], W=[tt])
    return ('pool', lambda e: e.tensor_tensor(out=out_ap, in0=tt[:, :], in1=b_b[:, :], op=ALU.add), [tt, b_b])


def gelu_tanh(kb, u, tmp, out_ap, out_buf, n):
    kb.op('dve', lambda e: e.tensor_tensor(out=tmp[:, 0:n], in0=u[:, 0:n], in1=u[:, 0:n], op=ALU.mult), R=[u], W=[tmp])
    kb.op('dve', lambda e: e.tensor_scalar(out=tmp[:, 0:n], in0=tmp[:, 0:n], scalar1=0.044715, scalar2=1.0, op0=ALU.mult, op1=ALU.add), R=[tmp], W=[tmp])
    kb.op('dve', lambda e: e.tensor_tensor(out=tmp[:, 0:n], in0=tmp[:, 0:n], in1=u[:, 0:n], op=ALU.mult), R=[tmp, u], W=[tmp])
    kb.op('act', lambda e: e.activation(out=tmp[:, 0:n], in_=tmp[:, 0:n], func=AF.Sigmoid, scale=1.5957691216057308), R=[tmp], W=[tmp])
    kb.op('dve', lambda e: e.tensor_tensor(out=out_ap, in0=tmp[:, 0:n], in1=u[:, 0:n], op=ALU.mult), R=[tmp, u], PW=[out_buf])


def transpose_in(kb, src, nsub, nfc, dst, ident, pss, cnt, dst2=None):
    for fc in range(nfc):
        ps = pss[cnt[0] % len(pss)]
        cnt[0] += 1
        for sub in range(nsub):
            kb.op('pe', lambda e, fc=fc, sub=sub, ps=ps: e.transpose(ps[:, sub * 128:(sub + 1) * 128], src[:, sub, fc * 128:(fc + 1) * 128], ident[:, :]),
                  R=[src, ident], PW=[ps])
        eng = ('act', 'dve')[fc % 2]
        if eng == 'act':
            kb.op('act', lambda e, fc=fc, ps=ps: e.copy(out=dst[:, fc, 0:nsub * 128], in_=ps[:, 0:nsub * 128]), R=[ps], PW=[dst])
        else:
            kb.op('dve', lambda e, fc=fc, ps=ps: e.tensor_copy(out=dst[:, fc, 0:nsub * 128], in_=ps[:, 0:nsub * 128]), R=[ps], PW=[dst])
        if dst2 is not None:
            kb.op('pool', lambda e, fc=fc: e.tensor_copy(out=dst2[:, fc, 0:nsub * 128], in_=dst[:, fc, 0:nsub * 128]), R=[dst], PW=[dst2])


class Prog:
    def __init__(self, n_layers=DEPTH, n_seq=2, dbg=(), only=None):
        self.only = only
        self.n_layers = n_layers
        self.n_seq = n_seq
        self.dbg = set(dbg)
        nc = bass.Bass("TRN2", target_bir_lowering=False)
        self.nc = nc
        S = n_seq

        def din(name, shape, dt=F32):
            return nc.dram_tensor(name, list(shape), dt, kind="ExternalInput").ap()

        def dscr(name, shape, dt):
            kind = "ExternalOutput" if name in self.dbg else "Internal"
            return nc.dram_tensor(name, list(shape), dt, kind=kind).ap()

        self.x = din("x", [S, L, D])
        self.p = din("p", [DEPTH, S, L, 256])
        self.pos = din("positions", [S, L], I32)
        self.w = {}
        for name, shape in [("w_in", [DEPTH, D, INW]), ("w_out", [DEPTH, D, D]), ("cmp_pe", [DEPTH, 2, 32, 64]),
                            ("cmp_w1", [DEPTH, 2, 2048, 256]), ("cmp_b1", [DEPTH, 2, 256]), ("cmp_w2", [DEPTH, 2, 256, 64]),
                            ("cmp_b2", [DEPTH, 2, 64]), ("conv_w", [DEPTH, 31, 256]), ("conv_b", [DEPTH, 256]),
                            ("conv_ln_g", [DEPTH, 256]), ("conv_ln_b", [DEPTH, 256]), ("s5_a_re", [DEPTH, 16, 64]),
                            ("s5_a_im", [DEPTH, 16, 64]), ("s5_log_dt", [DEPTH, 16]), ("s5_b_re", [DEPTH, 16, 64, 16]),
                            ("s5_b_im", [DEPTH, 16, 64, 16]), ("s5_c_re", [DEPTH, 16, 16, 64]), ("s5_c_im", [DEPTH, 16, 16, 64]),
                            ("s5_d", [DEPTH, 256]), ("s5_glu_w", [DEPTH, 256, 256]), ("s5_glu_b", [DEPTH, 256]),
                            ("ffn_w1", [2, D, D_FF]), ("ffn_w3", [2, D, D_FF]), ("ffn_w2", [2, D_FF, D]),
                            ("moe_router", [2, D, NE]), ("moe_w1", [2, NE, D, D_FFE]), ("moe_w3", [2, NE, D, D_FFE]),
                            ("moe_w2", [2, NE, D_FFE, D]), ("ple_gate_w", [DEPTH, D, D]), ("ple_gate_b", [DEPTH, D]),
                            ("ple_proj", [DEPTH, 256, D]), ("ln_g", [DEPTH, 3, D]), ("ln_b", [DEPTH, 3, D])]:
            self.w[name] = din(name, shape)
        self.c_ident = din("c_ident", [128, 128])
        self.c_invf = din("c_invf", [128, 1])
        self.c_emat = din("c_emat", [64, L], BF16)
        self.c_ovl = din("c_ovl", [256, 64], BF16)
        self.y = nc.dram_tensor("y", [S, L, D], F32, kind="ExternalOutput").ap()
        self.wb = {}
        for name in ["w_in", "w_out", "cmp_w1", "cmp_w2", "s5_glu_w", "ffn_w1", "ffn_w3", "ffn_w2", "moe_w1", "moe_w3", "moe_w2",
                     "ple_gate_w", "ple_proj"]:
            shp = list(self.w[name].shape)
            if name.startswith("ffn") or name.startswith("moe"):
                nl = (n_layers + 1) // 2 if name.startswith("ffn") else n_layers // 2
                shp[0] = max(nl, 1)
            else:
                shp[0] = n_layers
            self.wb[name] = dscr("b_" + name, shp, BF16)
        self.XR = dscr("XR", [S, L, D], F32)
        self.ROPE = dscr("ROPE", [S, 2, 128, L], F32)
        self.QT = dscr("QT", [S, 8, 64, L], BF16)
        self.KCT = dscr("KCT", [S, 2, 64, L], BF16)
        self.VCT = dscr("VCT", [S, 2, 64, L], BF16)
        self.KST = dscr("KST", [S, 2, 64, L], BF16)
        self.KWT = dscr("KWT", [S, 2, 64, L], BF16)
        self.VSW = dscr("VSW", [S, L, 256], BF16)
        self.G = dscr("G", [S, L, 24], F32)
        self.ZC = dscr("ZC", [S, 512, L], F32)
        self.ZS = dscr("ZS", [S, 256, L], F32)
        self.KCC = dscr("KCC", [S, 2, 64, 256], BF16)
        self.VCA = dscr("VCA", [S, 2, 256, 129], BF16)
        self.MIXT = dscr("MIXT", [S, D, L], BF16)

        with ExitStack() as es:
            self.kb = KB(nc, es)
            if 'LOG' in self.dbg:
                self.kb.log = []
            self.build()

    def consts(self, es):
        kb = self.kb
        c = {}
        c['ident'] = kb.buf(es, "ident", [128, 128], F32)
        kb.dma(c['ident'][:, :], self.c_ident[:, :], c['ident'], W=[c['ident']])
        c['eps'] = kb.buf(es, "epsb", [128, 1], F32)
        kb.op('pool', lambda e: e.memset(c['eps'][:, :], LN_EPS), W=[c['eps']])
        c['dum'] = kb.buf(es, "dumb", [128, 2], F32)
        kb.op('pool', lambda e: e.memset(c['dum'][:, :], 0.0), W=[c['dum']])
        return c

    def act_reset(self):
        d = self.C['dum']
        self.kb.op('act', lambda e: e.activation(out=d[:, 1:2], in_=d[:, 0:1], func=AF.Sigmoid), R=[d], PW=[d])

    def ln_scratch(self, es, tag):
        kb = self.kb
        return {'st': kb.buf(es, "lnst" + tag, [128, 12], F32), 'mv': kb.buf(es, "lnmv" + tag, [128, 2], F32),
                'rs': kb.buf(es, "lnrs" + tag, [128, 2], F32), 'eps': self.C['eps']}

    def build(self):
        kb = self.kb
        with ExitStack() as ces:
            self.C = self.consts(ces)
            kb.pre_barrier = self.act_reset
            on = lambda nm: (self.only is None) or (nm in self.only)
            if on('cast'):
                self.phase_cast()
            if on('rope'):
                self.phase_rope()
            for l in range(self.n_layers):
                src = self.x if l == 0 else self.XR
                for s in range(self.n_seq):
                    if on('inproj'):
                        self.phase_inproj(l, s, src)
                    if on('compress'):
                        self.phase_compress(l, s)
                    if on('attn'):
                        self.phase_attn(l, s)
                    if on('conv'):
                        self.phase_conv(l, s)
                    if on('s5'):
                        self.phase_s5(l, s)
                if on('outproj'):
                    self.phase_outproj(l, src)
                if on('ffn'):
                    self.phase_ffn(l, moe=(l % 2 == 1))
                if on('ple'):
                    self.phase_ple(l, self.y if l == self.n_layers - 1 else self.XR)
            kb.barrier()

    def phase_cast(self):
        kb = self.kb
        CH = 3584
        with ExitStack() as es:
            fb = [kb.buf(es, "cf%d" % i, [128, CH], F32) for i in range(3)]
            bb = [kb.buf(es, "cb%d" % i, [128, CH], BF16) for i in range(3)]
            n = 0
            for name, dst in self.wb.items():
                src = self.w[name]
                nl = dst.shape[0]
                s2 = src[0:nl].flatten_outer_dims()
                d2 = dst.flatten_outer_dims()
                R, Cc = s2.shape
                assert R % 128 == 0
                rpp = R // 128
                s3 = s2.rearrange("(p r) c -> p (r c)", p=128)
                d3 = d2.rearrange("(p r) c -> p (r c)", p=128)
                tot = rpp * Cc
                for c0 in range(0, tot, CH):
                    cw = min(CH, tot - c0)
                    f = fb[n % 3]
                    b = bb[n % 3]
                    kb.dma(f[:, 0:cw], s3[:, c0:c0 + cw], f, W=[f])
                    eng = ('act', 'dve', 'pool')[n % 3]
                    if eng == 'act':
                        kb.op('act', lambda e, f=f, b=b, cw=cw: e.copy(out=b[:, 0:cw], in_=f[:, 0:cw]), R=[f], W=[b])
                    else:
                        kb.op(eng, lambda e, f=f, b=b, cw=cw: e.tensor_copy(out=b[:, 0:cw], in_=f[:, 0:cw]), R=[f], W=[b])
                    kb.dma(d3[:, c0:c0 + cw], b[:, 0:cw], b, R=[b])
                    n += 1
            kb.barrier()

    def phase_rope(self):
        kb = self.kb
        with ExitStack() as es:
            pi_ = kb.buf(es, "rp_i", [128, L], I32)
            ang = kb.buf(es, "rp_a", [128, L], F32)
            kf = kb.buf(es, "rp_k", [128, L], F32)
            ki = kb.buf(es, "rp_ki", [128, L], I32)
            r = kb.buf(es, "rp_r", [128, L], F32)
            m = kb.buf(es, "rp_m", [128, L], F32)
            o = kb.buf(es, "rp_o", [128, L], F32)
            invf = kb.buf(es, "rp_f", [128, 1], F32)
            kb.dma(invf[:, :], self.c_invf[:, :], invf, W=[invf])
            HI = 6.28125
            LO = TWO_PI - HI
            for s in range(self.n_seq):
                kb.dma(pi_[:, :], self.pos[s:s + 1, :].partition_broadcast(128), pi_, W=[pi_])
                kb.op('dve', lambda e: e.tensor_copy(out=ang[:, :], in_=pi_[:, :]), R=[pi_], W=[ang])
                kb.op('dve', lambda e: e.tensor_scalar(out=ang[:, :], in0=ang[:, :], scalar1=invf[:, 0:1], scalar2=None, op0=ALU.mult), R=[ang, invf], W=[ang])
                kb.op('dve', lambda e: e.tensor_scalar(out=kf[:, :], in0=ang[:, :], scalar1=1.0 / TWO_PI, scalar2=None, op0=ALU.mult), R=[ang], W=[kf])
                kb.op('dve', lambda e: e.tensor_copy(out=ki[:, :], in_=kf[:, :]), R=[kf], W=[ki])
                kb.op('dve', lambda e: e.tensor_copy(out=kf[:, :], in_=ki[:, :]), R=[ki], W=[kf])
                kb.op('dve', lambda e: e.scalar_tensor_tensor(out=r[:, :], in0=kf[:, :], scalar=-HI, in1=ang[:, :], op0=ALU.mult, op1=ALU.add), R=[kf, ang], W=[r])
                kb.op('dve', lambda e: e.scalar_tensor_tensor(out=r[:, :], in0=kf[:, :], scalar=-LO, in1=r[:, :], op0=ALU.mult, op1=ALU.add), R=[kf, r], W=[r])
                for which, shift in ((1, 0.0), (0, math.pi / 2)):
                    kb.op('dve', lambda e, shift=shift: e.tensor_scalar(out=o[:, :], in0=r[:, :], scalar1=shift, scalar2=None, op0=ALU.add), R=[r], W=[o])
                    for _ in range(2):
                        kb.op('dve', lambda e: e.tensor_scalar(out=m[:, :], in0=o[:, :], scalar1=math.pi, scalar2=-TWO_PI, op0=ALU.is_gt, op1=ALU.mult), R=[o], W=[m])
                        kb.op('dve', lambda e: e.tensor_tensor(out=o[:, :], in0=o[:, :], in1=m[:, :], op=ALU.add), R=[o, m], W=[o])
                        kb.op('dve', lambda e: e.tensor_scalar(out=m[:, :], in0=o[:, :], scalar1=-math.pi, scalar2=TWO_PI, op0=ALU.is_lt, op1=ALU.mult), R=[o], W=[m])
                        kb.op('dve', lambda e: e.tensor_tensor(out=o[:, :], in0=o[:, :], in1=m[:, :], op=ALU.add), R=[o, m], W=[o])
                    kb.op('dve', lambda e: e.tensor_scalar(out=o[:, :], in0=o[:, :], scalar1=math.pi, scalar2=-math.pi, op0=ALU.min, op1=ALU.max), R=[o], W=[o])
                    kb.op('act', lambda e: e.activation(out=o[:, :], in_=o[:, :], func=(AF.Identity if os.environ.get('NOSIN') else AF.Sin)), R=[o], W=[o])
                    kb.dma(self.ROPE[s, which, :, :], o[:, :], o, R=[o])
            self.act_reset()
            kb.barrier()

    def phase_inproj(self, l, s, src):
        kb = self.kb
        C = self.C
        with ExitStack() as es:
            win = kb.buf(es, "ip_w", [128, 8, INW], BF16)
            for kc in range(8):
                kb.dma(win[:, kc, :], self.wb["w_in"][l, kc * 128:(kc + 1) * 128, :], win, PW=[win])
            xt = [kb.buf(es, "ip_x%d" % i, [128, 4, D], F32) for i in range(2)]
            xT = [kb.buf(es, "ip_xT%d" % i, [128, 8, 512], BF16) for i in range(2)]
            cs = [kb.buf(es, "ip_c%d" % i, [128, 2, 512], F32) for i in range(2)]
            pss = [kb.buf(es, "ip_ps%d" % i, [128, 512], F32, psum=True) for i in range(8)]
            fa = [kb.buf(es, "ip_fa%d" % i, [128, 512], F32) for i in range(2)]
            fb = [kb.buf(es, "ip_fb%d" % i, [128, 512], F32) for i in range(2)]
            t1 = [kb.buf(es, "ip_t1%d" % i, [128, 512], F32) for i in range(2)]
            t2 = [kb.buf(es, "ip_t2%d" % i, [128, 512], F32) for i in range(2)]
            oa = [kb.buf(es, "ip_oa%d" % i, [128, 512], BF16) for i in range(2)]
            ob = [kb.buf(es, "ip_ob%d" % i, [128, 512], BF16) for i in range(2)]
            of = [kb.buf(es, "ip_of%d" % i, [128, 512], F32) for i in range(3)]
            ov = [kb.buf(es, "ip_ov%d" % i, [128, 512], BF16) for i in range(2)]
            tv = [kb.buf(es, "ip_tv%d" % i, [128, 4, 256], BF16) for i in range(2)]
            tg = [kb.buf(es, "ip_tg%d" % i, [128, 4, 24], F32) for i in range(2)]
            cnt = [0]
            pc = [0]
            rc = [0]

            def nextps():
                pc[0] += 1
                return pss[2 + pc[0] % 6]

            for i in range(NT):
                t0 = i * 512
                x_ = xt[i % 2]
                xT_ = xT[i % 2]
                cs_ = cs[i % 2]
                kb.dma(x_[:, :, :], src[s, t0:t0 + 512, :].rearrange("(a p) d -> p a d", p=128), x_, W=[x_])
                kb.dma(cs_[:, 0, :], self.ROPE[s, 0, :, t0:t0 + 512], cs_, PW=[cs_])
                kb.dma(cs_[:, 1, :], self.ROPE[s, 1, :, t0:t0 + 512], cs_, PW=[cs_])
                transpose_in(kb, x_, 4, 8, xT_, C['ident'], pss[0:2], cnt)

                def fm(c0, m, ps):
                    for kc in range(8):
                        kb.op('pe', lambda e, kc=kc: e.matmul(ps[0:m, :], lhsT=win[:, kc, c0:c0 + m], rhs=xT_[:, kc, :], start=(kc == 0), stop=(kc == 7)),
                              R=[win, xT_], PW=[ps])

                pairs = [(0, 128, 128, [(32 * h, self.QT[s, h]) for h in range(4)]),
                         (256, 384, 128, [(32 * h, self.QT[s, 4 + h]) for h in range(4)]),
                         (512, 640, 128, [(0, self.KCT[s, 0]), (32, self.KCT[s, 1]), (64, self.KST[s, 0]), (96, self.KST[s, 1])]),
                         (768, 832, 64, [(0, self.KWT[s, 0]), (32, self.KWT[s, 1])])]
                for (ca, cb, m, dests) in pairs:
                    psa = nextps()
                    psb = nextps()
                    fm(ca, m, psa)
                    fm(cb, m, psb)
                    j = rc[0] % 2
                    rc[0] += 1
                    A, B, T1, T2, OA, OB = fa[j], fb[j], t1[j], t2[j], oa[j], ob[j]
                    kb.op('act', lambda e: e.copy(out=A[0:m, :], in_=psa[0:m, :]), R=[psa], W=[A])
                    kb.op('act', lambda e: e.copy(out=B[0:m, :], in_=psb[0:m, :]), R=[psb], W=[B])
                    kb.op('dve', lambda e: e.tensor_tensor(out=T1[0:m, :], in0=A[0:m, :], in1=cs_[0:m, 0, :], op=ALU.mult), R=[A, cs_], W=[T1])
                    kb.op('pool', lambda e: e.tensor_tensor(out=T2[0:m, :], in0=B[0:m, :], in1=cs_[0:m, 1, :], op=ALU.mult), R=[B, cs_], W=[T2])
                    kb.op('dve', lambda e: e.tensor_tensor(out=OA[0:m, :], in0=T1[0:m, :], in1=T2[0:m, :], op=ALU.subtract), R=[T1, T2], W=[OA])
                    kb.op('pool', lambda e: e.tensor_tensor(out=T1[0:m, :], in0=B[0:m, :], in1=cs_[0:m, 0, :], op=ALU.mult), R=[B, cs_], W=[T1])
                    kb.op('dve', lambda e: e.tensor_tensor(out=T2[0:m, :], in0=A[0:m, :], in1=cs_[0:m, 1, :], op=ALU.mult), R=[A, cs_], W=[T2])
                    kb.op('pool', lambda e: e.tensor_tensor(out=OB[0:m, :], in0=T1[0:m, :], in1=T2[0:m, :], op=ALU.add), R=[T1, T2], W=[OB])
                    for (ro, dst) in dests:
                        kb.dma(dst[0:32, t0:t0 + 512], OA[ro:ro + 32, :], OA, R=[OA])
                        kb.dma(dst[32:64, t0:t0 + 512], OB[ro:ro + 32, :], OB, R=[OB])
                ps = nextps()
                fm(896, 128, ps)
                o_ = ov[i % 2]
                kb.op('act', lambda e: e.copy(out=o_[:, :], in_=ps[:, :]), R=[ps], W=[o_])
                kb.dma(self.VCT[s, 0, :, t0:t0 + 512], o_[0:64, :], o_, R=[o_])
                kb.dma(self.VCT[s, 1, :, t0:t0 + 512], o_[64:128, :], o_, R=[o_])
                for ci in range(6):
                    ps = nextps()
                    fm(1024 + ci * 128, 128, ps)
                    o_ = of[ci % 3]
                    if ci % 2 == 0:
                        kb.op('act', lambda e, o_=o_, ps=ps: e.copy(out=o_[:, :], in_=ps[:, :]), R=[ps], W=[o_])
                    else:
                        kb.op('dve', lambda e, o_=o_, ps=ps: e.tensor_copy(out=o_[:, :], in_=ps[:, :]), R=[ps], W=[o_])
                    if ci < 4:
                        kb.dma(self.ZC[s, ci * 128:(ci + 1) * 128, t0:t0 + 512], o_[:, :], o_, R=[o_])
                    else:
                        kb.dma(self.ZS[s, (ci - 4) * 128:(ci - 3) * 128, t0:t0 + 512], o_[:, :], o_, R=[o_])
                tv_ = tv[i % 2]
                tg_ = tg[i % 2]
                for sub in range(4):
                    ps = nextps()
                    for kc in range(8):
                        kb.op('pe', lambda e, kc=kc, sub=sub, ps=ps: e.matmul(ps[:, 0:280], lhsT=xT_[:, kc, sub * 128:(sub + 1) * 128], rhs=win[:, kc, 1792:2072], start=(kc == 0), stop=(kc == 7)),
                              R=[win, xT_], PW=[ps])
                    kb.op('dve', lambda e, sub=sub, ps=ps: e.tensor_copy(out=tv_[:, sub, :], in_=ps[:, 0:256]), R=[ps], PW=[tv_])
                    kb.op('act', lambda e, sub=sub, ps=ps: e.activation(out=tg_[:, sub, :], in_=ps[:, 256:280], func=AF.Sigmoid), R=[ps], PW=[tg_])
                kb.dma(self.VSW[s, t0:t0 + 512, :].rearrange("(a p) d -> p a d", p=128), tv_[:, :, :], tv_, R=[tv_])
                kb.dma(self.G[s, t0:t0 + 512, :].rearrange("(a p) d -> p a d", p=128), tg_[:, :, :], tg_, R=[tg_])
            kb.barrier()

    def phase_compress(self, l, s):
        kb = self.kb
        with ExitStack() as es:
            w1 = [kb.buf(es, "cp_w1%d" % j, [64, 32, 256], BF16) for j in range(2)]
            w2 = [kb.buf(es, "cp_w2%d" % j, [128, 2, 64], BF16) for j in range(2)]
            b1 = [kb.buf(es, "cp_b1%d" % j, [128, 2], F32) for j in range(2)]
            peT = [kb.buf(es, "cp_pe%d" % j, [64, 32], F32) for j in range(2)]
            b2k = kb.buf(es, "cp_b2k", [64, 1], F32)
            b2v = kb.buf(es, "cp_b2v", [128, 64], F32)
            for j in range(2):
                kb.dma(w1[j][:, :, :], self.wb["cmp_w1"][l, j].rearrange("(t d) h -> d t h", d=64), w1[j], W=[w1[j]])
                kb.dma(w2[j][:, :, :], self.wb["cmp_w2"][l, j].rearrange("(c p) o -> p c o", p=128), w2[j], W=[w2[j]])
                kb.dma(b1[j][:, :], self.w["cmp_b1"][l, j].rearrange("(c p) -> p c", p=128), b1[j], W=[b1[j]], allow_slow_non_contiguous=True)
                kb.dma(peT[j][:, :], self.w["cmp_pe"][l, j].rearrange("t d -> d t"), peT[j], W=[peT[j]], allow_slow_non_contiguous=True)
            kb.dma(b2k[:, :], self.w["cmp_b2"][l, 0].rearrange("(p o) -> p o", o=1), b2k, W=[b2k], allow_slow_non_contiguous=True)
            kb.dma(b2v[:, :], self.w["cmp_b2"][l, 1:2, :].partition_broadcast(128), b2v, W=[b2v])
            kt = [kb.buf(es, "cp_kt%d" % i, [64, L], BF16) for i in range(2)]
            blk = [kb.buf(es, "cp_blk%d" % i, [64, 32, 256], BF16) for i in range(2)]
            u = kb.buf(es, "cp_u", [128, 256], F32)
            tmp = kb.buf(es, "cp_tmp", [128, 256], F32)
            gl = kb.buf(es, "cp_gl", [128, 2, 256], BF16)
            okc = kb.buf(es, "cp_okc", [64, 256], BF16)
            ovc = kb.buf(es, "cp_ovc", [128, 2, 129], BF16)
            pss = [kb.buf(es, "cp_ps%d" % i, [128, 512], F32, psum=True) for i in range(4)]
            n = 0
            for h in range(2):
                for j in range(2):
                    srcT = (self.KCT, self.VCT)[j][s, h]
                    kt_ = kt[n % 2]
                    blk_ = blk[n % 2]
                    kb.dma(kt_[:, :], srcT[:, :], kt_, W=[kt_])
                    ktv = kt_[:, :].rearrange("d (n t) -> d t n", t=16)
                    for tb in range(32):
                        eng = kb.ew()
                        if tb < 16:
                            src_ap = ktv[:, tb, 0:255]
                        else:
                            src_ap = ktv[:, tb - 16, 1:256]
                        kb.op(eng, lambda e, tb=tb, src_ap=src_ap: e.tensor_scalar(out=blk_[:, tb, 0:255], in0=src_ap, scalar1=peT[j][:, tb:tb + 1], scalar2=None, op0=ALU.add),
                              R=[kt_, peT[j]], PW=[blk_])
                    for hc in range(2):
                        ps = pss[hc]
                        for tb in range(32):
                            kb.op('pe', lambda e, tb=tb, hc=hc, ps=ps: e.matmul(ps[:, 0:255], lhsT=w1[j][:, tb, hc * 128:(hc + 1) * 128], rhs=blk_[:, tb, 0:255], start=(tb == 0), stop=(tb == 31)),
                                  R=[w1[j], blk_], PW=[ps])
                        kb.op('act', lambda e, hc=hc, ps=ps: e.activation(out=u[:, 0:255], in_=ps[:, 0:255], func=AF.Identity, bias=b1[j][:, hc:hc + 1], scale=1.0), R=[ps, b1[j]], W=[u])
                        gelu_tanh(kb, u, tmp, gl[:, hc, 0:255], gl, 255)
                    if j == 0:
                        ps = pss[2]
                        for hc in range(2):
                            kb.op('pe', lambda e, hc=hc: e.matmul(ps[0:64, 0:255], lhsT=w2[0][:, hc, :], rhs=gl[:, hc, 0:255], start=(hc == 0), stop=(hc == 1)), R=[w2[0], gl], PW=[ps])
                        kb.op('pool', lambda e: e.memset(okc[:, :], 0.0), W=[okc])
                        kb.op('act', lambda e: e.activation(out=okc[:, 0:255], in_=ps[0:64, 0:255], func=AF.Identity, bias=b2k[:, 0:1], scale=1.0), R=[ps, b2k], PW=[okc])
                        kb.dma(self.KCC[s, h, :, :], okc[:, :], okc, R=[okc])
                    else:
                        kb.op('pool', lambda e: e.memset(ovc[:, :, :], 0.0), W=[ovc])
                        kb.dma(ovc[:, :, 65:129], self.c_ovl.rearrange("(c p) o -> p c o", p=128), ovc, PW=[ovc])
                        kb.op('pool', lambda e: e.memset(ovc[:, :, 64:65], 1.0), PW=[ovc])
                        for c in range(2):
                            rows = 128 if c == 0 else 127
                            ps = pss[2 + c]
                            for hc in range(2):
                                kb.op('pe', lambda e, hc=hc, c=c, rows=rows, ps=ps: e.matmul(ps[0:rows, 0:64], lhsT=gl[:, hc, c * 128:c * 128 + rows], rhs=w2[1][:, hc, :], start=(hc == 0), stop=(hc == 1)),
                                      R=[w2[1], gl], PW=[ps])
                            kb.op('dve', lambda e, c=c, rows=rows, ps=ps: e.tensor_tensor(out=ovc[0:rows, c, 0:64], in0=ps[0:rows, 0:64], in1=b2v[0:rows, :], op=ALU.add), R=[ps, b2v], PW=[ovc])
                        kb.dma(self.VCA[s, h].rearrange("(c p) o -> p c o", p=128), ovc[:, :, :], ovc, R=[ovc])
                    n += 1
            kb.barrier()

    def phase_attn(self, l, s):
        kb = self.kb
        C = self.C
        with ExitStack() as es:
            kcT = kb.buf(es, "at_kc", [64, 256], BF16)
            vca = kb.buf(es, "at_vca", [128, 2, 129], BF16)
            kaug = kb.buf(es, "at_kaug", [128, L], BF16)
            kw = kb.buf(es, "at_kw", [64, L], BF16)
            vs = kb.buf(es, "at_vs", [128, 32, 65], BF16)
            vw = kb.buf(es, "at_vw", [128, 32, 65], BF16)
            qa = [[kb.buf(es, "at_q%d_%d" % (g, i), [128, 512], BF16) for i in range(2)] for g in range(4)]
            gt = [kb.buf(es, "at_g%d" % i, [128, 4, 24], F32) for i in range(2)]
            oacc = kb.buf(es, "at_oacc", [128, 4, 256], F32)
            imp = kb.buf(es, "at_imp", [128, 4, 64], F32)
            sc2 = kb.buf(es, "at_sc2", [128, 64], F32)
            selm = kb.buf(es, "at_selm", [128, 64], F32)
            t8 = kb.buf(es, "at_t8", [128, 16], F32)
            rv = kb.buf(es, "at_rv", [128, 4], F32)
            pt = [kb.buf(es, "at_pt%d" % i, [128, 512], BF16) for i in range(3)]
            mixo = [kb.buf(es, "at_mo%d" % i, [128, 2, 512], BF16) for i in range(2)]
            ps_s = [kb.buf(es, "at_pss%d" % i, [128, 512], F32, psum=True) for i in range(2)]
            ps_a = [kb.buf(es, "at_psa%d" % i, [128, 512], F32, psum=True) for i in range(4)]
            ps_t = [kb.buf(es, "at_pst%d" % i, [128, 512], F32, psum=True) for i in range(2)]
            kb.dma(kaug[64:128, :], self.c_emat[:, :], kaug, PW=[kaug])
            kb.op('pool', lambda e: e.memset(vs[:, :, 64:65], 1.0), PW=[vs])
            kb.op('pool', lambda e: e.memset(vw[:, :, 64:65], 1.0), PW=[vw])
            sc = [0]
            pcnt = [0]
            tcnt = [0]
            for k in range(2):
                kb.dma(kcT[:, :], self.KCC[s, k], kcT, W=[kcT])
                kb.dma(vca[:, :, :], self.VCA[s, k].rearrange("(c p) o -> p c o", p=128), vca, W=[vca])
                kb.dma(kaug[0:64, :], self.KST[s, k], kaug, PW=[kaug])
                kb.dma(kw[:, :], self.KWT[s, k], kw, W=[kw])
                kb.dma(vs[:, :, 0:64], self.VSW[s, :, k * 64:(k + 1) * 64].rearrange("(c p) d -> p c d", p=128), vs, PW=[vs])
                kb.dma(vw[:, :, 0:64], self.VSW[s, :, 128 + k * 64:128 + (k + 1) * 64].rearrange("(c p) d -> p c d", p=128), vw, PW=[vw])
                for i in range(NT):
                    q0 = i * 512
                    Q = [qa[g][i % 2] for g in range(4)]
                    gt_ = gt[i % 2]
                    for g in range(4):
                        kb.dma(Q[g][0:64, :], self.QT[s, 4 * k + g, :, q0:q0 + 512], Q[g], PW=[Q[g]])
                    kb.dma(gt_[:, :, :], self.G[s, q0:q0 + 512, :].rearrange("(a p) d -> p a d", p=128), gt_, W=[gt_])

                    def score(lhsT_ap, lbufs, g, krows, c0, c1, rows=128):
                        ps = ps_s[sc[0] % 2]
                        sc[0] += 1
                        kb.op('pe', lambda e: e.matmul(ps[0:rows, c0:c1], lhsT=lhsT_ap, rhs=Q[g][0:krows, c0:c1], start=True, stop=True), R=lbufs + [Q[g]], W=[ps])
                        p_ = pt[pcnt[0] % 3]
                        pcnt[0] += 1
                        kb.op('act', lambda e: e.activation(out=p_[0:rows, c0:c1], in_=ps[0:rows, c0:c1], func=AF.Exp, scale=0.125), R=[ps], W=[p_])
                        return p_

                    def finish(g, br, first, with_imp=False):
                        col = 3 * (4 * k + g) + br
                        for sub in range(4):
                            a = ps_a[sub]
                            kb.op('dve', lambda e, a=a, sub=sub: e.tensor_scalar(out=rv[:, sub:sub + 1], in0=a[:, 64:65], scalar1=1e-30, scalar2=None, op0=ALU.max), R=[a], PW=[rv])
                        kb.op('dve', lambda e: e.reciprocal(out=rv[:, 0:4], in_=rv[:, 0:4]), R=[rv], W=[rv])
                        for sub in range(4):
                            a = ps_a[sub]
                            if with_imp:
                                if g == 0:
                                    kb.op('dve', lambda e, a=a, sub=sub: e.tensor_scalar(out=imp[:, sub, :], in0=a[:, 65:129], scalar1=rv[:, sub:sub + 1], scalar2=None, op0=ALU.mult), R=[a, rv], PW=[imp])
                                else:
                                    kb.op('dve', lambda e, a=a, sub=sub: e.scalar_tensor_tensor(out=imp[:, sub, :], in0=a[:, 65:129], scalar=rv[:, sub:sub + 1], in1=imp[:, sub, :], op0=ALU.mult, op1=ALU.add), R=[a, rv, imp], PW=[imp])
                        kb.op('dve', lambda e, col=col: e.tensor_tensor(out=rv[:, 0:4], in0=rv[:, 0:4], in1=gt_[:, :, col], op=ALU.mult), R=[rv, gt_], W=[rv])
                        for sub in range(4):
                            a = ps_a[sub]
                            if first:
                                kb.op('dve', lambda e, a=a, sub=sub: e.tensor_scalar(out=oacc[:, sub, g * 64:(g + 1) * 64], in0=a[:, 0:64], scalar1=rv[:, sub:sub + 1], scalar2=None, op0=ALU.mult), R=[a, rv], PW=[oacc])
                            else:
                                kb.op('dve', lambda e, a=a, sub=sub: e.scalar_tensor_tensor(out=oacc[:, sub, g * 64:(g + 1) * 64], in0=a[:, 0:64], scalar=rv[:, sub:sub + 1], in1=oacc[:, sub, g * 64:(g + 1) * 64], op0=ALU.mult, op1=ALU.add), R=[a, rv, oacc], PW=[oacc])

                    ncnt = min(255, 32 * i + 31)
                    chunks = [(0, min(128, ncnt))] + ([(1, ncnt - 128)] if ncnt > 128 else [])
                    for g in range(4):
                        for (c, rows) in chunks:
                            p_ = score(kcT[0:64, c * 128:c * 128 + rows], [kcT], g, 64, 0, 512, rows)
                            last_end = 16 * (c * 128 + rows - 1) + 31
                            if last_end > q0:
                                kb.op('pool', lambda e, p_=p_, c=c, rows=rows: e.affine_select(out=p_[0:rows, :], in_=p_[0:rows, :], pattern=[[1, 512]], compare_op=ALU.is_ge, fill=0.0,
                                                                                         base=q0 - 31 - 16 * 128 * c, channel_multiplier=-16), R=[p_], W=[p_])
                            for sub in range(4):
                                kb.op('pe', lambda e, p_=p_, c=c, rows=rows, sub=sub: e.matmul(ps_a[sub][:, 0:129], lhsT=p_[0:rows, sub * 128:(sub + 1) * 128], rhs=vca[0:rows, c, :],
                                                                                         start=(c == 0), stop=(c == chunks[-1][0])), R=[p_, vca], PW=[ps_a[sub]])
                        finish(g, 0, True, with_imp=True)
                    for sub in range(4):
                        b0 = 8 * i + 2 * sub
                        for hh in range(2):
                            b = b0 + hh
                            r0 = 64 * hh
                            if b + 1 < 64:
                                kb.op('pool', lambda e, sub=sub, r0=r0, b=b: e.memset(imp[r0:r0 + 64, sub, b + 1:64], -1.0), PW=[imp])
                            kb.op('pool', lambda e, sub=sub, r0=r0, b=b: e.memset(imp[r0:r0 + 64, sub, max(b - 1, 0):b + 1], 1e9), PW=[imp])
                            kb.op('pool', lambda e, sub=sub, r0=r0: e.memset(imp[r0:r0 + 64, sub, 0:1], 1e9), PW=[imp])
                        kb.op('dve', lambda e, sub=sub: e.max(out=t8[:, 0:8], in_=imp[:, sub, :]), R=[imp], PW=[t8])
                        kb.op('dve', lambda e, sub=sub: e.match_replace(out=sc2[:, :], in_to_replace=t8[:, 0:8], in_values=imp[:, sub, :], imm_value=-1e30), R=[imp, t8], W=[sc2])
                        kb.op('dve', lambda e: e.max(out=t8[:, 8:16], in_=sc2[:, :]), R=[sc2], PW=[t8])
                        kb.op('dve', lambda e, sub=sub: e.tensor_scalar(out=selm[:, :], in0=imp[:, sub, :], scalar1=t8[:, 15:16], scalar2=None, op0=ALU.is_ge), R=[imp, t8], W=[selm])
                        pst = ps_t[tcnt[0] % 2]
                        tcnt[0] += 1
                        kb.op('pe', lambda e, pst=pst: e.transpose(pst[0:64, 0:128], selm[:, :], C['ident'][:, :]), R=[selm, C['ident']], W=[pst])
                        for g in range(4):
                            kb.op('act', lambda e, g=g, sub=sub, pst=pst: e.activation(out=Q[g][64:128, sub * 128:(sub + 1) * 128], in_=pst[0:64, 0:128], func=AF.Identity, bias=-BIG, scale=BIG),
                                  R=[pst], PW=[Q[g]])
                    for g in range(4):
                        nch = 4 * i + 4
                        for c in range(nch):
                            off = 128 * c - q0
                            c0 = max(off, 0)
                            p_ = score(kaug[:, c * 128:(c + 1) * 128], [kaug], g, 128, c0, 512)
                            if off >= 0:
                                kb.op('pool', lambda e, p_=p_, c0=c0: e.affine_select(out=p_[:, c0:c0 + 128], in_=p_[:, c0:c0 + 128], pattern=[[1, 128]], compare_op=ALU.is_ge, fill=0.0,
                                                                                base=0, channel_multiplier=-1), R=[p_], W=[p_])
                            for sub in range(4):
                                if sub * 128 < c0:
                                    continue
                                kb.op('pe', lambda e, p_=p_, c=c, sub=sub: e.matmul(ps_a[sub][:, 0:65], lhsT=p_[:, sub * 128:(sub + 1) * 128], rhs=vs[:, c, :],
                                                                               start=(c == 0), stop=(c == 4 * i + sub)), R=[p_, vs], PW=[ps_a[sub]])
                        finish(g, 1, False)
                    for g in range(4):
                        for c in range(max(0, 4 * i - 4), 4 * i + 4):
                            off = 128 * c - q0
                            if off >= 0:
                                c0, c1 = off, 512
                            else:
                                m = (off + 512) // 128
                                c0, c1 = 0, 128 * (m + 1)
                            p_ = score(kw[0:64, c * 128:(c + 1) * 128], [kw], g, 64, c0, c1)
                            if off >= 0:
                                kb.op('pool', lambda e, p_=p_, c0=c0: e.affine_select(out=p_[:, c0:c0 + 128], in_=p_[:, c0:c0 + 128], pattern=[[1, 128]], compare_op=ALU.is_ge, fill=0.0,
                                                                                base=0, channel_multiplier=-1), R=[p_], W=[p_])
                            else:
                                kb.op('pool', lambda e, p_=p_, c1=c1: e.affine_select(out=p_[:, c1 - 128:c1], in_=p_[:, c1 - 128:c1], pattern=[[-1, 128]], compare_op=ALU.is_ge, fill=0.0,
                                                                                base=-1, channel_multiplier=1), R=[p_], W=[p_])
                            for sub in range(4):
                                if not (c0 <= sub * 128 < c1):
                                    continue
                                kb.op('pe', lambda e, p_=p_, c=c, sub=sub: e.matmul(ps_a[sub][:, 0:65], lhsT=p_[:, sub * 128:(sub + 1) * 128], rhs=vw[:, c, :],
                                                                               start=(c == max(0, 4 * i + sub - 4)), stop=(c == 4 * i + sub)), R=[p_, vw], PW=[ps_a[sub]])
                        finish(g, 2, False)
                    mo = mixo[i % 2]
                    for fc in range(2):
                        pst = ps_t[tcnt[0] % 2]
                        tcnt[0] += 1
                        for sub in range(4):
                            kb.op('pe', lambda e, fc=fc, sub=sub, pst=pst: e.transpose(pst[:, sub * 128:(sub + 1) * 128], oacc[:, sub, fc * 128:(fc + 1) * 128], C['ident'][:, :]),
                                  R=[oacc, C['ident']], PW=[pst])
                        kb.op('act', lambda e, fc=fc, pst=pst: e.copy(out=mo[:, fc, :], in_=pst[:, :]), R=[pst], PW=[mo])
                    kb.dma(self.MIXT[s, k * 256:(k + 1) * 256, q0:q0 + 512].rearrange("(c p) t -> p c t", p=128), mo[:, :, :], mo, R=[mo])
            kb.barrier()

    def phase_conv(self, l, s):
        kb = self.kb
        with ExitStack() as es:
            cw = kb.buf(es, "cv_w", [128, 2, 31], F32)
            cb = kb.buf(es, "cv_b", [128, 2], F32)
            lg = kb.buf(es, "cv_g", [128, 2], F32)
            lb = kb.buf(es, "cv_lb", [128, 2], F32)
            ones = kb.buf(es, "cv_ones", [128, 128], F32)
            for c in range(2):
                kb.dma(cw[:, c, :], self.w["conv_w"][l][:, c * 128:(c + 1) * 128].rearrange("k p -> p k"), cw, PW=[cw], allow_slow_non_contiguous=True)
            for (t_, nm) in ((cb, "conv_b"), (lg, "conv_ln_g"), (lb, "conv_ln_b")):
                kb.dma(t_[:, :], self.w[nm][l].rearrange("(c p) -> p c", p=128), t_, W=[t_], allow_slow_non_contiguous=True)
            kb.op('pool', lambda e: e.memset(ones[:, :], 1.0 / 256.0), W=[ones])
            a = [kb.buf(es, "cv_a%d" % c, [128, L], F32) for c in range(2)]
            u = [kb.buf(es, "cv_u%d" % c, [128, L + 32], F32) for c in range(2)]
            y = [kb.buf(es, "cv_y%d" % c, [128, L], F32) for c in range(2)]
            sq = [kb.buf(es, "cv_sq%d" % c, [128, 512], F32) for c in range(2)]
            mean = kb.buf(es, "cv_mean", [128, 512], F32)
            rstd = kb.buf(es, "cv_rstd", [128, 512], F32)
            tmp = kb.buf(es, "cv_tmp", [128, 512], F32)
            ob = [kb.buf(es, "cv_ob%d" % c, [128, 512], BF16) for c in range(2)]
            ps1 = kb.buf(es, "cv_ps1", [128, 512], F32, psum=True)
            ps2 = kb.buf(es, "cv_ps2", [128, 512], F32, psum=True)
            eps = self.C['eps']
            for c in range(2):
                kb.dma(a[c][:, :], self.ZC[s, c * 128:(c + 1) * 128, :], a[c], W=[a[c]])
                kb.dma(y[c][:, :], self.ZC[s, 256 + c * 128:256 + (c + 1) * 128, :], y[c], W=[y[c]])
                kb.op('act', lambda e, c=c: e.activation(out=y[c][:, :], in_=y[c][:, :], func=AF.Sigmoid), R=[y[c]], W=[y[c]])
                kb.op('pool', lambda e, c=c: e.memset(u[c][:, 0:32], 0.0), PW=[u[c]])
                kb.op('pool', lambda e, c=c: e.tensor_tensor(out=u[c][:, 32:L + 32], in0=a[c][:, :], in1=y[c][:, :], op=ALU.mult), R=[a[c], y[c]], PW=[u[c]])
                kb.op('dve', lambda e, c=c: e.tensor_scalar(out=y[c][:, :], in0=u[c][:, 2:L + 2], scalar1=cw[:, c, 0:1], scalar2=cb[:, c:c + 1], op0=ALU.mult, op1=ALU.add), R=[u[c], cw, cb], W=[y[c]])
                for k in range(1, 31):
                    kb.op('dve', lambda e, c=c, k=k: e.scalar_tensor_tensor(out=y[c][:, :], in0=u[c][:, 2 + k:L + 2 + k], scalar=cw[:, c, k:k + 1], in1=y[c][:, :], op0=ALU.mult, op1=ALU.add),
                          R=[u[c], cw, y[c]], W=[y[c]])
            for i in range(NT):
                t0 = i * 512
                for c in range(2):
                    kb.op('act', lambda e, c=c: e.activation(out=sq[c][:, :], in_=y[c][:, t0:t0 + 512], func=AF.Square), R=[y[c]], W=[sq[c]])
                for c in range(2):
                    kb.op('pe', lambda e, c=c: e.matmul(ps1[:, :], lhsT=ones[:, :], rhs=y[c][:, t0:t0 + 512], start=(c == 0), stop=(c == 1)), R=[ones, y[c]], PW=[ps1])
                for c in range(2):
                    kb.op('pe', lambda e, c=c: e.matmul(ps2[:, :], lhsT=ones[:, :], rhs=sq[c][:, :], start=(c == 0), stop=(c == 1)), R=[ones, sq[c]], PW=[ps2])
                kb.op('act', lambda e: e.copy(out=mean[:, :], in_=ps1[:, :]), R=[ps1], W=[mean])
                kb.op('dve', lambda e: e.tensor_tensor(out=tmp[:, :], in0=mean[:, :], in1=mean[:, :], op=ALU.mult), R=[mean], W=[tmp])
                kb.op('dve', lambda e: e.tensor_tensor(out=rstd[:, :], in0=ps2[:, :], in1=tmp[:, :], op=ALU.subtract), R=[ps2, tmp], W=[rstd])
                kb.op('act', lambda e: e.activation(out=rstd[:, :], in_=rstd[:, :], func=AF.Sqrt, bias=eps[:, 0:1], scale=1.0), R=[rstd, eps], W=[rstd])
                kb.op('dve', lambda e: e.reciprocal(out=rstd[:, :], in_=rstd[:, :]), R=[rstd], W=[rstd])
                for c in range(2):
                    kb.op('dve', lambda e, c=c: e.tensor_tensor(out=tmp[:, :], in0=y[c][:, t0:t0 + 512], in1=mean[:, :], op=ALU.subtract), R=[y[c], mean], W=[tmp])
                    kb.op('pool', lambda e: e.tensor_tensor(out=tmp[:, :], in0=tmp[:, :], in1=rstd[:, :], op=ALU.mult), R=[tmp, rstd], W=[tmp])
                    o_ = ob[c]
                    kb.op('act', lambda e, c=c, o_=o_: e.activation(out=o_[:, :], in_=tmp[:, :], func=AF.Silu, bias=lb[:, c:c + 1], scale=lg[:, c:c + 1]), R=[tmp, lb, lg], W=[o_])
                    kb.dma(self.MIXT[s, 512 + c * 128:512 + (c + 1) * 128, t0:t0 + 512], o_[:, :], o_, R=[o_])
            kb.barrier()

    def sincos_small(self, es, th, n, cs_out, sn_out, tag):
        kb = self.kb
        kf = kb.buf(es, "sc_kf" + tag, [128, n], F32)
        ki = kb.buf(es, "sc_ki" + tag, [128, n], I32)
        r = kb.buf(es, "sc_r" + tag, [128, n], F32)
        m = kb.buf(es, "sc_m" + tag, [128, n], F32)
        HI = 6.28125
        LO = TWO_PI - HI
        kb.op('dve', lambda e: e.tensor_scalar(out=kf[:, :], in0=th[:, :], scalar1=1.0 / TWO_PI, scalar2=None, op0=ALU.mult), R=[th], W=[kf])
        kb.op('dve', lambda e: e.tensor_copy(out=ki[:, :], in_=kf[:, :]), R=[kf], W=[ki])
        kb.op('dve', lambda e: e.tensor_copy(out=kf[:, :], in_=ki[:, :]), R=[ki], W=[kf])
        kb.op('dve', lambda e: e.scalar_tensor_tensor(out=r[:, :], in0=kf[:, :], scalar=-HI, in1=th[:, :], op0=ALU.mult, op1=ALU.add), R=[kf, th], W=[r])
        kb.op('dve', lambda e: e.scalar_tensor_tensor(out=r[:, :], in0=kf[:, :], scalar=-LO, in1=r[:, :], op0=ALU.mult, op1=ALU.add), R=[kf, r], W=[r])
        for o, shift in ((sn_out, 0.0), (cs_out, math.pi / 2)):
            kb.op('dve', lambda e, o=o, shift=shift: e.tensor_scalar(out=o[:, :], in0=r[:, :], scalar1=shift, scalar2=None, op0=ALU.add), R=[r], W=[o])
            for _ in range(2):
                kb.op('dve', lambda e, o=o: e.tensor_scalar(out=m[:, :], in0=o[:, :], scalar1=math.pi, scalar2=-TWO_PI, op0=ALU.is_gt, op1=ALU.mult), R=[o], W=[m])
                kb.op('dve', lambda e, o=o: e.tensor_tensor(out=o[:, :], in0=o[:, :], in1=m[:, :], op=ALU.add), R=[o, m], W=[o])
                kb.op('dve', lambda e, o=o: e.tensor_scalar(out=m[:, :], in0=o[:, :], scalar1=-math.pi, scalar2=TWO_PI, op0=ALU.is_lt, op1=ALU.mult), R=[o], W=[m])
                kb.op('dve', lambda e, o=o: e.tensor_tensor(out=o[:, :], in0=o[:, :], in1=m[:, :], op=ALU.add), R=[o, m], W=[o])
            kb.op('dve', lambda e, o=o: e.tensor_scalar(out=o[:, :], in0=o[:, :], scalar1=math.pi, scalar2=-math.pi, op0=ALU.min, op1=ALU.max), R=[o], W=[o])
            kb.op('act', lambda e, o=o: e.activation(out=o[:, :], in_=o[:, :], func=AF.Sin), R=[o], W=[o])
        self.act_reset()

    def phase_s5(self, l, s):
        kb = self.kb
        C = self.C
        TT = ALU.mult
        with ExitStack() as es:
            are = kb.buf(es, "s5_are", [128, 8], F32)
            aim = kb.buf(es, "s5_aim", [128, 8], F32)
            dt = kb.buf(es, "s5_dt", [128, 8], F32)
            kb.dma(are[:, :], self.w["s5_a_re"][l].rearrange("g n -> (g n)").rearrange("(m p) -> p m", p=128), are, W=[are], allow_slow_non_contiguous=True)
            kb.dma(aim[:, :], self.w["s5_a_im"][l].rearrange("g n -> (g n)").rearrange("(m p) -> p m", p=128), aim, W=[aim], allow_slow_non_contiguous=True)
            ldt2 = self.w["s5_log_dt"][l:l + 1, :].rearrange("o (m h) -> o h m", h=2)
            kb.dma(dt[0:64, :], ldt2[:, 0, :].partition_broadcast(64), dt, PW=[dt], allow_slow_non_contiguous=True)
            kb.dma(dt[64:128, :], ldt2[:, 1, :].partition_broadcast(64), dt, PW=[dt], allow_slow_non_contiguous=True)
            kb.op('act', lambda e: e.activation(out=dt[:, :], in_=dt[:, :], func=AF.Exp), R=[dt], W=[dt])
            rho = kb.buf(es, "s5_rho", [128, 8], F32)
            th = kb.buf(es, "s5_th", [128, 8], F32)
            kb.op('dve', lambda e: e.tensor_tensor(out=rho[:, :], in0=are[:, :], in1=dt[:, :], op=TT), R=[are, dt], W=[rho])
            kb.op('act', lambda e: e.activation(out=rho[:, :], in_=rho[:, :], func=AF.Exp), R=[rho], W=[rho])
            kb.op('dve', lambda e: e.tensor_tensor(out=th[:, :], in0=aim[:, :], in1=dt[:, :], op=TT), R=[aim, dt], W=[th])
            c1 = kb.buf(es, "s5_c1", [128, 8], F32)
            s1 = kb.buf(es, "s5_s1", [128, 8], F32)
            self.sincos_small(es, th, 8, c1, s1, "a")
            abr = kb.buf(es, "s5_abr", [128, 8], F32)
            abi = kb.buf(es, "s5_abi", [128, 8], F32)
            den = kb.buf(es, "s5_den", [128, 8], F32)
            t1 = kb.buf(es, "s5_t1", [128, 8], F32)
            cfr = kb.buf(es, "s5_cfr", [128, 8], F32)
            cfi = kb.buf(es, "s5_cfi", [128, 8], F32)
            V = lambda fn, R, W: kb.op('dve', fn, R=R, W=W)
            V(lambda e: e.tensor_tensor(out=abr[:, :], in0=rho[:, :], in1=c1[:, :], op=TT), [rho, c1], [abr])
            V(lambda e: e.tensor_tensor(out=abi[:, :], in0=rho[:, :], in1=s1[:, :], op=TT), [rho, s1], [abi])
            V(lambda e: e.tensor_tensor(out=den[:, :], in0=are[:, :], in1=are[:, :], op=TT), [are], [den])
            V(lambda e: e.tensor_tensor(out=t1[:, :], in0=aim[:, :], in1=aim[:, :], op=TT), [aim], [t1])
            V(lambda e: e.tensor_tensor(out=den[:, :], in0=den[:, :], in1=t1[:, :], op=ALU.add), [den, t1], [den])
            V(lambda e: e.reciprocal(out=den[:, :], in_=den[:, :]), [den], [den])
            V(lambda e: e.tensor_scalar(out=abr[:, :], in0=abr[:, :], scalar1=-1.0, scalar2=None, op0=ALU.add), [abr], [abr])
            V(lambda e: e.tensor_tensor(out=cfr[:, :], in0=abr[:, :], in1=are[:, :], op=TT), [abr, are], [cfr])
            V(lambda e: e.tensor_tensor(out=t1[:, :], in0=abi[:, :], in1=aim[:, :], op=TT), [abi, aim], [t1])
            V(lambda e: e.tensor_tensor(out=cfr[:, :], in0=cfr[:, :], in1=t1[:, :], op=ALU.add), [cfr, t1], [cfr])
            V(lambda e: e.tensor_tensor(out=cfr[:, :], in0=cfr[:, :], in1=den[:, :], op=TT), [cfr, den], [cfr])
            V(lambda e: e.tensor_tensor(out=cfi[:, :], in0=abi[:, :], in1=are[:, :], op=TT), [abi, are], [cfi])
            V(lambda e: e.tensor_tensor(out=t1[:, :], in0=abr[:, :], in1=aim[:, :], op=TT), [abr, aim], [t1])
            V(lambda e: e.tensor_tensor(out=cfi[:, :], in0=cfi[:, :], in1=t1[:, :], op=ALU.subtract), [cfi, t1], [cfi])
            V(lambda e: e.tensor_tensor(out=cfi[:, :], in0=cfi[:, :], in1=den[:, :], op=TT), [cfi, den], [cfi])
            bre = kb.buf(es, "s5_bre", [128, 8, 16], F32)
            bim = kb.buf(es, "s5_bim", [128, 8, 16], F32)
            kb.dma(bre[:, :, :], self.w["s5_b_re"][l].rearrange("g n c -> (g n) c").rearrange("(m p) c -> p m c", p=128), bre, W=[bre])
            kb.dma(bim[:, :, :], self.w["s5_b_im"][l].rearrange("g n c -> (g n) c").rearrange("(m p) c -> p m c", p=128), bim, W=[bim])
            bpad = [kb.buf(es, "s5_bpad%d" % j, [128, 8, 128], F32) for j in range(2)]
            t16 = kb.buf(es, "s5_t16", [128, 16], F32)
            for j in range(2):
                kb.op('pool', lambda e, j=j: e.memset(bpad[j][:, :, :], 0.0), W=[bpad[j]])
            for m in range(8):
                for hh in range(2):
                    r0 = 64 * hh
                    co = ((2 * m + hh) * 16) % 128
                    rs_ = slice(r0, r0 + 64)
                    V(lambda e, m=m, rs_=rs_: e.tensor_scalar(out=t16[rs_, :], in0=bim[rs_, m, :], scalar1=cfi[rs_, m:m + 1], scalar2=None, op0=TT), [bim, cfi], [t16])
                    kb.op('dve', lambda e, m=m, rs_=rs_, co=co: e.scalar_tensor_tensor(out=bpad[0][rs_, m, co:co + 16], in0=bre[rs_, m, :], scalar=cfr[rs_, m:m + 1], in1=t16[rs_, :], op0=TT, op1=ALU.subtract),
                          R=[bre, cfr, t16], PW=[bpad[0]])
                    V(lambda e, m=m, rs_=rs_: e.tensor_scalar(out=t16[rs_, :], in0=bre[rs_, m, :], scalar1=cfi[rs_, m:m + 1], scalar2=None, op0=TT), [bre, cfi], [t16])
                    kb.op('dve', lambda e, m=m, rs_=rs_, co=co: e.scalar_tensor_tensor(out=bpad[1][rs_, m, co:co + 16], in0=bim[rs_, m, :], scalar=cfr[rs_, m:m + 1], in1=t16[rs_, :], op0=TT, op1=ALU.add),
                          R=[bim, cfr, t16], PW=[bpad[1]])
            pss = [kb.buf(es, "s5_ps%d" % i, [128, 512], F32, psum=True) for i in range(8)]
            wB = [kb.buf(es, "s5_wB%d" % j, [128, 8, 128], BF16) for j in range(2)]
            for j in range(2):
                for m in range(8):
                    ps = pss[m % 2]
                    kb.op('pe', lambda e, j=j, m=m, ps=ps: e.transpose(ps[:, 0:128], bpad[j][:, m, :], C['ident'][:, :]), R=[bpad[j], C['ident']], W=[ps])
                    kb.op('act', lambda e, j=j, m=m, ps=ps: e.copy(out=wB[j][:, m, :], in_=ps[:, 0:128]), R=[ps], PW=[wB[j]])
            craw = [kb.buf(es, "s5_craw%d" % j, [128, 2, 64], F32) for j in range(2)]
            cT = [kb.buf(es, "s5_cT%d" % j, [64, 256], F32) for j in range(2)]
            wC = [kb.buf(es, "s5_wC%d" % j, [128, 8, 128], BF16) for j in range(2)]
            for j, nm in enumerate(("s5_c_re", "s5_c_im")):
                kb.dma(craw[j][:, :, :], self.w[nm][l].rearrange("g c n -> (g c) n").rearrange("(a p) n -> p a n", p=128), craw[j], W=[craw[j]])
                kb.op('pool', lambda e, j=j: e.memset(wC[j][:, :, :], 0.0), W=[wC[j]])
                for a_ in range(2):
                    ps = pss[2 + a_]
                    kb.op('pe', lambda e, j=j, a_=a_, ps=ps: e.transpose(ps[0:64, 0:128], craw[j][:, a_, :], C['ident'][:, :]), R=[craw[j], C['ident']], W=[ps])
                    kb.op('act', lambda e, j=j, a_=a_, ps=ps: e.copy(out=cT[j][:, a_ * 128:(a_ + 1) * 128], in_=ps[0:64, 0:128]), R=[ps], PW=[cT[j]])
                sgn = 1.0 if j == 0 else -1.0
                for m in range(8):
                    for hh in range(2):
                        g_ = 2 * m + hh
                        co = (g_ * 16) % 128
                        kb.op('dve', lambda e, j=j, m=m, hh=hh, g_=g_, co=co, sgn=sgn: e.tensor_scalar(out=wC[j][64 * hh:64 * hh + 64, m, co:co + 16], in0=cT[j][:, g_ * 16:(g_ + 1) * 16],
                                                                                                  scalar1=sgn, scalar2=None, op0=TT), R=[cT[j]], PW=[wC[j]])
            CT = kb.buf(es, "s5_CT", [128, 8, 512], F32)
            ST = kb.buf(es, "s5_ST", [128, 8, 512], F32)
            RH = kb.buf(es, "s5_RH", [128, 8, 512], F32)
            ck = kb.buf(es, "s5_ck", [128, 8], F32)
            sk = kb.buf(es, "s5_sk", [128, 8], F32)
            tk = kb.buf(es, "s5_tk", [128, 8], F32)
            tk2 = kb.buf(es, "s5_tk2", [128, 8], F32)
            tb1 = kb.buf(es, "s5_tb1", [128, 8, 256], F32)
            tb2 = kb.buf(es, "s5_tb2", [128, 8, 256], F32)
            V(lambda e: e.tensor_copy(out=ck[:, :], in_=c1[:, :]), [c1], [ck])
            V(lambda e: e.tensor_copy(out=sk[:, :], in_=s1[:, :]), [s1], [sk])
            kb.op('pool', lambda e: e.memset(CT[:, :, 0:1], 1.0), PW=[CT])
            kb.op('pool', lambda e: e.memset(ST[:, :, 0:1], 0.0), PW=[ST])
            for m in range(8):
                V(lambda e, m=m: e.tensor_copy(out=RH[:, m, :], in_=rho[:, m:m + 1].to_broadcast([128, 512])), [rho], [RH])
            w_ = 1
            for kk in range(10):
                if kk < 9:
                    cb_ = ck[:, :].unsqueeze(2).to_broadcast([128, 8, w_])
                    sb_ = sk[:, :].unsqueeze(2).to_broadcast([128, 8, w_])
                    V(lambda e, w_=w_, cb_=cb_: e.tensor_tensor(out=tb1[:, :, 0:w_], in0=CT[:, :, 0:w_], in1=cb_, op=TT), [CT, ck], [tb1])
                    V(lambda e, w_=w_, sb_=sb_: e.tensor_tensor(out=tb2[:, :, 0:w_], in0=ST[:, :, 0:w_], in1=sb_, op=TT), [ST, sk], [tb2])
                    kb.op('dve', lambda e, w_=w_: e.tensor_tensor(out=CT[:, :, w_:2 * w_], in0=tb1[:, :, 0:w_], in1=tb2[:, :, 0:w_], op=ALU.subtract), R=[tb1, tb2], PW=[CT])
                    V(lambda e, w_=w_, sb_=sb_: e.tensor_tensor(out=tb1[:, :, 0:w_], in0=CT[:, :, 0:w_], in1=sb_, op=TT), [CT, sk], [tb1])
                    V(lambda e, w_=w_, cb_=cb_: e.tensor_tensor(out=tb2[:, :, 0:w_], in0=ST[:, :, 0:w_], in1=cb_, op=TT), [ST, ck], [tb2])
                    kb.op('dve', lambda e, w_=w_: e.tensor_tensor(out=ST[:, :, w_:2 * w_], in0=tb1[:, :, 0:w_], in1=tb2[:, :, 0:w_], op=ALU.add), R=[tb1, tb2], PW=[ST])
                    w_ *= 2
                    V(lambda e: e.tensor_tensor(out=tk[:, :], in0=ck[:, :], in1=ck[:, :], op=TT), [ck], [tk])
                    V(lambda e: e.tensor_tensor(out=tk2[:, :], in0=sk[:, :], in1=sk[:, :], op=TT), [sk], [tk2])
                    V(lambda e: e.tensor_tensor(out=tk[:, :], in0=tk[:, :], in1=tk2[:, :], op=ALU.subtract), [tk, tk2], [tk])
                    V(lambda e: e.tensor_tensor(out=tk2[:, :], in0=ck[:, :], in1=sk[:, :], op=TT), [ck, sk], [tk2])
                    V(lambda e: e.tensor_scalar(out=sk[:, :], in0=tk2[:, :], scalar1=2.0, scalar2=None, op0=TT), [tk2], [sk])
                    V(lambda e: e.tensor_copy(out=ck[:, :], in_=tk[:, :]), [tk], [ck])
            nsk = kb.buf(es, "s5_nsk", [128, 8], F32)
            V(lambda e: e.tensor_scalar(out=nsk[:, :], in0=sk[:, :], scalar1=-1.0, scalar2=None, op0=TT), [sk], [nsk])
            dsk = kb.buf(es, "s5_dsk", [128, 2], F32)
            glb = kb.buf(es, "s5_glb", [128, 2], F32)
            glw = kb.buf(es, "s5_glw", [128, 2, 256], BF16)
            kb.dma(dsk[:, :], self.w["s5_d"][l].rearrange("(c p) -> p c", p=128), dsk, W=[dsk], allow_slow_non_contiguous=True)
            kb.dma(glb[:, :], self.w["s5_glu_b"][l].rearrange("(c p) -> p c", p=128), glb, W=[glb], allow_slow_non_contiguous=True)
            kb.dma(glw[:, :, :], self.wb["s5_glu_w"][l].rearrange("(c p) o -> p c o", p=128), glw, W=[glw])
            uf = [kb.buf(es, "s5_uf%d" % i, [128, 2, 512], F32) for i in range(2)]
            ub = [kb.buf(es, "s5_ub%d" % i, [128, 2, 512], BF16) for i in range(2)]
            ini = kb.buf(es, "s5_ini", [128, 8, 2], F32)
            kb.op('pool', lambda e: e.memset(ini[:, :, :], 0.0), W=[ini])
            vr = kb.buf(es, "s5_vr", [128, 512], F32)
            vi = kb.buf(es, "s5_vi", [128, 512], F32)
            ta = kb.buf(es, "s5_ta", [128, 512], F32)
            tbb = kb.buf(es, "s5_tb", [128, 512], F32)
            gr = kb.buf(es, "s5_gr", [128, 512], F32)
            gi = kb.buf(es, "s5_gi", [128, 512], F32)
            hr = [kb.buf(es, "s5_hr%d" % i, [128, 512], BF16) for i in range(2)]
            hi = [kb.buf(es, "s5_hi%d" % i, [128, 512], BF16) for i in range(2)]
            yb = kb.buf(es, "s5_y", [128, 512], F32)
            tm = kb.buf(es, "s5_tm", [128, 512], F32)
            zf = kb.buf(es, "s5_zf", [128, 2, 512], F32)
            zb = kb.buf(es, "s5_zb", [128, 2, 512], BF16)
            sg = kb.buf(es, "s5_sg", [128, 512], F32)
            ob = [kb.buf(es, "s5_ob%d" % i, [128, 512], BF16) for i in range(2)]
            tcol = kb.buf(es, "s5_tcol", [128, 2], F32)
            n = 0
            for i in range(NT):
                t0 = i * 512
                uf_ = uf[i % 2]
                ub_ = ub[i % 2]
                kb.dma(uf_[:, :, :], self.ZS[s, :, t0:t0 + 512].rearrange("(c p) t -> p c t", p=128), uf_, W=[uf_])
                kb.op('act', lambda e: e.copy(out=ub_[:, :, :], in_=uf_[:, :, :]), R=[uf_], W=[ub_])
                for cc in range(2):
                    yps = pss[4 + cc]
                    for mm in range(4):
                        m = cc * 4 + mm
                        pr = pss[(2 * n) % 4]
                        pi = pss[(2 * n + 1) % 4]
                        hr_ = hr[n % 2]
                        hi_ = hi[n % 2]
                        n += 1
                        kb.op('pe', lambda e, m=m, pr=pr, cc=cc: e.matmul(pr[:, :], lhsT=wB[0][:, m, :], rhs=ub_[:, cc, :], start=True, stop=True), R=[wB[0], ub_], W=[pr])
                        kb.op('pe', lambda e, m=m, pi=pi, cc=cc: e.matmul(pi[:, :], lhsT=wB[1][:, m, :], rhs=ub_[:, cc, :], start=True, stop=True), R=[wB[1], ub_], W=[pi])
                        V(lambda e, m=m, pr=pr: e.tensor_tensor(out=ta[:, :], in0=pr[:, :], in1=CT[:, m, :], op=TT), [pr, CT], [ta])
                        V(lambda e, m=m, pi=pi: e.tensor_tensor(out=tbb[:, :], in0=pi[:, :], in1=ST[:, m, :], op=TT), [pi, ST], [tbb])
                        kb.op('pool', lambda e: e.tensor_tensor(out=vr[:, :], in0=ta[:, :], in1=tbb[:, :], op=ALU.add), R=[ta, tbb], W=[vr])
                        V(lambda e, m=m, pi=pi: e.tensor_tensor(out=ta[:, :], in0=pi[:, :], in1=CT[:, m, :], op=TT), [pi, CT], [ta])
                        V(lambda e, m=m, pr=pr: e.tensor_tensor(out=tbb[:, :], in0=pr[:, :], in1=ST[:, m, :], op=TT), [pr, ST], [tbb])
                        kb.op('pool', lambda e: e.tensor_tensor(out=vi[:, :], in0=ta[:, :], in1=tbb[:, :], op=ALU.subtract), R=[ta, tbb], W=[vi])
                        V(lambda e, m=m: e.tensor_tensor_scan(out=gr[:, :], data0=RH[:, m, :], data1=vr[:, :], initial=ini[:, m, 0:1], op0=ALU.mult, op1=ALU.add), [RH, vr, ini], [gr])
                        V(lambda e, m=m: e.tensor_tensor_scan(out=gi[:, :], data0=RH[:, m, :], data1=vi[:, :], initial=ini[:, m, 1:2], op0=ALU.mult, op1=ALU.add), [RH, vi, ini], [gi])
                        V(lambda e, m=m: e.tensor_scalar(out=tcol[:, 0:1], in0=gr[:, 511:512], scalar1=ck[:, m:m + 1], scalar2=None, op0=TT), [gr, ck], [tcol])
                        kb.op('dve', lambda e, m=m: e.scalar_tensor_tensor(out=ini[:, m, 0:1], in0=gi[:, 511:512], scalar=nsk[:, m:m + 1], in1=tcol[:, 0:1], op0=TT, op1=ALU.add), R=[gi, nsk, tcol], PW=[ini])
                        V(lambda e, m=m: e.tensor_scalar(out=tcol[:, 1:2], in0=gi[:, 511:512], scalar1=ck[:, m:m + 1], scalar2=None, op0=TT), [gi, ck], [tcol])
                        kb.op('dve', lambda e, m=m: e.scalar_tensor_tensor(out=ini[:, m, 1:2], in0=gr[:, 511:512], scalar=sk[:, m:m + 1], in1=tcol[:, 1:2], op0=TT, op1=ALU.add), R=[gr, sk, tcol], PW=[ini])
                        kb.op('pool', lambda e, m=m: e.tensor_tensor(out=ta[:, :], in0=gr[:, :], in1=CT[:, m, :], op=TT), R=[gr, CT], W=[ta])
                        kb.op('pool', lambda e, m=m: e.tensor_tensor(out=tbb[:, :], in0=gi[:, :], in1=ST[:, m, :], op=TT), R=[gi, ST], W=[tbb])
                        V(lambda e, hr_=hr_: e.tensor_tensor(out=hr_[:, :], in0=ta[:, :], in1=tbb[:, :], op=ALU.subtract), [ta, tbb], [hr_])
                        kb.op('pool', lambda e, m=m: e.tensor_tensor(out=ta[:, :], in0=gr[:, :], in1=ST[:, m, :], op=TT), R=[gr, ST], W=[ta])
                        kb.op('pool', lambda e, m=m: e.tensor_tensor(out=tbb[:, :], in0=gi[:, :], in1=CT[:, m, :], op=TT), R=[gi, CT], W=[tbb])
                        V(lambda e, hi_=hi_: e.tensor_tensor(out=hi_[:, :], in0=ta[:, :], in1=tbb[:, :], op=ALU.add), [ta, tbb], [hi_])
                        kb.op('pe', lambda e, m=m, hr_=hr_, mm=mm, yps=yps: e.matmul(yps[:, :], lhsT=wC[0][:, m, :], rhs=hr_[:, :], start=(mm == 0), stop=False), R=[wC[0], hr_], PW=[yps])
                        kb.op('pe', lambda e, m=m, hi_=hi_, mm=mm, yps=yps: e.matmul(yps[:, :], lhsT=wC[1][:, m, :], rhs=hi_[:, :], start=False, stop=(mm == 3)), R=[wC[1], hi_], PW=[yps])
                    kb.op('dve', lambda e, cc=cc, yps=yps: e.scalar_tensor_tensor(out=yb[:, :], in0=uf_[:, cc, :], scalar=dsk[:, cc:cc + 1], in1=yps[:, :], op0=TT, op1=ALU.add), R=[uf_, dsk, yps], W=[yb])
                    gelu_tanh(kb, yb, tm, zf[:, cc, :], zf, 512)
                    kb.op('act', lambda e, cc=cc: e.copy(out=zb[:, cc, :], in_=zf[:, cc, :]), R=[zf], PW=[zb])
                for oc in range(2):
                    ps = pss[6 + oc]
                    for kc in range(2):
                        kb.op('pe', lambda e, oc=oc, kc=kc, ps=ps: e.matmul(ps[:, :], lhsT=glw[:, kc, oc * 128:(oc + 1) * 128], rhs=zb[:, kc, :], start=(kc == 0), stop=(kc == 1)), R=[glw, zb], PW=[ps])
                    kb.op('act', lambda e, oc=oc, ps=ps: e.activation(out=sg[:, :], in_=ps[:, :], func=AF.Sigmoid, bias=glb[:, oc:oc + 1], scale=1.0), R=[ps, glb], W=[sg])
                    o_ = ob[oc]
                    kb.op('dve', lambda e, oc=oc, o_=o_: e.tensor_tensor(out=o_[:, :], in0=zf[:, oc, :], in1=sg[:, :], op=TT), R=[zf, sg], W=[o_])
                    kb.dma(self.MIXT[s, 768 + oc * 128:768 + (oc + 1) * 128, t0:t0 + 512], o_[:, :], o_, R=[o_])
            kb.barrier()

    def load_ln(self, es, l, j, tag):
        kb = self.kb
        g = kb.buf(es, "lng" + tag, [128, D], F32)
        b = kb.buf(es, "lnb" + tag, [128, D], F32)
        kb.dma(g[:, :], self.w["ln_g"][l, j:j + 1, :].partition_broadcast(128), g, W=[g])
        kb.dma(b[:, :], self.w["ln_b"][l, j:j + 1, :].partition_broadcast(128), b, W=[b])
        return g, b

    def phase_outproj(self, l, src):
        kb = self.kb
        with ExitStack() as es:
            wo = kb.buf(es, "op_w", [128, 8, D], BF16)
            kb.dma(wo[:, :, :], self.wb["w_out"][l].rearrange("(c p) o -> p c o", p=128), wo, W=[wo])
            g, b = self.load_ln(es, l, 0, "op")
            scr = self.ln_scratch(es, "op")
            mx = [kb.buf(es, "op_mx%d" % i, [128, 8, 512], BF16) for i in range(2)]
            xt = [kb.buf(es, "op_x%d" % i, [128, 4, D], F32) for i in range(2)]
            ot = [kb.buf(es, "op_o%d" % i, [128, 4, D], F32) for i in range(2)]
            tt = [kb.buf(es, "op_t%d" % i, [128, D], F32) for i in range(2)]
            pss = [kb.buf(es, "op_ps%d" % i, [128, 512], F32, psum=True) for i in range(4)]
            n = 0
            for s in range(self.n_seq):
                for i in range(NT):
                    t0 = i * 512
                    mx_, x_, o_ = mx[n % 2], xt[n % 2], ot[n % 2]
                    kb.dma(mx_[:, :, :], self.MIXT[s, :, t0:t0 + 512].rearrange("(c p) t -> p c t", p=128), mx_, W=[mx_])
                    kb.dma(x_[:, :, :], src[s, t0:t0 + 512, :].rearrange("(a p) d -> p a d", p=128), x_, W=[x_])
                    for sub in range(4):
                        tt_ = tt[sub % 2]
                        for half in range(2):
                            ps = pss[(sub * 2 + half) % 4]
                            for kc in range(8):
                                kb.op('pe', lambda e, kc=kc, sub=sub, half=half, ps=ps: e.matmul(ps[:, :], lhsT=mx_[:, kc, sub * 128:(sub + 1) * 128], rhs=wo[:, kc, half * 512:(half + 1) * 512],
                                                                                         start=(kc == 0), stop=(kc == 7)), R=[mx_, wo], PW=[ps])
                            kb.op('dve', lambda e, sub=sub, half=half, ps=ps, tt_=tt_: e.scalar_tensor_tensor(out=tt_[:, half * 512:(half + 1) * 512], in0=x_[:, sub, half * 512:(half + 1) * 512], scalar=ALPHA,
                                                                                                   in1=ps[:, :], op0=ALU.mult, op1=ALU.add), R=[x_, ps], PW=[tt_])
                        eng, fn, rd = layer_norm_tm(kb, tt_, g, b, o_[:, sub, :], scr)
                        kb.op(eng, fn, R=rd, PW=[o_])
                    kb.dma(self.XR[s, t0:t0 + 512, :].rearrange("(a p) d -> p a d", p=128), o_[:, :, :], o_, R=[o_])
                    n += 1
            kb.barrier()

    def phase_ffn(self, l, moe):
        kb = self.kb
        C = self.C
        j = l // 2
        if moe:
            FF, GS, experts = D_FFE, 4, NE
            W1, W3, W2 = self.wb["moe_w1"][j], self.wb["moe_w3"][j], self.wb["moe_w2"][j]
        else:
            FF, GS, experts = D_FF, 2, 1
            W1, W3, W2 = self.wb["ffn_w1"][j:j + 1], self.wb["ffn_w3"][j:j + 1], self.wb["ffn_w2"][j:j + 1]
        NFC = FF // 128
        NG = NFC // GS
        GW = GS * 128
        with ExitStack() as es:
            g, b = self.load_ln(es, l, 1, "ff")
            scr = self.ln_scratch(es, "ff")
            xt = [kb.buf(es, "ff_x%d" % i, [128, 4, D], F32) for i in range(1 if moe else 2)]
            ot = kb.buf(es, "ff_o", [128, 4, D], F32)
            xT = kb.buf(es, "ff_xT", [128, 8, 512], BF16)
            w1 = [kb.buf(es, "ff_w1%d" % i, [128, 8, GW], BF16) for i in range(2)]
            w3 = [kb.buf(es, "ff_w3%d" % i, [128, 8, GW], BF16) for i in range(2)]
            w2 = [kb.buf(es, "ff_w2%d" % i, [128, NFC, 128], BF16) for i in range(2)]
            gT = kb.buf(es, "ff_g", [128, NFC, 512], BF16)
            sl = [kb.buf(es, "ff_s%d" % i, [128, 512], F32) for i in range(2)]
            facc = kb.buf(es, "ff_acc", [128, 8, 512], F32)
            tt = [kb.buf(es, "ff_t%d" % i, [128, D], F32) for i in range(2)]
            ps_h1 = [kb.buf(es, "ff_ph1%d" % i, [128, 512], F32, psum=True) for i in range(2)]
            ps_h3 = [kb.buf(es, "ff_ph3%d" % i, [128, 512], F32, psum=True) for i in range(2)]
            ps_o = [kb.buf(es, "ff_po%d" % i, [128, 512], F32, psum=True) for i in range(2)]
            ps_t = [kb.buf(es, "ff_pt%d" % i, [128, 512], F32, psum=True) for i in range(2)]
            if moe:
                xTf = kb.buf(es, "ff_xTf", [128, 8, 512], F32)
                rt = kb.buf(es, "ff_rt", [128, 8, 128], F32)
                kb.op('pool', lambda e: e.memset(rt[:, :, :], 0.0), W=[rt])
                kb.dma(rt[:, :, 0:NE], self.w["moe_router"][j].rearrange("(c p) e -> p c e", p=128), rt, PW=[rt])
                lg = kb.buf(es, "ff_lg", [128, 4, 8], F32)
                t8 = kb.buf(es, "ff_t8", [128, 4, 8], F32)
                wv = kb.buf(es, "ff_wv", [128, 4, 2], F32)
                cmb = kb.buf(es, "ff_cmb", [128, 4, 8], F32)
                cm2 = kb.buf(es, "ff_cm2", [128, 8], F32)
                cmB = kb.buf(es, "ff_cmB", [128, 8, 512], BF16)
                cexp = [kb.buf(es, "ff_cexp%d" % i, [128, 128], F32) for i in range(2)]
            cnt = [0]
            nw = [0]
            n2 = [0]
            n = 0
            for s in range(self.n_seq):
                for i in range(NT):
                    t0 = i * 512
                    x_ = xt[n % len(xt)]
                    n += 1
                    kb.dma(x_[:, :, :], self.XR[s, t0:t0 + 512, :].rearrange("(a p) d -> p a d", p=128), x_, W=[x_])
                    if moe:
                        transpose_in(kb, x_, 4, 8, xTf, C['ident'], ps_t, cnt, dst2=xT)
                    else:
                        transpose_in(kb, x_, 4, 8, xT, C['ident'], ps_t, cnt)
                    if moe and os.environ.get('MOE_SKIP_ROUTE'):
                        kb.op('pool', lambda e: e.memset(cmB[:, :, :], 0.5), W=[cmB])
                    elif moe:
                        for sub in range(4):
                            ps = ps_o[sub % 2]
                            for kc in range(8):
                                kb.op('pe', lambda e, kc=kc, sub=sub, ps=ps: e.matmul(ps[:, 0:128], lhsT=xTf[:, kc, sub * 128:(sub + 1) * 128], rhs=rt[:, kc, :], start=(kc == 0), stop=(kc == 7)), R=[xTf, rt], PW=[ps])
                            kb.op('dve', lambda e, sub=sub, ps=ps: e.tensor_copy(out=lg[:, sub, :], in_=ps[:, 0:8]), R=[ps], PW=[lg])
                            kb.op('dve', lambda e, sub=sub: e.max(out=t8[:, sub, :], in_=lg[:, sub, :]), R=[lg], PW=[t8])
                            kb.op('dve', lambda e, sub=sub: e.tensor_tensor(out=wv[:, sub, 0:1], in0=t8[:, sub, 0:1], in1=t8[:, sub, 1:2], op=ALU.subtract), R=[t8], PW=[wv])
                            kb.op('act', lambda e, sub=sub: e.activation(out=wv[:, sub, 0:1], in_=wv[:, sub, 0:1], func=AF.Sigmoid), R=[wv], PW=[wv])
                            kb.op('dve', lambda e, sub=sub: e.tensor_scalar(out=wv[:, sub, 1:2], in0=wv[:, sub, 0:1], scalar1=-1.0, scalar2=1.0, op0=ALU.mult, op1=ALU.add), R=[wv], PW=[wv])
                            kb.op('dve', lambda e, sub=sub: e.tensor_scalar(out=cmb[:, sub, :], in0=lg[:, sub, :], scalar1=t8[:, sub, 0:1], scalar2=wv[:, sub, 0:1], op0=ALU.is_equal, op1=ALU.mult), R=[lg, t8, wv], PW=[cmb])
                            kb.op('dve', lambda e, sub=sub: e.tensor_scalar(out=cm2[:, :], in0=lg[:, sub, :], scalar1=t8[:, sub, 1:2], scalar2=wv[:, sub, 1:2], op0=ALU.is_equal, op1=ALU.mult), R=[lg, t8, wv], W=[cm2])
                            kb.op('dve', lambda e, sub=sub: e.tensor_tensor(out=cmb[:, sub, :], in0=cmb[:, sub, :], in1=cm2[:, :], op=ALU.add), R=[cmb, cm2], PW=[cmb])
                        for ex in range(NE):
                            ps = ps_o[ex % 2]
                            for sub in range(4):
                                ce = cexp[(ex * 4 + sub) % 2]
                                kb.op('dve', lambda e, ex=ex, sub=sub, ce=ce: e.tensor_copy(out=ce[:, :], in_=cmb[:, sub, ex:ex + 1].to_broadcast([128, 128])), R=[cmb], W=[ce])
                                kb.op('pe', lambda e, sub=sub, ps=ps, ce=ce: e.matmul(ps[:, sub * 128:(sub + 1) * 128], lhsT=ce[:, :], rhs=C['ident'][:, :], start=True, stop=True), R=[ce, C['ident']], PW=[ps])
                            kb.op('act', lambda e, ex=ex, ps=ps: e.copy(out=cmB[:, ex, :], in_=ps[:, :]), R=[ps], PW=[cmB])
                    for ex in range(int(os.environ.get('MOE_NE', experts)) if moe else experts):
                        for gi_ in range(0 if (moe and os.environ.get('MOE_MODE') == 's2') else NG):
                            w1_, w3_ = w1[nw[0] % 2], w3[nw[0] % 2]
                            nw[0] += 1
                            kb.dma(w1_[:, :, :], W1[ex, :, gi_ * GW:(gi_ + 1) * GW].rearrange("(c p) f -> p c f", p=128), w1_, W=[w1_])
                            kb.dma(w3_[:, :, :], W3[ex, :, gi_ * GW:(gi_ + 1) * GW].rearrange("(c p) f -> p c f", p=128), w3_, W=[w3_])
                            for fi in range(GS):
                                fc = gi_ * GS + fi
                                p1, p3 = ps_h1[fc % 2], ps_h3[fc % 2]
                                for kc in range(8):
                                    kb.op('pe', lambda e, kc=kc, fi=fi, p1=p1, w1_=w1_: e.matmul(p1[:, :], lhsT=w1_[:, kc, fi * 128:(fi + 1) * 128], rhs=xT[:, kc, :], start=(kc == 0), stop=(kc == 7)), R=[w1_, xT], PW=[p1])
                                for kc in range(8):
                                    kb.op('pe', lambda e, kc=kc, fi=fi, p3=p3, w3_=w3_: e.matmul(p3[:, :], lhsT=w3_[:, kc, fi * 128:(fi + 1) * 128], rhs=xT[:, kc, :], start=(kc == 0), stop=(kc == 7)), R=[w3_, xT], PW=[p3])
                                s_ = sl[fc % 2]
                                kb.op('act', lambda e, p1=p1, s_=s_: e.activation(out=s_[:, :], in_=p1[:, :], func=AF.Silu), R=[p1], W=[s_])
                                if moe:
                                    kb.op('dve', lambda e, p3=p3, s_=s_: e.tensor_tensor(out=s_[:, :], in0=s_[:, :], in1=p3[:, :], op=ALU.mult), R=[s_, p3], W=[s_])
                                    kb.op('dve', lambda e, fc=fc, s_=s_, ex=ex: e.tensor_tensor(out=gT[:, fc, :], in0=s_[:, :], in1=cmB[:, ex, :], op=ALU.mult), R=[s_, cmB], PW=[gT])
                                else:
                                    kb.op('dve', lambda e, fc=fc, p3=p3, s_=s_: e.tensor_tensor(out=gT[:, fc, :], in0=s_[:, :], in1=p3[:, :], op=ALU.mult), R=[s_, p3], PW=[gT])
                        for dc in range(0 if (moe and os.environ.get('MOE_MODE') == 's1') else 8):
                            w2_ = w2[n2[0] % 2]
                            n2[0] += 1
                            kb.dma(w2_[:, :, :], W2[ex, :, dc * 128:(dc + 1) * 128].rearrange("(c p) o -> p c o", p=128), w2_, W=[w2_])
                            po = ps_o[dc % 2]
                            for fc in range(NFC):
                                kb.op('pe', lambda e, fc=fc, po=po, w2_=w2_: e.matmul(po[:, :], lhsT=w2_[:, fc, :], rhs=gT[:, fc, :], start=(fc == 0), stop=(fc == NFC - 1)), R=[w2_, gT], PW=[po])
                            if ex == 0:
                                kb.op('act', lambda e, dc=dc, po=po: e.copy(out=facc[:, dc, :], in_=po[:, :]), R=[po], PW=[facc])
                            else:
                                kb.op('dve', lambda e, dc=dc, po=po: e.tensor_tensor(out=facc[:, dc, :], in0=facc[:, dc, :], in1=po[:, :], op=ALU.add), R=[facc, po], PW=[facc])
                    for sub in range(4):
                        tt_ = tt[sub % 2]
                        for half in range(2):
                            pst = ps_t[cnt[0] % 2]
                            cnt[0] += 1
                            for q in range(4):
                                dc = half * 4 + q
                                kb.op('pe', lambda e, dc=dc, q=q, sub=sub, pst=pst: e.transpose(pst[:, q * 128:(q + 1) * 128], facc[:, dc, sub * 128:(sub + 1) * 128], C['ident'][:, :]), R=[facc, C['ident']], PW=[pst])
                            kb.op('dve', lambda e, sub=sub, half=half, pst=pst, tt_=tt_: e.scalar_tensor_tensor(out=tt_[:, half * 512:(half + 1) * 512], in0=x_[:, sub, half * 512:(half + 1) * 512], scalar=ALPHA,
                                                                                                    in1=pst[:, :], op0=ALU.mult, op1=ALU.add), R=[x_, pst], PW=[tt_])
                        eng, fn, rd = layer_norm_tm(kb, tt_, g, b, ot[:, sub, :], scr)
                        kb.op(eng, fn, R=rd, PW=[ot])
                    kb.dma(self.XR[s, t0:t0 + 512, :].rearrange("(a p) d -> p a d", p=128), ot[:, :, :], ot, R=[ot])
            kb.barrier()

    def phase_ple(self, l, dst):
        kb = self.kb
        C = self.C
        with ExitStack() as es:
            wg = kb.buf(es, "pl_wg", [128, 8, D], BF16)
            wp = kb.buf(es, "pl_wp", [128, 2, D], BF16)
            bg = kb.buf(es, "pl_bg", [128, D], F32)
            kb.dma(wg[:, :, :], self.wb["ple_gate_w"][l].rearrange("(c p) o -> p c o", p=128), wg, W=[wg])
            kb.dma(wp[:, :, :], self.wb["ple_proj"][l].rearrange("(c p) o -> p c o", p=128), wp, W=[wp])
            kb.dma(bg[:, :], self.w["ple_gate_b"][l:l + 1, :].partition_broadcast(128), bg, W=[bg])
            g, b = self.load_ln(es, l, 2, "pl")
            scr = self.ln_scratch(es, "pl")
            xt = [kb.buf(es, "pl_x%d" % i, [128, 4, D], F32) for i in range(2)]
            pt = [kb.buf(es, "pl_p%d" % i, [128, 4, 256], F32) for i in range(2)]
            ot = [kb.buf(es, "pl_o%d" % i, [128, 4, D], F32) for i in range(2)]
            xT = kb.buf(es, "pl_xT", [128, 8, 512], BF16)
            pT = kb.buf(es, "pl_pT", [128, 2, 512], BF16)
            tt = [kb.buf(es, "pl_t%d" % i, [128, D], F32) for i in range(2)]
            uu = [kb.buf(es, "pl_u%d" % i, [128, 512], F32) for i in range(2)]
            ps_t = [kb.buf(es, "pl_pt%d" % i, [128, 512], F32, psum=True) for i in range(2)]
            ps_a = [kb.buf(es, "pl_pa%d" % i, [128, 512], F32, psum=True) for i in range(2)]
            ps_b = [kb.buf(es, "pl_pb%d" % i, [128, 512], F32, psum=True) for i in range(2)]
            cnt = [0]
            n = 0
            for s in range(self.n_seq):
                for i in range(NT):
                    t0 = i * 512
                    x_, p_, o_ = xt[n % 2], pt[n % 2], ot[n % 2]
                    n += 1
                    kb.dma(x_[:, :, :], self.XR[s, t0:t0 + 512, :].rearrange("(a p) d -> p a d", p=128), x_, W=[x_])
                    kb.dma(p_[:, :, :], self.p[l, s, t0:t0 + 512, :].rearrange("(a p) d -> p a d", p=128), p_, W=[p_])
                    transpose_in(kb, x_, 4, 8, xT, C['ident'], ps_t, cnt)
                    transpose_in(kb, p_, 4, 2, pT, C['ident'], ps_t, cnt)
                    for sub in range(4):
                        tt_ = tt[sub % 2]
                        for half in range(2):
                            pa, pb = ps_a[half], ps_b[half]
                            u_ = uu[half]
                            hs = slice(half * 512, (half + 1) * 512)
                            for kc in range(8):
                                kb.op('pe', lambda e, kc=kc, sub=sub, pa=pa, hs=hs: e.matmul(pa[:, :], lhsT=xT[:, kc, sub * 128:(sub + 1) * 128], rhs=wg[:, kc, hs], start=(kc == 0), stop=(kc == 7)), R=[xT, wg], PW=[pa])
                            for kc in range(2):
                                kb.op('pe', lambda e, kc=kc, sub=sub, pb=pb, hs=hs: e.matmul(pb[:, :], lhsT=pT[:, kc, sub * 128:(sub + 1) * 128], rhs=wp[:, kc, hs], start=(kc == 0), stop=(kc == 1)), R=[pT, wp], PW=[pb])
                            kb.op('dve', lambda e, pa=pa, u_=u_, hs=hs: e.tensor_tensor(out=u_[:, :], in0=pa[:, :], in1=bg[:, hs], op=ALU.add), R=[pa, bg], W=[u_])
                            kb.op('act', lambda e, u_=u_: e.activation(out=u_[:, :], in_=u_[:, :], func=AF.Sigmoid), R=[u_], W=[u_])
                            kb.op('dve', lambda e, pb=pb, u_=u_: e.tensor_tensor(out=u_[:, :], in0=u_[:, :], in1=pb[:, :], op=ALU.mult), R=[u_, pb], W=[u_])
                            kb.op('dve', lambda e, sub=sub, u_=u_, hs=hs, tt_=tt_: e.scalar_tensor_tensor(out=tt_[:, hs], in0=x_[:, sub, hs], scalar=ALPHA, in1=u_[:, :], op0=ALU.mult, op1=ALU.add), R=[x_, u_], PW=[tt_])
                        eng, fn, rd = layer_norm_tm(kb, tt_, g, b, o_[:, sub, :], scr)
                        kb.op(eng, fn, R=rd, PW=[o_])
                    kb.dma(dst[s, t0:t0 + 512, :].rearrange("(a p) d -> p a d", p=128), o_[:, :, :], o_, R=[o_])
            kb.barrier()


def win_perm():
    q = lambda h, half: [h * 64 + half * 32 + d for d in range(32)]
    kv = lambda br, h, half: [512 + (br * 2 + h) * 64 + half * 32 + d for d in range(32)]
    vfull = lambda br, h: [512 + (br * 2 + h) * 64 + d for d in range(64)]
    cols = []
    for hs in (range(0, 4), range(4, 8)):
        for half in range(2):
            for h in hs:
                cols += q(h, half)
    for half in range(2):
        cols += kv(0, 0, half) + kv(0, 1, half) + kv(2, 0, half) + kv(2, 1, half)
    for half in range(2):
        cols += kv(4, 0, half) + kv(4, 1, half)
    cols += vfull(1, 0) + vfull(1, 1)
    cols += list(range(512 + 768 + 24, 512 + 768 + 24 + 512))
    cols += list(range(512 + 768 + 24 + 512, 2072))
    cols += vfull(3, 0) + vfull(3, 1) + vfull(5, 0) + vfull(5, 1)
    cols += list(range(512 + 768, 512 + 768 + 24))
    assert len(cols) == INW and len(set(cols)) == INW
    return np.array(cols)


def make_consts():
    ident = np.eye(128, dtype=np.float32)
    inv_freq = (np.float32(10000.0) ** (-np.arange(0, 64, 2, dtype=np.float32) / np.float32(64))).astype(np.float32)
    invf = np.tile(inv_freq, 4).reshape(128, 1).astype(np.float32)
    j = np.arange(L)
    emat = (j[None, :] // 64 == np.arange(64)[:, None]).astype(np.float32).astype(ml_dtypes.bfloat16)
    n_cmp = 255
    c_start = np.arange(n_cmp)[:, None] * 16
    s_start = np.arange(64)[None, :] * 64
    ovl = np.zeros((256, 64), np.float32)
    ovl[:n_cmp] = ((c_start < s_start + 64) & (c_start + 32 > s_start)).astype(np.float32)
    return {"c_ident": ident, "c_invf": invf, "c_emat": emat, "c_ovl": ovl.astype(ml_dtypes.bfloat16)}


_PROG = {}


def kernel(**inputs):
    n_cores = 8
    if 'full' not in _PROG:
        _PROG['full'] = Prog()
    prog = _PROG['full']
    consts = make_consts()
    perm = win_perm()
    shared = {k: np.ascontiguousarray(np.asarray(v)) for k, v in inputs.items() if k not in ("x", "p", "positions")}
    shared["w_in"] = np.ascontiguousarray(shared["w_in"][:, :, perm])
    shared.update(consts)
    x = np.asarray(inputs["x"])
    p = np.asarray(inputs["p"])
    pos = np.asarray(inputs["positions"]).astype(np.int32)
    in_maps = []
    for c in range(n_cores):
        m = dict(shared)
        m["x"] = np.ascontiguousarray(x[2 * c:2 * c + 2])
        m["p"] = np.ascontiguousarray(p[:, 2 * c:2 * c + 2])
        m["positions"] = np.ascontiguousarray(pos[2 * c:2 * c + 2])
        in_maps.append(m)
    res = run_bass_kernel_spmd(prog.nc, in_maps, core_ids=list(range(n_cores)))
    out = np.concatenate([np.asarray(r["y"]) for r in res.results], axis=0)
    return out.astype(np.float32)
```

```python
import math
import os
from contextlib import ExitStack
import numpy as np
import ml_dtypes
import concourse.bass as bass
import concourse.mybir as mybir
from concourse.bass_utils import run_bass_kernel_spmd

F32 = mybir.dt.float32
BF16 = mybir.dt.bfloat16
I32 = mybir.dt.int32
AF = mybir.ActivationFunctionType
ALU = mybir.AluOpType

D = 1024
L = 4096
DEPTH = 4
NT = L // 512
ALPHA = (2 * DEPTH) ** 0.25
LN_EPS = 1e-5
INW = 2072
D_FF = 2816
D_FFE = 3584
NE = 8
BIG = 30000.0
TWO_PI = 2.0 * math.pi
NDSEM = 80


class Buf:
    def __init__(self, t):
        self.t = t
        self.w = {}
        self.wf = {}
        self.r = {}
        self.prev = {}
        self.dsem = None

    def __getitem__(self, idx):
        return self.t[idx]


def _merge(d, src):
    for s, v in src.items():
        if d.get(s, 0) < v:
            d[s] = v


class KB:
    def __init__(self, nc, es):
        self.nc = nc
        self.E = {'pe': nc.tensor, 'act': nc.scalar, 'dve': nc.vector, 'pool': nc.gpsimd, 'sp': nc.sync}
        self.sem = {}
        self.tot = {}
        for e in self.E:
            self._mksem('E_' + e, es)
        self.dfree = []
        for i in range(NDSEM):
            self._mksem('D%d' % i, es)
            self.dfree.append('D%d' % i)
        self.waited = {e: {} for e in self.E}
        ss = os.environ.get('SYNC_SAME', '111')
        self.sync_same = {'pe': False, 'act': ss[0] == '1', 'dve': ss[1] == '1', 'pool': ss[2] == '1', 'sp': False}
        self.phase_bufs = []
        self.rr = 0
        self.log = None

    def _mksem(self, name, es):
        self.sem[name] = es.enter_context(self.nc.semaphore(name))
        self.tot[name] = 0

    def buf(self, es, name, shape, dt, psum=False):
        self.nbuf = getattr(self, 'nbuf', 0) + 1
        name = "%s_%d" % (name, self.nbuf)
        if psum:
            t = es.enter_context(self.nc.psum_tensor(name, shape, dt))
        else:
            t = es.enter_context(self.nc.sbuf_tensor(name, shape, dt))
        b = Buf(t)
        self.phase_bufs.append(b)
        return b

    def _wait(self, eng, deps):
        own = 'E_' + eng
        for s, v in deps.items():
            if s == own and not self.sync_same[eng]:
                continue
            if self.waited[eng].get(s, 0) >= v:
                continue
            self.E[eng].wait_ge(self.sem[s], v)
            self.waited[eng][s] = v
            if self.log is not None:
                self.log.append((eng, 'w', s, v))

    def _deps(self, R, W, PW):
        deps = {}
        for b in R:
            _merge(deps, b.w)
        for b in W:
            _merge(deps, b.w)
            _merge(deps, b.r)
            _merge(deps, b.prev)
        for b in PW:
            if b.r:
                p = {}
                _merge(p, b.r)
                _merge(p, b.w)
                b.prev = p
                b.w = {}
                b.wf = {}
                b.r = {}
            _merge(deps, b.prev)
            _merge(deps, b.wf)
        return deps

    def _post(self, tok, R, W, PW):
        for b in R:
            _merge(b.r, tok)
        for b in W:
            b.w = dict(tok)
            b.wf = dict(tok)
            b.r = {}
            b.prev = {}
        for b in PW:
            _merge(b.w, tok)

    def op(self, eng, fn, R=(), W=(), PW=()):
        deps = self._deps(R, W, PW)
        self._wait(eng, deps)
        ins = fn(self.E[eng])
        s = 'E_' + eng
        self.tot[s] += 1
        ins.then_inc(self.sem[s], 1)
        if self.log is not None:
            self.log.append((eng, 'i', s, 1))
        self._post({s: self.tot[s]}, R, W, PW)

    def dma(self, out, in_, sb, R=(), W=(), PW=(), q='sp', **kw):
        deps = self._deps(R, W, PW)
        self._wait(q, deps)
        if sb.dsem is None:
            sb.dsem = self.dfree.pop()
        ins = self.E[q].dma_start(out=out, in_=in_, **kw)
        s = sb.dsem
        self.tot[s] += 16
        ins.then_inc(self.sem[s], 16)
        if self.log is not None:
            self.log.append((q, 'i', s, 16))
        self._post({s: self.tot[s]}, R, W, PW)

    def barrier(self):
        if getattr(self, 'pre_barrier', None) is not None and not os.environ.get('NO_PREBAR'):
            self.pre_barrier()
        allt = dict(self.tot)
        for e in self.E:
            self._wait(e, allt)
        for b in self.phase_bufs:
            if b.dsem is not None:
                self.dfree.append(b.dsem)
                b.dsem = None
        self.phase_bufs = []

    def ew(self):
        self.rr += 1
        return ('dve', 'pool')[self.rr % 2]


def layer_norm_tm(kb, tt, g_b, b_b, out_ap, scr):
    st, mv, rs = scr['st'], scr['mv'], scr['rs']
    kb.op('dve', lambda e: e.bn_stats(out=st[:, 0:6], in_=tt[:, 0:512]), R=[tt], PW=[st])
    kb.op('dve', lambda e: e.bn_stats(out=st[:, 6:12], in_=tt[:, 512:1024]), R=[tt], PW=[st])
    kb.op('dve', lambda e: e.bn_aggr(out=mv[:, 0:2], in_=st[:, 0:12]), R=[st], W=[mv])
    kb.op('act', lambda e: e.activation(out=rs[:, 0:1], in_=mv[:, 1:2], func=AF.Sqrt, bias=scr['eps'][:, 0:1], scale=1.0), R=[mv, scr['eps']], W=[rs])
    kb.op('dve', lambda e: e.reciprocal(out=rs[:, 1:2], in_=rs[:, 0:1]), R=[rs], PW=[rs])
    kb.op('dve', lambda e: e.tensor_scalar(out=tt[:, :], in0=tt[:, :], scalar1=mv[:, 0:1], scalar2=rs[:, 1:2], op0=ALU.subtract, op1=ALU.mult), R=[tt, mv, rs], W=[tt])
    kb.op('pool', lambda e: e.tensor_tensor(out=tt[:, :], in0=tt[:, :], in1=g_b[:, :], op=ALU.mult), R=[tt, g_b], W=[tt])
    return ('pool', lambda e: e.tensor_tensor(out=out_ap, in0=tt[:, :], in1=b_b[:, :], op=ALU.add), [tt, b_b])


def gelu_tanh(kb, u, tmp, out_ap, out_buf, n):
    kb.op('dve', lambda e: e.tensor_tensor(out=tmp[:, 0:n], in0=u[:, 0:n], in1=u[:, 0:n], op=ALU.mult), R=[u], W=[tmp])
    kb.op('dve', lambda e: e.tensor_scalar(out=tmp[:, 0:n], in0=tmp[:, 0:n], scalar1=0.044715, scalar2=1.0, op0=ALU.mult, op1=ALU.add), R=[tmp], W=[tmp])
    kb.op('dve', lambda e: e.tensor_tensor(out=tmp[:, 0:n], in0=tmp[:, 0:n], in1=u[:, 0:n], op=ALU.mult), R=[tmp, u], W=[tmp])
    kb.op('act', lambda e: e.activation(out=tmp[:, 0:n], in_=tmp[:, 0:n], func=AF.Sigmoid, scale=1.5957691216057308), R=[tmp], W=[tmp])
    kb.op('dve', lambda e: e.tensor_tensor(out=out_ap, in0=tmp[:, 0:n], in1=u[:, 0:n], op=ALU.mult), R=[tmp, u], PW=[out_buf])


def transpose_in(kb, src, nsub, nfc, dst, ident, pss, cnt, dst2=None):
    for fc in range(nfc):
        ps = pss[cnt[0] % len(pss)]
        cnt[0] += 1
        for sub in range(nsub):
            kb.op('pe', lambda e, fc=fc, sub=sub, ps=ps: e.transpose(ps[:, sub * 128:(sub + 1) * 128], src[:, sub, fc * 128:(fc + 1) * 128], ident[:, :]),
                  R=[src, ident], PW=[ps])
        eng = ('act', 'dve')[fc % 2]
        if eng == 'act':
            kb.op('act', lambda e, fc=fc, ps=ps: e.copy(out=dst[:, fc, 0:nsub * 128], in_=ps[:, 0:nsub * 128]), R=[ps], PW=[dst])
        else:
            kb.op('dve', lambda e, fc=fc, ps=ps: e.tensor_copy(out=dst[:, fc, 0:nsub * 128], in_=ps[:, 0:nsub * 128]), R=[ps], PW=[dst])
        if dst2 is not None:
            kb.op('pool', lambda e, fc=fc: e.tensor_copy(out=dst2[:, fc, 0:nsub * 128], in_=dst[:, fc, 0:nsub * 128]), R=[dst], PW=[dst2])


class Prog:
    def __init__(self, n_layers=DEPTH, n_seq=2, dbg=(), only=None):
        self.only = only
        self.n_layers = n_layers
        self.n_seq = n_seq
        self.dbg = set(dbg)
        nc = bass.Bass("TRN2", target_bir_lowering=False)
        self.nc = nc
        S = n_seq

        def din(name, shape, dt=F32):
            return nc.dram_tensor(name, list(shape), dt, kind="ExternalInput").ap()

        def dscr(name, shape, dt):
            kind = "ExternalOutput" if name in self.dbg else "Internal"
            return nc.dram_tensor(name, list(shape), dt, kind=kind).ap()

        self.x = din("x", [S, L, D])
        self.p = din("p", [DEPTH, S, L, 256])
        self.pos = din("positions", [S, L], I32)
        self.w = {}
        for name, shape in [("w_in", [DEPTH, D, INW]), ("w_out", [DEPTH, D, D]), ("cmp_pe", [DEPTH, 2, 32, 64]),
                            ("cmp_w1", [DEPTH, 2, 2048, 256]), ("cmp_b1", [DEPTH, 2, 256]), ("cmp_w2", [DEPTH, 2, 256, 64]),
                            ("cmp_b2", [DEPTH, 2, 64]), ("conv_w", [DEPTH, 31, 256]), ("conv_b", [DEPTH, 256]),
                            ("conv_ln_g", [DEPTH, 256]), ("conv_ln_b", [DEPTH, 256]), ("s5_a_re", [DEPTH, 16, 64]),
                            ("s5_a_im", [DEPTH, 16, 64]), ("s5_log_dt", [DEPTH, 16]), ("s5_b_re", [DEPTH, 16, 64, 16]),
                            ("s5_b_im", [DEPTH, 16, 64, 16]), ("s5_c_re", [DEPTH, 16, 16, 64]), ("s5_c_im", [DEPTH, 16, 16, 64]),
                            ("s5_d", [DEPTH, 256]), ("s5_glu_w", [DEPTH, 256, 256]), ("s5_glu_b", [DEPTH, 256]),
                            ("ffn_w1", [2, D, D_FF]), ("ffn_w3", [2, D, D_FF]), ("ffn_w2", [2, D_FF, D]),
                            ("moe_router", [2, D, NE]), ("moe_w1", [2, NE, D, D_FFE]), ("moe_w3", [2, NE, D, D_FFE]),
                            ("moe_w2", [2, NE, D_FFE, D]), ("ple_gate_w", [DEPTH, D, D]), ("ple_gate_b", [DEPTH, D]),
                            ("ple_proj", [DEPTH, 256, D]), ("ln_g", [DEPTH, 3, D]), ("ln_b", [DEPTH, 3, D])]:
            self.w[name] = din(name, shape)
        self.c_ident = din("c_ident", [128, 128])
        self.c_invf = din("c_invf", [128, 1])
        self.c_emat = din("c_emat", [64, L], BF16)
        self.c_ovl = din("c_ovl", [256, 64], BF16)
        self.y = nc.dram_tensor("y", [S, L, D], F32, kind="ExternalOutput").ap()
        self.wb = {}
        for name in ["w_in", "w_out", "cmp_w1", "cmp_w2", "s5_glu_w", "ffn_w1", "ffn_w3", "ffn_w2", "moe_w1", "moe_w3", "moe_w2",
                     "ple_gate_w", "ple_proj"]:
            shp = list(self.w[name].shape)
            if name.startswith("ffn") or name.startswith("moe"):
                nl = (n_layers + 1) // 2 if name.startswith("ffn") else n_layers // 2
                shp[0] = max(nl, 1)
            else:
                shp[0] = n_layers
            self.wb[name] = dscr("b_" + name, shp, BF16)
        self.XR = dscr("XR", [S, L, D], F32)
        self.ROPE = dscr("ROPE", [S, 2, 128, L], F32)
        self.QT = dscr("QT", [S, 8, 64, L], BF16)
        self.KCT = dscr("KCT", [S, 2, 64, L], BF16)
        self.VCT = dscr("VCT", [S, 2, 64, L], BF16)
        self.KST = dscr("KST", [S, 2, 64, L], BF16)
        self.KWT = dscr("KWT", [S, 2, 64, L], BF16)
        self.VSW = dscr("VSW", [S, L, 256], BF16)
        self.G = dscr("G", [S, L, 24], F32)
        self.ZC = dscr("ZC", [S, 512, L], F32)
        self.ZS = dscr("ZS", [S, 256, L], F32)
        self.KCC = dscr("KCC", [S, 2, 64, 256], BF16)
        self.VCA = dscr("VCA", [S, 2, 256, 129], BF16)
        self.MIXT = dscr("MIXT", [S, D, L], BF16)

        with ExitStack() as es:
            self.kb = KB(nc, es)
            if 'LOG' in self.dbg:
                self.kb.log = []
            self.build()

    def consts(self, es):
        kb = self.kb
        c = {}
        c['ident'] = kb.buf(es, "ident", [128, 128], F32)
        kb.dma(c['ident'][:, :], self.c_ident[:, :], c['ident'], W=[c['ident']])
        c['eps'] = kb.buf(es, "epsb", [128, 1], F32)
        kb.op('pool', lambda e: e.memset(c['eps'][:, :], LN_EPS), W=[c['eps']])
        c['dum'] = kb.buf(es, "dumb", [128, 2], F32)
        kb.op('pool', lambda e: e.memset(c['dum'][:, :], 0.0), W=[c['dum']])
        return c

    def act_reset(self):
        d = self.C['dum']
        self.kb.op('act', lambda e: e.activation(out=d[:, 1:2], in_=d[:, 0:1], func=AF.Sigmoid), R=[d], PW=[d])

    def ln_scratch(self, es, tag):
        kb = self.kb
        return {'st': kb.buf(es, "lnst" + tag, [128, 12], F32), 'mv': kb.buf(es, "lnmv" + tag, [128, 2], F32),
                'rs': kb.buf(es, "lnrs" + tag, [128, 2], F32), 'eps': self.C['eps']}

    def build(self):
        kb = self.kb
        with ExitStack() as ces:
            self.C = self.consts(ces)
            kb.pre_barrier = self.act_reset
            on = lambda nm: (self.only is None) or (nm in self.only)
            if on('cast'):
                self.phase_cast()
            if on('rope'):
                self.phase_rope()
            for l in range(self.n_layers):
                src = self.x if l == 0 else self.XR
                for s in range(self.n_seq):
                    if on('inproj'):
                        self.phase_inproj(l, s, src)
                    if on('compress'):
                        self.phase_compress(l, s)
                    if on('attn'):
                        self.phase_attn(l, s)
                    if on('conv'):
                        self.phase_conv(l, s)
                    if on('s5'):
                        self.phase_s5(l, s)
                if on('outproj'):
                    self.phase_outproj(l, src)
                if on('ffn'):
                    self.phase_ffn(l, moe=(l % 2 == 1))
                if on('ple'):
                    self.phase_ple(l, self.y if l == self.n_layers - 1 else self.XR)
            kb.barrier()

    def phase_cast(self):
        kb = self.kb
        CH = 3584
        with ExitStack() as es:
            fb = [kb.buf(es, "cf%d" % i, [128, CH], F32) for i in range(3)]
            bb = [kb.buf(es, "cb%d" % i, [128, CH], BF16) for i in range(3)]
            n = 0
            for name, dst in self.wb.items():
                src = self.w[name]
                nl = dst.shape[0]
                s2 = src[0:nl].flatten_outer_dims()
                d2 = dst.flatten_outer_dims()
                R, Cc = s2.shape
                assert R % 128 == 0
                rpp = R // 128
                s3 = s2.rearrange("(p r) c -> p (r c)", p=128)
                d3 = d2.rearrange("(p r) c -> p (r c)", p=128)
                tot = rpp * Cc
                for c0 in range(0, tot, CH):
                    cw = min(CH, tot - c0)
                    f = fb[n % 3]
                    b = bb[n % 3]
                    kb.dma(f[:, 0:cw], s3[:, c0:c0 + cw], f, W=[f])
                    eng = ('act', 'dve', 'pool')[n % 3]
                    if eng == 'act':
                        kb.op('act', lambda e, f=f, b=b, cw=cw: e.copy(out=b[:, 0:cw], in_=f[:, 0:cw]), R=[f], W=[b])
                    else:
                        kb.op(eng, lambda e, f=f, b=b, cw=cw: e.tensor_copy(out=b[:, 0:cw], in_=f[:, 0:cw]), R=[f], W=[b])
                    kb.dma(d3[:, c0:c0 + cw], b[:, 0:cw], b, R=[b])
                    n += 1
            kb.barrier()

    def phase_rope(self):
        kb = self.kb
        with ExitStack() as es:
            pi_ = kb.buf(es, "rp_i", [128, L], I32)
            ang = kb.buf(es, "rp_a", [128, L], F32)
            kf = kb.buf(es, "rp_k", [128, L], F32)
            ki = kb.buf(es, "rp_ki", [128, L], I32)
            r = kb.buf(es, "rp_r", [128, L], F32)
            m = kb.buf(es, "rp_m", [128, L], F32)
            o = kb.buf(es, "rp_o", [128, L], F32)
            invf = kb.buf(es, "rp_f", [128, 1], F32)
            kb.dma(invf[:, :], self.c_invf[:, :], invf, W=[invf])
            HI = 6.28125
            LO = TWO_PI - HI
            for s in range(self.n_seq):
                kb.dma(pi_[:, :], self.pos[s:s + 1, :].partition_broadcast(128), pi_, W=[pi_])
                kb.op('dve', lambda e: e.tensor_copy(out=ang[:, :], in_=pi_[:, :]), R=[pi_], W=[ang])
                kb.op('dve', lambda e: e.tensor_scalar(out=ang[:, :], in0=ang[:, :], scalar1=invf[:, 0:1], scalar2=None, op0=ALU.mult), R=[ang, invf], W=[ang])
                kb.op('dve', lambda e: e.tensor_scalar(out=kf[:, :], in0=ang[:, :], scalar1=1.0 / TWO_PI, scalar2=None, op0=ALU.mult), R=[ang], W=[kf])
                kb.op('dve', lambda e: e.tensor_copy(out=ki[:, :], in_=kf[:, :]), R=[kf], W=[ki])
                kb.op('dve', lambda e: e.tensor_copy(out=kf[:, :], in_=ki[:, :]), R=[ki], W=[kf])
                kb.op('dve', lambda e: e.scalar_tensor_tensor(out=r[:, :], in0=kf[:, :], scalar=-HI, in1=ang[:, :], op0=ALU.mult, op1=ALU.add), R=[kf, ang], W=[r])
                kb.op('dve', lambda e: e.scalar_tensor_tensor(out=r[:, :], in0=kf[:, :], scalar=-LO, in1=r[:, :], op0=ALU.mult, op1=ALU.add), R=[kf, r], W=[r])
                for which, shift in ((1, 0.0), (0, math.pi / 2)):
                    kb.op('dve', lambda e, shift=shift: e.tensor_scalar(out=o[:, :], in0=r[:, :], scalar1=shift, scalar2=None, op0=ALU.add), R=[r], W=[o])
                    for _ in range(2):
                        kb.op('dve', lambda e: e.tensor_scalar(out=m[:, :], in0=o[:, :], scalar1=math.pi, scalar2=-TWO_PI, op0=ALU.is_gt, op1=ALU.mult), R=[o], W=[m])
                        kb.op('dve', lambda e: e.tensor_tensor(out=o[:, :], in0=o[:, :], in1=m[:, :], op=ALU.add), R=[o, m], W=[o])
                        kb.op('dve', lambda e: e.tensor_scalar(out=m[:, :], in0=o[:, :], scalar1=-math.pi, scalar2=TWO_PI, op0=ALU.is_lt, op1=ALU.mult), R=[o], W=[m])
                        kb.op('dve', lambda e: e.tensor_tensor(out=o[:, :], in0=o[:, :], in1=m[:, :], op=ALU.add), R=[o, m], W=[o])
                    kb.op('dve', lambda e: e.tensor_scalar(out=o[:, :], in0=o[:, :], scalar1=math.pi, scalar2=-math.pi, op0=ALU.min, op1=ALU.max), R=[o], W=[o])
                    kb.op('act', lambda e: e.activation(out=o[:, :], in_=o[:, :], func=(AF.Identity if os.environ.get('NOSIN') else AF.Sin)), R=[o], W=[o])
                    kb.dma(self.ROPE[s, which, :, :], o[:, :], o, R=[o])
            self.act_reset()
            kb.barrier()

    def phase_inproj(self, l, s, src):
        kb = self.kb
        C = self.C
        with ExitStack() as es:
            win = kb.buf(es, "ip_w", [128, 8, INW], BF16)
            for kc in range(8):
                kb.dma(win[:, kc, :], self.wb["w_in"][l, kc * 128:(kc + 1) * 128, :], win, PW=[win])
            xt = [kb.buf(es, "ip_x%d" % i, [128, 4, D], F32) for i in range(2)]
            xT = [kb.buf(es, "ip_xT%d" % i, [128, 8, 512], BF16) for i in range(2)]
            cs = [kb.buf(es, "ip_c%d" % i, [128, 2, 512], F32) for i in range(2)]
            pss = [kb.buf(es, "ip_ps%d" % i, [128, 512], F32, psum=True) for i in range(8)]
            fa = [kb.buf(es, "ip_fa%d" % i, [128, 512], F32) for i in range(2)]
            fb = [kb.buf(es, "ip_fb%d" % i, [128, 512], F32) for i in range(2)]
            t1 = [kb.buf(es, "ip_t1%d" % i, [128, 512], F32) for i in range(2)]
            t2 = [kb.buf(es, "ip_t2%d" % i, [128, 512], F32) for i in range(2)]
            oa = [kb.buf(es, "ip_oa%d" % i, [128, 512], BF16) for i in range(2)]
            ob = [kb.buf(es, "ip_ob%d" % i, [128, 512], BF16) for i in range(2)]
            of = [kb.buf(es, "ip_of%d" % i, [128, 512], F32) for i in range(3)]
            ov = [kb.buf(es, "ip_ov%d" % i, [128, 512], BF16) for i in range(2)]
            tv = [kb.buf(es, "ip_tv%d" % i, [128, 4, 256], BF16) for i in range(2)]
            tg = [kb.buf(es, "ip_tg%d" % i, [128, 4, 24], F32) for i in range(2)]
            cnt = [0]
            pc = [0]
            rc = [0]

            def nextps():
                pc[0] += 1
                return pss[2 + pc[0] % 6]

            for i in range(NT):
                t0 = i * 512
                x_ = xt[i % 2]
                xT_ = xT[i % 2]
                cs_ = cs[i % 2]
                kb.dma(x_[:, :, :], src[s, t0:t0 + 512, :].rearrange("(a p) d -> p a d", p=128), x_, W=[x_])
                kb.dma(cs_[:, 0, :], self.ROPE[s, 0, :, t0:t0 + 512], cs_, PW=[cs_])
                kb.dma(cs_[:, 1, :], self.ROPE[s, 1, :, t0:t0 + 512], cs_, PW=[cs_])
                transpose_in(kb, x_, 4, 8, xT_, C['ident'], pss[0:2], cnt)

                def fm(c0, m, ps):
                    for kc in range(8):
                        kb.op('pe', lambda e, kc=kc: e.matmul(ps[0:m, :], lhsT=win[:, kc, c0:c0 + m], rhs=xT_[:, kc, :], start=(kc == 0), stop=(kc == 7)),
                              R=[win, xT_], PW=[ps])

                pairs = [(0, 128, 128, [(32 * h, self.QT[s, h]) for h in range(4)]),
                         (256, 384, 128, [(32 * h, self.QT[s, 4 + h]) for h in range(4)]),
                         (512, 640, 128, [(0, self.KCT[s, 0]), (32, self.KCT[s, 1]), (64, self.KST[s, 0]), (96, self.KST[s, 1])]),
                         (768, 832, 64, [(0, self.KWT[s, 0]), (32, self.KWT[s, 1])])]
                for (ca, cb, m, dests) in pairs:
                    psa = nextps()
                    psb = nextps()
                    fm(ca, m, psa)
                    fm(cb, m, psb)
                    j = rc[0] % 2
                    rc[0] += 1
                    A, B, T1, T2, OA, OB = fa[j], fb[j], t1[j], t2[j], oa[j], ob[j]
                    kb.op('act', lambda e: e.copy(out=A[0:m, :], in_=psa[0:m, :]), R=[psa], W=[A])
                    kb.op('act', lambda e: e.copy(out=B[0:m, :], in_=psb[0:m, :]), R=[psb], W=[B])
                    kb.op('dve', lambda e: e.tensor_tensor(out=T1[0:m, :], in0=A[0:m, :], in1=cs_[0:m, 0, :], op=ALU.mult), R=[A, cs_], W=[T1])
                    kb.op('pool', lambda e: e.tensor_tensor(out=T2[0:m, :], in0=B[0:m, :], in1=cs_[0:m, 1, :], op=ALU.mult), R=[B, cs_], W=[T2])
                    kb.op('dve', lambda e: e.tensor_tensor(out=OA[0:m, :], in0=T1[0:m, :], in1=T2[0:m, :], op=ALU.subtract), R=[T1, T2], W=[OA])
                    kb.op('pool', lambda e: e.tensor_tensor(out=T1[0:m, :], in0=B[0:m, :], in1=cs_[0:m, 0, :], op=ALU.mult), R=[B, cs_], W=[T1])
                    kb.op('dve', lambda e: e.tensor_tensor(out=T2[0:m, :], in0=A[0:m, :], in1=cs_[0:m, 1, :], op=ALU.mult), R=[A, cs_], W=[T2])
                    kb.op('pool', lambda e: e.tensor_tensor(out=OB[0:m, :], in0=T1[0:m, :], in1=T2[0:m, :], op=ALU.add), R=[T1, T2], W=[OB])
                    for (ro, dst) in dests:
                        kb.dma(dst[0:32, t0:t0 + 512], OA[ro:ro + 32, :], OA, R=[OA])
                        kb.dma(dst[32:64, t0:t0 + 512], OB[ro:ro + 32, :], OB, R=[OB])
                ps = nextps()
                fm(896, 128, ps)
                o_ = ov[i % 2]
                kb.op('act', lambda e: e.copy(out=o_[:, :], in_=ps[:, :]), R=[ps], W=[o_])
                kb.dma(self.VCT[s, 0, :, t0:t0 + 512], o_[0:64, :], o_, R=[o_])
                kb.dma(self.VCT[s, 1, :, t0:t0 + 512], o_[64:128, :], o_, R=[o_])
                for ci in range(6):
                    ps = nextps()
                    fm(1024 + ci * 128, 128, ps)
                    o_ = of[ci % 3]
                    if ci % 2 == 0:
                        kb.op('act', lambda e, o_=o_, ps=ps: e.copy(out=o_[:, :], in_=ps[:, :]), R=[ps], W=[o_])
                    else:
                        kb.op('dve', lambda e, o_=o_, ps=ps: e.tensor_copy(out=o_[:, :], in_=ps[:, :]), R=[ps], W=[o_])
                    if ci < 4:
                        kb.dma(self.ZC[s, ci * 128:(ci + 1) * 128, t0:t0 + 512], o_[:, :], o_, R=[o_])
                    else:
                        kb.dma(self.ZS[s, (ci - 4) * 128:(ci - 3) * 128, t0:t0 + 512], o_[:, :], o_, R=[o_])
                tv_ = tv[i % 2]
                tg_ = tg[i % 2]
                for sub in range(4):
                    ps = nextps()
                    for kc in range(8):
                        kb.op('pe', lambda e, kc=kc, sub=sub, ps=ps: e.matmul(ps[:, 0:280], lhsT=xT_[:, kc, sub * 128:(sub + 1) * 128], rhs=win[:, kc, 1792:2072], start=(kc == 0), stop=(kc == 7)),
                              R=[win, xT_], PW=[ps])
                    kb.op('dve', lambda e, sub=sub, ps=ps: e.tensor_copy(out=tv_[:, sub, :], in_=ps[:, 0:256]), R=[ps], PW=[tv_])
                    kb.op('act', lambda e, sub=sub, ps=ps: e.activation(out=tg_[:, sub, :], in_=ps[:, 256:280], func=AF.Sigmoid), R=[ps], PW=[tg_])
                kb.dma(self.VSW[s, t0:t0 + 512, :].rearrange("(a p) d -> p a d", p=128), tv_[:, :, :], tv_, R=[tv_])
                kb.dma(self.G[s, t0:t0 + 512, :].rearrange("(a p) d -> p a d", p=128), tg_[:, :, :], tg_, R=[tg_])
            kb.barrier()

    def phase_compress(self, l, s):
        kb = self.kb
        with ExitStack() as es:
            w1 = [kb.buf(es, "cp_w1%d" % j, [64, 32, 256], BF16) for j in range(2)]
            w2 = [kb.buf(es, "cp_w2%d" % j, [128, 2, 64], BF16) for j in range(2)]
            b1 = [kb.buf(es, "cp_b1%d" % j, [128, 2], F32) for j in range(2)]
            peT = [kb.buf(es, "cp_pe%d" % j, [64, 32], F32) for j in range(2)]
            b2k = kb.buf(es, "cp_b2k", [64, 1], F32)
            b2v = kb.buf(es, "cp_b2v", [128, 64], F32)
            for j in range(2):
                kb.dma(w1[j][:, :, :], self.wb["cmp_w1"][l, j].rearrange("(t d) h -> d t h", d=64), w1[j], W=[w1[j]])
                kb.dma(w2[j][:, :, :], self.wb["cmp_w2"][l, j].rearrange("(c p) o -> p c o", p=128), w2[j], W=[w2[j]])
                kb.dma(b1[j][:, :], self.w["cmp_b1"][l, j].rearrange("(c p) -> p c", p=128), b1[j], W=[b1[j]], allow_slow_non_contiguous=True)
                kb.dma(peT[j][:, :], self.w["cmp_pe"][l, j].rearrange("t d -> d t"), peT[j], W=[peT[j]], allow_slow_non_contiguous=True)
            kb.dma(b2k[:, :], self.w["cmp_b2"][l, 0].rearrange("(p o) -> p o", o=1), b2k, W=[b2k], allow_slow_non_contiguous=True)
            kb.dma(b2v[:, :], self.w["cmp_b2"][l, 1:2, :].partition_broadcast(128), b2v, W=[b2v])
            kt = [kb.buf(es, "cp_kt%d" % i, [64, L], BF16) for i in range(2)]
            blk = [kb.buf(es, "cp_blk%d" % i, [64, 32, 256], BF16) for i in range(2)]
            u = kb.buf(es, "cp_u", [128, 256], F32)
            tmp = kb.buf(es, "cp_tmp", [128, 256], F32)
            gl = kb.buf(es, "cp_gl", [128, 2, 256], BF16)
            okc = kb.buf(es, "cp_okc", [64, 256], BF16)
            ovc = kb.buf(es, "cp_ovc", [128, 2, 129], BF16)
            pss = [kb.buf(es, "cp_ps%d" % i, [128, 512], F32, psum=True) for i in range(4)]
            n = 0
            for h in range(2):
                for j in range(2):
                    srcT = (self.KCT, self.VCT)[j][s, h]
                    kt_ = kt[n % 2]
                    blk_ = blk[n % 2]
                    kb.dma(kt_[:, :], srcT[:, :], kt_, W=[kt_])
                    ktv = kt_[:, :].rearrange("d (n t) -> d t n", t=16)
                    for tb in range(32):
                        eng = kb.ew()
                        if tb < 16:
                            src_ap = ktv[:, tb, 0:255]
                        else:
                            src_ap = ktv[:, tb - 16, 1:256]
                        kb.op(eng, lambda e, tb=tb, src_ap=src_ap: e.tensor_scalar(out=blk_[:, tb, 0:255], in0=src_ap, scalar1=peT[j][:, tb:tb + 1], scalar2=None, op0=ALU.add),
                              R=[kt_, peT[j]], PW=[blk_])
                    for hc in range(2):
                        ps = pss[hc]
                        for tb in range(32):
                            kb.op('pe', lambda e, tb=tb, hc=hc, ps=ps: e.matmul(ps[:, 0:255], lhsT=w1[j][:, tb, hc * 128:(hc + 1) * 128], rhs=blk_[:, tb, 0:255], start=(tb == 0), stop=(tb == 31)),
                                  R=[w1[j], blk_], PW=[ps])
                        kb.op('act', lambda e, hc=hc, ps=ps: e.activation(out=u[:, 0:255], in_=ps[:, 0:255], func=AF.Identity, bias=b1[j][:, hc:hc + 1], scale=1.0), R=[ps, b1[j]], W=[u])
                        gelu_tanh(kb, u, tmp, gl[:, hc, 0:255], gl, 255)
                    if j == 0:
                        ps = pss[2]
                        for hc in range(2):
                            kb.op('pe', lambda e, hc=hc: e.matmul(ps[0:64, 0:255], lhsT=w2[0][:, hc, :], rhs=gl[:, hc, 0:255], start=(hc == 0), stop=(hc == 1)), R=[w2[0], gl], PW=[ps])
                        kb.op('pool', lambda e: e.memset(okc[:, :], 0.0), W=[okc])
                        kb.op('act', lambda e: e.activation(out=okc[:, 0:255], in_=ps[0:64, 0:255], func=AF.Identity, bias=b2k[:, 0:1], scale=1.0), R=[ps, b2k], PW=[okc])
                        kb.dma(self.KCC[s, h, :, :], okc[:, :], okc, R=[okc])
                    else:
                        kb.op('pool', lambda e: e.memset(ovc[:, :, :], 0.0), W=[ovc])
                        kb.dma(ovc[:, :, 65:129], self.c_ovl.rearrange("(c p) o -> p c o", p=128), ovc, PW=[ovc])
                        kb.op('pool', lambda e: e.memset(ovc[:, :, 64:65], 1.0), PW=[ovc])
                        for c in range(2):
                            rows = 128 if c == 0 else 127
                            ps = pss[2 + c]
                            for hc in range(2):
                                kb.op('pe', lambda e, hc=hc, c=c, rows=rows, ps=ps: e.matmul(ps[0:rows, 0:64], lhsT=gl[:, hc, c * 128:c * 128 + rows], rhs=w2[1][:, hc, :], start=(hc == 0), stop=(hc == 1)),
                                      R=[w2[1], gl], PW=[ps])
                            kb.op('dve', lambda e, c=c, rows=rows, ps=ps: e.tensor_tensor(out=ovc[0:rows, c, 0:64], in0=ps[0:rows, 0:64], in1=b2v[0:rows, :], op=ALU.add), R=[ps, b2v], PW=[ovc])
                        kb.dma(self.VCA[s, h].rearrange("(c p) o -> p c o", p=128), ovc[:, :, :], ovc, R=[ovc])
                    n += 1
            kb.barrier()

    def phase_attn(self, l, s):
        kb = self.kb
        C = self.C
        with ExitStack() as es:
            kcT = kb.buf(es, "at_kc", [64, 256], BF16)
            vca = kb.buf(es, "at_vca", [128, 2, 129], BF16)
            kaug = kb.buf(es, "at_kaug", [128, L], BF16)
            kw = kb.buf(es, "at_kw", [64, L], BF16)
            vs = kb.buf(es, "at_vs", [128, 32, 65], BF16)
            vw = kb.buf(es, "at_vw", [128, 32, 65], BF16)
            qa = [[kb.buf(es, "at_q%d_%d" % (g, i), [128, 512], BF16) for i in range(2)] for g in range(4)]
            gt = [kb.buf(es, "at_g%d" % i, [128, 4, 24], F32) for i in range(2)]
            oacc = kb.buf(es, "at_oacc", [128, 4, 256], F32)
            imp = kb.buf(es, "at_imp", [128, 4, 64], F32)
            sc2 = kb.buf(es, "at_sc2", [128, 64], F32)
            selm = kb.buf(es, "at_selm", [128, 64], F32)
            t8 = kb.buf(es, "at_t8", [128, 16], F32)
            rv = kb.buf(es, "at_rv", [128, 8], F32)
            pt = [kb.buf(es, "at_pt%d" % i, [128, 512], BF16) for i in range(3)]
            mixo = [kb.buf(es, "at_mo%d" % i, [128, 2, 512], BF16) for i in range(2)]
            ps_s = [kb.buf(es, "at_pss%d" % i, [128, 512], F32, psum=True) for i in range(2)]
            acc = kb.buf(es, "at_acc", [128, 4, 512], F32, psum=True)
            ftmp = kb.buf(es, "at_ftmp", [128, 4, 64], F32)
            ps_t = [kb.buf(es, "at_pst%d" % i, [128, 512], F32, psum=True) for i in range(2)]
            kb.dma(kaug[64:128, :], self.c_emat[:, :], kaug, PW=[kaug])
            kb.op('pool', lambda e: e.memset(vs[:, :, 64:65], 1.0), PW=[vs])
            kb.op('pool', lambda e: e.memset(vw[:, :, 64:65], 1.0), PW=[vw])
            sc = [0]
            pcnt = [0]
            tcnt = [0]
            for k in range(2):
                kb.dma(kcT[:, :], self.KCC[s, k], kcT, W=[kcT])
                kb.dma(vca[:, :, :], self.VCA[s, k].rearrange("(c p) o -> p c o", p=128), vca, W=[vca])
                kb.dma(kaug[0:64, :], self.KST[s, k], kaug, PW=[kaug])
                kb.dma(kw[:, :], self.KWT[s, k], kw, W=[kw])
                kb.dma(vs[:, :, 0:64], self.VSW[s, :, k * 64:(k + 1) * 64].rearrange("(c p) d -> p c d", p=128), vs, PW=[vs])
                kb.dma(vw[:, :, 0:64], self.VSW[s, :, 128 + k * 64:128 + (k + 1) * 64].rearrange("(c p) d -> p c d", p=128), vw, PW=[vw])
                for i in range(NT):
                    q0 = i * 512
                    Q = [qa[g][i % 2] for g in range(4)]
                    gt_ = gt[i % 2]
                    for g in range(4):
                        kb.dma(Q[g][0:64, :], self.QT[s, 4 * k + g, :, q0:q0 + 512], Q[g], PW=[Q[g]])
                    kb.dma(gt_[:, :, :], self.G[s, q0:q0 + 512, :].rearrange("(a p) d -> p a d", p=128), gt_, W=[gt_])

                    def score(lhsT_ap, lbufs, g, krows, c0, c1, rows=128):
                        ps = ps_s[sc[0] % 2]
                        sc[0] += 1
                        kb.op('pe', lambda e: e.matmul(ps[0:rows, c0:c1], lhsT=lhsT_ap, rhs=Q[g][0:krows, c0:c1], start=True, stop=True), R=lbufs + [Q[g]], W=[ps])
                        p_ = pt[pcnt[0] % 3]
                        pcnt[0] += 1
                        kb.op('act', lambda e: e.activation(out=p_[0:rows, c0:c1], in_=ps[0:rows, c0:c1], func=AF.Exp, scale=0.125), R=[ps], W=[p_])
                        return p_

                    def run_jobs(jobs):
                        st = [None] * len(jobs)
                        if jobs:
                            st[0] = jobs[0][0]()
                        for j in range(len(jobs)):
                            if j + 1 < len(jobs):
                                st[j + 1] = jobs[j + 1][0]()
                            jobs[j][1](st[j])

                    def finish(g, br, first, with_imp=False):
                        col = 3 * (4 * k + g) + br
                        kb.op('dve', lambda e: e.tensor_scalar(out=rv[:, 0:4], in0=acc[:, :, 64], scalar1=1e-30, scalar2=None, op0=ALU.max), R=[acc], W=[rv])
                        kb.op('dve', lambda e: e.reciprocal(out=rv[:, 0:4], in_=rv[:, 0:4]), R=[rv], W=[rv])
                        if with_imp:
                            rb = rv[:, 0:4].unsqueeze(2).to_broadcast([128, 4, 64])
                            if g == 0:
                                kb.op('dve', lambda e: e.tensor_tensor(out=imp[:, :, :], in0=acc[:, :, 65:129], in1=rb, op=ALU.mult), R=[acc, rv], W=[imp])
                            else:
                                kb.op('dve', lambda e: e.tensor_tensor(out=ftmp[:, :, :], in0=acc[:, :, 65:129], in1=rb, op=ALU.mult), R=[acc, rv], W=[ftmp])
                                kb.op('pool', lambda e: e.tensor_tensor(out=imp[:, :, :], in0=imp[:, :, :], in1=ftmp[:, :, :], op=ALU.add), R=[imp, ftmp], W=[imp])
                        kb.op('dve', lambda e, col=col: e.tensor_tensor(out=rv[:, 4:8], in0=rv[:, 0:4], in1=gt_[:, :, col], op=ALU.mult), R=[rv, gt_], PW=[rv])
                        wb = rv[:, 4:8].unsqueeze(2).to_broadcast([128, 4, 64])
                        if first:
                            kb.op('dve', lambda e: e.tensor_tensor(out=oacc[:, :, g * 64:(g + 1) * 64], in0=acc[:, :, 0:64], in1=wb, op=ALU.mult), R=[acc, rv], PW=[oacc])
                        else:
                            kb.op('dve', lambda e: e.tensor_tensor(out=ftmp[:, :, :], in0=acc[:, :, 0:64], in1=wb, op=ALU.mult), R=[acc, rv], W=[ftmp])
                            kb.op('pool', lambda e: e.tensor_tensor(out=oacc[:, :, g * 64:(g + 1) * 64], in0=oacc[:, :, g * 64:(g + 1) * 64], in1=ftmp[:, :, :], op=ALU.add), R=[oacc, ftmp], PW=[oacc])

                    ncnt = min(255, 32 * i + 31)
                    chunks = [(0, min(128, ncnt))] + ([(1, ncnt - 128)] if ncnt > 128 else [])
                    jobs = []
                    for g in range(4):
                        for (c, rows) in chunks:
                            def sfn(g=g, c=c, rows=rows):
                                p_ = score(kcT[0:64, c * 128:c * 128 + rows], [kcT], g, 64, 0, 512, rows)
                                last_end = 16 * (c * 128 + rows - 1) + 31
                                if last_end > q0:
                                    kb.op('pool', lambda e: e.affine_select(out=p_[0:rows, :], in_=p_[0:rows, :], pattern=[[1, 512]], compare_op=ALU.is_ge, fill=0.0,
                                                                             base=q0 - 31 - 16 * 128 * c, channel_multiplier=-16), R=[p_], W=[p_])
                                return p_

                            def pfn(p_, g=g, c=c, rows=rows):
                                for sub in range(4):
                                    kb.op('pe', lambda e, sub=sub: e.matmul(acc[:, sub, 0:129], lhsT=p_[0:rows, sub * 128:(sub + 1) * 128], rhs=vca[0:rows, c, :],
                                                                              start=(c == 0), stop=(c == chunks[-1][0])), R=[p_, vca], PW=[acc])
                                if c == chunks[-1][0]:
                                    finish(g, 0, True, with_imp=True)
                            jobs.append((sfn, pfn))
                    run_jobs(jobs)
                    for sub in range(4):
                        b0 = 8 * i + 2 * sub
                        for hh in range(2):
                            b = b0 + hh
                            r0 = 64 * hh
                            if b + 1 < 64:
                                kb.op('pool', lambda e, sub=sub, r0=r0, b=b: e.memset(imp[r0:r0 + 64, sub, b + 1:64], -1.0), PW=[imp])
                            kb.op('pool', lambda e, sub=sub, r0=r0, b=b: e.memset(imp[r0:r0 + 64, sub, max(b - 1, 0):b + 1], 1e9), PW=[imp])
                            kb.op('pool', lambda e, sub=sub, r0=r0: e.memset(imp[r0:r0 + 64, sub, 0:1], 1e9), PW=[imp])
                        kb.op('dve', lambda e, sub=sub: e.max(out=t8[:, 0:8], in_=imp[:, sub, :]), R=[imp], PW=[t8])
                        kb.op('dve', lambda e, sub=sub: e.match_replace(out=sc2[:, :], in_to_replace=t8[:, 0:8], in_values=imp[:, sub, :], imm_value=-1e30), R=[imp, t8], W=[sc2])
                        kb.op('dve', lambda e: e.max(out=t8[:, 8:16], in_=sc2[:, :]), R=[sc2], PW=[t8])
                        kb.op('dve', lambda e, sub=sub: e.tensor_scalar(out=selm[:, :], in0=imp[:, sub, :], scalar1=t8[:, 15:16], scalar2=None, op0=ALU.is_ge), R=[imp, t8], W=[selm])
                        pst = ps_t[tcnt[0] % 2]
                        tcnt[0] += 1
                        kb.op('pe', lambda e, pst=pst: e.transpose(pst[0:64, 0:128], selm[:, :], C['ident'][:, :]), R=[selm, C['ident']], W=[pst])
                        for g in range(4):
                            kb.op('act', lambda e, g=g, sub=sub, pst=pst: e.activation(out=Q[g][64:128, sub * 128:(sub + 1) * 128], in_=pst[0:64, 0:128], func=AF.Identity, bias=-BIG, scale=BIG),
                                  R=[pst], PW=[Q[g]])
                    jobs = []
                    for g in range(4):
                        nch = 4 * i + 4
                        for c in range(nch):
                            def sfn(g=g, c=c):
                                off = 128 * c - q0
                                c0 = max(off, 0)
                                p_ = score(kaug[:, c * 128:(c + 1) * 128], [kaug], g, 128, c0, 512)
                                if off >= 0:
                                    kb.op('pool', lambda e: e.affine_select(out=p_[:, c0:c0 + 128], in_=p_[:, c0:c0 + 128], pattern=[[1, 128]], compare_op=ALU.is_ge, fill=0.0,
                                                                             base=0, channel_multiplier=-1), R=[p_], W=[p_])
                                return p_

                            def pfn(p_, g=g, c=c, nch=nch):
                                c0 = max(128 * c - q0, 0)
                                for sub in range(4):
                                    if sub * 128 < c0:
                                        continue
                                    kb.op('pe', lambda e, sub=sub: e.matmul(acc[:, sub, 0:65], lhsT=p_[:, sub * 128:(sub + 1) * 128], rhs=vs[:, c, :],
                                                                              start=(c == 0), stop=(c == 4 * i + sub)), R=[p_, vs], PW=[acc])
                                if c == nch - 1:
                                    finish(g, 1, False)
                            jobs.append((sfn, pfn))
                    run_jobs(jobs)
                    jobs = []
                    for g in range(4):
                        clist = list(range(max(0, 4 * i - 4), 4 * i + 4))
                        for c in clist:
                            off = 128 * c - q0
                            if off >= 0:
                                c0, c1 = off, 512
                            else:
                                m = (off + 512) // 128
                                c0, c1 = 0, 128 * (m + 1)

                            def sfn(g=g, c=c, off=off, c0=c0, c1=c1):
                                p_ = score(kw[0:64, c * 128:(c + 1) * 128], [kw], g, 64, c0, c1)
                                if off >= 0:
                                    kb.op('pool', lambda e: e.affine_select(out=p_[:, c0:c0 + 128], in_=p_[:, c0:c0 + 128], pattern=[[1, 128]], compare_op=ALU.is_ge, fill=0.0,
                                                                             base=0, channel_multiplier=-1), R=[p_], W=[p_])
                                else:
                                    kb.op('pool', lambda e: e.affine_select(out=p_[:, c1 - 128:c1], in_=p_[:, c1 - 128:c1], pattern=[[-1, 128]], compare_op=ALU.is_ge, fill=0.0,
                                                                             base=-1, channel_multiplier=1), R=[p_], W=[p_])
                                return p_

                            def pfn(p_, g=g, c=c, c0=c0, c1=c1, last=(c == clist[-1])):
                                for sub in range(4):
                                    if not (c0 <= sub * 128 < c1):
                                        continue
                                    kb.op('pe', lambda e, sub=sub: e.matmul(acc[:, sub, 0:65], lhsT=p_[:, sub * 128:(sub + 1) * 128], rhs=vw[:, c, :],
                                                                              start=(c == max(0, 4 * i + sub - 4)), stop=(c == 4 * i + sub)), R=[p_, vw], PW=[acc])
                                if last:
                                    finish(g, 2, False)
                            jobs.append((sfn, pfn))
                    run_jobs(jobs)
                    mo = mixo[i % 2]
                    for fc in range(2):
                        pst = ps_t[tcnt[0] % 2]
                        tcnt[0] += 1
                        for sub in range(4):
                            kb.op('pe', lambda e, fc=fc, sub=sub, pst=pst: e.transpose(pst[:, sub * 128:(sub + 1) * 128], oacc[:, sub, fc * 128:(fc + 1) * 128], C['ident'][:, :]),
                                  R=[oacc, C['ident']], PW=[pst])
                        kb.op('act', lambda e, fc=fc, pst=pst: e.copy(out=mo[:, fc, :], in_=pst[:, :]), R=[pst], PW=[mo])
                    kb.dma(self.MIXT[s, k * 256:(k + 1) * 256, q0:q0 + 512].rearrange("(c p) t -> p c t", p=128), mo[:, :, :], mo, R=[mo])
            kb.barrier()

    def phase_conv(self, l, s):
        kb = self.kb
        with ExitStack() as es:
            cw = kb.buf(es, "cv_w", [128, 2, 31], F32)
            cb = kb.buf(es, "cv_b", [128, 2], F32)
            lg = kb.buf(es, "cv_g", [128, 2], F32)
            lb = kb.buf(es, "cv_lb", [128, 2], F32)
            ones = kb.buf(es, "cv_ones", [128, 128], F32)
            for c in range(2):
                kb.dma(cw[:, c, :], self.w["conv_w"][l][:, c * 128:(c + 1) * 128].rearrange("k p -> p k"), cw, PW=[cw], allow_slow_non_contiguous=True)
            for (t_, nm) in ((cb, "conv_b"), (lg, "conv_ln_g"), (lb, "conv_ln_b")):
                kb.dma(t_[:, :], self.w[nm][l].rearrange("(c p) -> p c", p=128), t_, W=[t_], allow_slow_non_contiguous=True)
            kb.op('pool', lambda e: e.memset(ones[:, :], 1.0 / 256.0), W=[ones])
            a = [kb.buf(es, "cv_a%d" % c, [128, L], F32) for c in range(2)]
            u = [kb.buf(es, "cv_u%d" % c, [128, L + 32], F32) for c in range(2)]
            y = [kb.buf(es, "cv_y%d" % c, [128, L], F32) for c in range(2)]
            sq = [kb.buf(es, "cv_sq%d" % c, [128, 512], F32) for c in range(2)]
            mean = kb.buf(es, "cv_mean", [128, 512], F32)
            rstd = kb.buf(es, "cv_rstd", [128, 512], F32)
            tmp = kb.buf(es, "cv_tmp", [128, 512], F32)
            ob = [kb.buf(es, "cv_ob%d" % c, [128, 512], BF16) for c in range(2)]
            ps1 = kb.buf(es, "cv_ps1", [128, 512], F32, psum=True)
            ps2 = kb.buf(es, "cv_ps2", [128, 512], F32, psum=True)
            eps = self.C['eps']
            for c in range(2):
                kb.dma(a[c][:, :], self.ZC[s, c * 128:(c + 1) * 128, :], a[c], W=[a[c]])
                kb.dma(y[c][:, :], self.ZC[s, 256 + c * 128:256 + (c + 1) * 128, :], y[c], W=[y[c]])
                kb.op('act', lambda e, c=c: e.activation(out=y[c][:, :], in_=y[c][:, :], func=AF.Sigmoid), R=[y[c]], W=[y[c]])
                kb.op('pool', lambda e, c=c: e.memset(u[c][:, 0:32], 0.0), PW=[u[c]])
                kb.op('pool', lambda e, c=c: e.tensor_tensor(out=u[c][:, 32:L + 32], in0=a[c][:, :], in1=y[c][:, :], op=ALU.mult), R=[a[c], y[c]], PW=[u[c]])
                kb.op('dve', lambda e, c=c: e.tensor_scalar(out=y[c][:, :], in0=u[c][:, 2:L + 2], scalar1=cw[:, c, 0:1], scalar2=cb[:, c:c + 1], op0=ALU.mult, op1=ALU.add), R=[u[c], cw, cb], W=[y[c]])
                for k in range(1, 31):
                    kb.op('dve', lambda e, c=c, k=k: e.scalar_tensor_tensor(out=y[c][:, :], in0=u[c][:, 2 + k:L + 2 + k], scalar=cw[:, c, k:k + 1], in1=y[c][:, :], op0=ALU.mult, op1=ALU.add),
                          R=[u[c], cw, y[c]], W=[y[c]])
            for i in range(NT):
                t0 = i * 512
                for c in range(2):
                    kb.op('act', lambda e, c=c: e.activation(out=sq[c][:, :], in_=y[c][:, t0:t0 + 512], func=AF.Square), R=[y[c]], W=[sq[c]])
                for c in range(2):
                    kb.op('pe', lambda e, c=c: e.matmul(ps1[:, :], lhsT=ones[:, :], rhs=y[c][:, t0:t0 + 512], start=(c == 0), stop=(c == 1)), R=[ones, y[c]], PW=[ps1])
                for c in range(2):
                    kb.op('pe', lambda e, c=c: e.matmul(ps2[:, :], lhsT=ones[:, :], rhs=sq[c][:, :], start=(c == 0), stop=(c == 1)), R=[ones, sq[c]], PW=[ps2])
                kb.op('act', lambda e: e.copy(out=mean[:, :], in_=ps1[:, :]), R=[ps1], W=[mean])
                kb.op('dve', lambda e: e.tensor_tensor(out=tmp[:, :], in0=mean[:, :], in1=mean[:, :], op=ALU.mult), R=[mean], W=[tmp])
                kb.op('dve', lambda e: e.tensor_tensor(out=rstd[:, :], in0=ps2[:, :], in1=tmp[:, :], op=ALU.subtract), R=[ps2, tmp], W=[rstd])
                kb.op('act', lambda e: e.activation(out=rstd[:, :], in_=rstd[:, :], func=AF.Sqrt, bias=eps[:, 0:1], scale=1.0), R=[rstd, eps], W=[rstd])
                kb.op('dve', lambda e: e.reciprocal(out=rstd[:, :], in_=rstd[:, :]), R=[rstd], W=[rstd])
                for c in range(2):
                    kb.op('dve', lambda e, c=c: e.tensor_tensor(out=tmp[:, :], in0=y[c][:, t0:t0 + 512], in1=mean[:, :], op=ALU.subtract), R=[y[c], mean], W=[tmp])
                    kb.op('pool', lambda e: e.tensor_tensor(out=tmp[:, :], in0=tmp[:, :], in1=rstd[:, :], op=ALU.mult), R=[tmp, rstd], W=[tmp])
                    o_ = ob[c]
                    kb.op('act', lambda e, c=c, o_=o_: e.activation(out=o_[:, :], in_=tmp[:, :], func=AF.Silu, bias=lb[:, c:c + 1], scale=lg[:, c:c + 1]), R=[tmp, lb, lg], W=[o_])
                    kb.dma(self.MIXT[s, 512 + c * 128:512 + (c + 1) * 128, t0:t0 + 512], o_[:, :], o_, R=[o_])
            kb.barrier()

    def sincos_small(self, es, th, n, cs_out, sn_out, tag):
        kb = self.kb
        kf = kb.buf(es, "sc_kf" + tag, [128, n], F32)
        ki = kb.buf(es, "sc_ki" + tag, [128, n], I32)
        r = kb.buf(es, "sc_r" + tag, [128, n], F32)
        m = kb.buf(es, "sc_m" + tag, [128, n], F32)
        HI = 6.28125
        LO = TWO_PI - HI
        kb.op('dve', lambda e: e.tensor_scalar(out=kf[:, :], in0=th[:, :], scalar1=1.0 / TWO_PI, scalar2=None, op0=ALU.mult), R=[th], W=[kf])
        kb.op('dve', lambda e: e.tensor_copy(out=ki[:, :], in_=kf[:, :]), R=[kf], W=[ki])
        kb.op('dve', lambda e: e.tensor_copy(out=kf[:, :], in_=ki[:, :]), R=[ki], W=[kf])
        kb.op('dve', lambda e: e.scalar_tensor_tensor(out=r[:, :], in0=kf[:, :], scalar=-HI, in1=th[:, :], op0=ALU.mult, op1=ALU.add), R=[kf, th], W=[r])
        kb.op('dve', lambda e: e.scalar_tensor_tensor(out=r[:, :], in0=kf[:, :], scalar=-LO, in1=r[:, :], op0=ALU.mult, op1=ALU.add), R=[kf, r], W=[r])
        for o, shift in ((sn_out, 0.0), (cs_out, math.pi / 2)):
            kb.op('dve', lambda e, o=o, shift=shift: e.tensor_scalar(out=o[:, :], in0=r[:, :], scalar1=shift, scalar2=None, op0=ALU.add), R=[r], W=[o])
            for _ in range(2):
                kb.op('dve', lambda e, o=o: e.tensor_scalar(out=m[:, :], in0=o[:, :], scalar1=math.pi, scalar2=-TWO_PI, op0=ALU.is_gt, op1=ALU.mult), R=[o], W=[m])
                kb.op('dve', lambda e, o=o: e.tensor_tensor(out=o[:, :], in0=o[:, :], in1=m[:, :], op=ALU.add), R=[o, m], W=[o])
                kb.op('dve', lambda e, o=o: e.tensor_scalar(out=m[:, :], in0=o[:, :], scalar1=-math.pi, scalar2=TWO_PI, op0=ALU.is_lt, op1=ALU.mult), R=[o], W=[m])
                kb.op('dve', lambda e, o=o: e.tensor_tensor(out=o[:, :], in0=o[:, :], in1=m[:, :], op=ALU.add), R=[o, m], W=[o])
            kb.op('dve', lambda e, o=o: e.tensor_scalar(out=o[:, :], in0=o[:, :], scalar1=math.pi, scalar2=-math.pi, op0=ALU.min, op1=ALU.max), R=[o], W=[o])
            kb.op('act', lambda e, o=o: e.activation(out=o[:, :], in_=o[:, :], func=AF.Sin), R=[o], W=[o])
        self.act_reset()

    def phase_s5(self, l, s):
        kb = self.kb
        C = self.C
        TT = ALU.mult
        with ExitStack() as es:
            are = kb.buf(es, "s5_are", [128, 8], F32)
            aim = kb.buf(es, "s5_aim", [128, 8], F32)
            dt = kb.buf(es, "s5_dt", [128, 8], F32)
            kb.dma(are[:, :], self.w["s5_a_re"][l].rearrange("g n -> (g n)").rearrange("(m p) -> p m", p=128), are, W=[are], allow_slow_non_contiguous=True)
            kb.dma(aim[:, :], self.w["s5_a_im"][l].rearrange("g n -> (g n)").rearrange("(m p) -> p m", p=128), aim, W=[aim], allow_slow_non_contiguous=True)
            ldt2 = self.w["s5_log_dt"][l:l + 1, :].rearrange("o (m h) -> o h m", h=2)
            kb.dma(dt[0:64, :], ldt2[:, 0, :].partition_broadcast(64), dt, PW=[dt], allow_slow_non_contiguous=True)
            kb.dma(dt[64:128, :], ldt2[:, 1, :].partition_broadcast(64), dt, PW=[dt], allow_slow_non_contiguous=True)
            kb.op('act', lambda e: e.activation(out=dt[:, :], in_=dt[:, :], func=AF.Exp), R=[dt], W=[dt])
            rho = kb.buf(es, "s5_rho", [128, 8], F32)
            th = kb.buf(es, "s5_th", [128, 8], F32)
            kb.op('dve', lambda e: e.tensor_tensor(out=rho[:, :], in0=are[:, :], in1=dt[:, :], op=TT), R=[are, dt], W=[rho])
            kb.op('act', lambda e: e.activation(out=rho[:, :], in_=rho[:, :], func=AF.Exp), R=[rho], W=[rho])
            kb.op('dve', lambda e: e.tensor_tensor(out=th[:, :], in0=aim[:, :], in1=dt[:, :], op=TT), R=[aim, dt], W=[th])
            c1 = kb.buf(es, "s5_c1", [128, 8], F32)
            s1 = kb.buf(es, "s5_s1", [128, 8], F32)
            self.sincos_small(es, th, 8, c1, s1, "a")
            abr = kb.buf(es, "s5_abr", [128, 8], F32)
            abi = kb.buf(es, "s5_abi", [128, 8], F32)
            den = kb.buf(es, "s5_den", [128, 8], F32)
            t1 = kb.buf(es, "s5_t1", [128, 8], F32)
            cfr = kb.buf(es, "s5_cfr", [128, 8], F32)
            cfi = kb.buf(es, "s5_cfi", [128, 8], F32)
            V = lambda fn, R, W: kb.op('dve', fn, R=R, W=W)
            V(lambda e: e.tensor_tensor(out=abr[:, :], in0=rho[:, :], in1=c1[:, :], op=TT), [rho, c1], [abr])
            V(lambda e: e.tensor_tensor(out=abi[:, :], in0=rho[:, :], in1=s1[:, :], op=TT), [rho, s1], [abi])
            V(lambda e: e.tensor_tensor(out=den[:, :], in0=are[:, :], in1=are[:, :], op=TT), [are], [den])
            V(lambda e: e.tensor_tensor(out=t1[:, :], in0=aim[:, :], in1=aim[:, :], op=TT), [aim], [t1])
            V(lambda e: e.tensor_tensor(out=den[:, :], in0=den[:, :], in1=t1[:, :], op=ALU.add), [den, t1], [den])
            V(lambda e: e.reciprocal(out=den[:, :], in_=den[:, :]), [den], [den])
            V(lambda e: e.tensor_scalar(out=abr[:, :], in0=abr[:, :], scalar1=-1.0, scalar2=None, op0=ALU.add), [abr], [abr])
            V(lambda e: e.tensor_tensor(out=cfr[:, :], in0=abr[:, :], in1=are[:, :], op=TT), [abr, are], [cfr])
            V(lambda e: e.tensor_tensor(out=t1[:, :], in0=abi[:, :], in1=aim[:, :], op=TT), [abi, aim], [t1])
            V(lambda e: e.tensor_tensor(out=cfr[:, :], in0=cfr[:, :], in1=t1[:, :], op=ALU.add), [cfr, t1], [cfr])
            V(lambda e: e.tensor_tensor(out=cfr[:, :], in0=cfr[:, :], in1=den[:, :], op=TT), [cfr, den], [cfr])
            V(lambda e: e.tensor_tensor(out=cfi[:, :], in0=abi[:, :], in1=are[:, :], op=TT), [abi, are], [cfi])
            V(lambda e: e.tensor_tensor(out=t1[:, :], in0=abr[:, :], in1=aim[:, :], op=TT), [abr, aim], [t1])
            V(lambda e: e.tensor_tensor(out=cfi[:, :], in0=cfi[:, :], in1=t1[:, :], op=ALU.subtract), [cfi, t1], [cfi])
            V(lambda e: e.tensor_tensor(out=cfi[:, :], in0=cfi[:, :], in1=den[:, :], op=TT), [cfi, den], [cfi])
            bre = kb.buf(es, "s5_bre", [128, 8, 16], F32)
            bim = kb.buf(es, "s5_bim", [128, 8, 16], F32)
            kb.dma(bre[:, :, :], self.w["s5_b_re"][l].rearrange("g n c -> (g n) c").rearrange("(m p) c -> p m c", p=128), bre, W=[bre])
            kb.dma(bim[:, :, :], self.w["s5_b_im"][l].rearrange("g n c -> (g n) c").rearrange("(m p) c -> p m c", p=128), bim, W=[bim])
            bpad = [kb.buf(es, "s5_bpad%d" % j, [128, 8, 128], F32) for j in range(2)]
            t16 = kb.buf(es, "s5_t16", [128, 16], F32)
            for j in range(2):
                kb.op('pool', lambda e, j=j: e.memset(bpad[j][:, :, :], 0.0), W=[bpad[j]])
            for m in range(8):
                for hh in range(2):
                    r0 = 64 * hh
                    co = ((2 * m + hh) * 16) % 128
                    rs_ = slice(r0, r0 + 64)
                    V(lambda e, m=m, rs_=rs_: e.tensor_scalar(out=t16[rs_, :], in0=bim[rs_, m, :], scalar1=cfi[rs_, m:m + 1], scalar2=None, op0=TT), [bim, cfi], [t16])
                    kb.op('dve', lambda e, m=m, rs_=rs_, co=co: e.scalar_tensor_tensor(out=bpad[0][rs_, m, co:co + 16], in0=bre[rs_, m, :], scalar=cfr[rs_, m:m + 1], in1=t16[rs_, :], op0=TT, op1=ALU.subtract),
                          R=[bre, cfr, t16], PW=[bpad[0]])
                    V(lambda e, m=m, rs_=rs_: e.tensor_scalar(out=t16[rs_, :], in0=bre[rs_, m, :], scalar1=cfi[rs_, m:m + 1], scalar2=None, op0=TT), [bre, cfi], [t16])
                    kb.op('dve', lambda e, m=m, rs_=rs_, co=co: e.scalar_tensor_tensor(out=bpad[1][rs_, m, co:co + 16], in0=bim[rs_, m, :], scalar=cfr[rs_, m:m + 1], in1=t16[rs_, :], op0=TT, op1=ALU.add),
                          R=[bim, cfr, t16], PW=[bpad[1]])
            pss = [kb.buf(es, "s5_ps%d" % i, [128, 512], F32, psum=True) for i in range(8)]
            wB = [kb.buf(es, "s5_wB%d" % j, [128, 8, 128], BF16) for j in range(2)]
            for j in range(2):
                for m in range(8):
                    ps = pss[m % 2]
                    kb.op('pe', lambda e, j=j, m=m, ps=ps: e.transpose(ps[:, 0:128], bpad[j][:, m, :], C['ident'][:, :]), R=[bpad[j], C['ident']], W=[ps])
                    kb.op('act', lambda e, j=j, m=m, ps=ps: e.copy(out=wB[j][:, m, :], in_=ps[:, 0:128]), R=[ps], PW=[wB[j]])
            craw = [kb.buf(es, "s5_craw%d" % j, [128, 2, 64], F32) for j in range(2)]
            cT = [kb.buf(es, "s5_cT%d" % j, [64, 256], F32) for j in range(2)]
            wC = [kb.buf(es, "s5_wC%d" % j, [128, 8, 128], BF16) for j in range(2)]
            for j, nm in enumerate(("s5_c_re", "s5_c_im")):
                kb.dma(craw[j][:, :, :], self.w[nm][l].rearrange("g c n -> (g c) n").rearrange("(a p) n -> p a n", p=128), craw[j], W=[craw[j]])
                kb.op('pool', lambda e, j=j: e.memset(wC[j][:, :, :], 0.0), W=[wC[j]])
                for a_ in range(2):
                    ps = pss[2 + a_]
                    kb.op('pe', lambda e, j=j, a_=a_, ps=ps: e.transpose(ps[0:64, 0:128], craw[j][:, a_, :], C['ident'][:, :]), R=[craw[j], C['ident']], W=[ps])
                    kb.op('act', lambda e, j=j, a_=a_, ps=ps: e.copy(out=cT[j][:, a_ * 128:(a_ + 1) * 128], in_=ps[0:64, 0:128]), R=[ps], PW=[cT[j]])
                sgn = 1.0 if j == 0 else -1.0
                for m in range(8):
                    for hh in range(2):
                        g_ = 2 * m + hh
                        co = (g_ * 16) % 128
                        kb.op('dve', lambda e, j=j, m=m, hh=hh, g_=g_, co=co, sgn=sgn: e.tensor_scalar(out=wC[j][64 * hh:64 * hh + 64, m, co:co + 16], in0=cT[j][:, g_ * 16:(g_ + 1) * 16],
                                                                                                  scalar1=sgn, scalar2=None, op0=TT), R=[cT[j]], PW=[wC[j]])
            CT = kb.buf(es, "s5_CT", [128, 8, 512], F32)
            ST = kb.buf(es, "s5_ST", [128, 8, 512], F32)
            RH = kb.buf(es, "s5_RH", [128, 8, 512], F32)
            ck = kb.buf(es, "s5_ck", [128, 8], F32)
            sk = kb.buf(es, "s5_sk", [128, 8], F32)
            tk = kb.buf(es, "s5_tk", [128, 8], F32)
            tk2 = kb.buf(es, "s5_tk2", [128, 8], F32)
            tb1 = kb.buf(es, "s5_tb1", [128, 8, 256], F32)
            tb2 = kb.buf(es, "s5_tb2", [128, 8, 256], F32)
            V(lambda e: e.tensor_copy(out=ck[:, :], in_=c1[:, :]), [c1], [ck])
            V(lambda e: e.tensor_copy(out=sk[:, :], in_=s1[:, :]), [s1], [sk])
            kb.op('pool', lambda e: e.memset(CT[:, :, 0:1], 1.0), PW=[CT])
            kb.op('pool', lambda e: e.memset(ST[:, :, 0:1], 0.0), PW=[ST])
            for m in range(8):
                V(lambda e, m=m: e.tensor_copy(out=RH[:, m, :], in_=rho[:, m:m + 1].to_broadcast([128, 512])), [rho], [RH])
            w_ = 1
            for kk in range(10):
                if kk < 9:
                    cb_ = ck[:, :].unsqueeze(2).to_broadcast([128, 8, w_])
                    sb_ = sk[:, :].unsqueeze(2).to_broadcast([128, 8, w_])
                    V(lambda e, w_=w_, cb_=cb_: e.tensor_tensor(out=tb1[:, :, 0:w_], in0=CT[:, :, 0:w_], in1=cb_, op=TT), [CT, ck], [tb1])
                    V(lambda e, w_=w_, sb_=sb_: e.tensor_tensor(out=tb2[:, :, 0:w_], in0=ST[:, :, 0:w_], in1=sb_, op=TT), [ST, sk], [tb2])
                    kb.op('dve', lambda e, w_=w_: e.tensor_tensor(out=CT[:, :, w_:2 * w_], in0=tb1[:, :, 0:w_], in1=tb2[:, :, 0:w_], op=ALU.subtract), R=[tb1, tb2], PW=[CT])
                    V(lambda e, w_=w_, sb_=sb_: e.tensor_tensor(out=tb1[:, :, 0:w_], in0=CT[:, :, 0:w_], in1=sb_, op=TT), [CT, sk], [tb1])
                    V(lambda e, w_=w_, cb_=cb_: e.tensor_tensor(out=tb2[:, :, 0:w_], in0=ST[:, :, 0:w_], in1=cb_, op=TT), [ST, ck], [tb2])
                    kb.op('dve', lambda e, w_=w_: e.tensor_tensor(out=ST[:, :, w_:2 * w_], in0=tb1[:, :, 0:w_], in1=tb2[:, :, 0:w_], op=ALU.add), R=[tb1, tb2], PW=[ST])
                    w_ *= 2
                    V(lambda e: e.tensor_tensor(out=tk[:, :], in0=ck[:, :], in1=ck[:, :], op=TT), [ck], [tk])
                    V(lambda e: e.tensor_tensor(out=tk2[:, :], in0=sk[:, :], in1=sk[:, :], op=TT), [sk], [tk2])
                    V(lambda e: e.tensor_tensor(out=tk[:, :], in0=tk[:, :], in1=tk2[:, :], op=ALU.subtract), [tk, tk2], [tk])
                    V(lambda e: e.tensor_tensor(out=tk2[:, :], in0=ck[:, :], in1=sk[:, :], op=TT), [ck, sk], [tk2])
                    V(lambda e: e.tensor_scalar(out=sk[:, :], in0=tk2[:, :], scalar1=2.0, scalar2=None, op0=TT), [tk2], [sk])
                    V(lambda e: e.tensor_copy(out=ck[:, :], in_=tk[:, :]), [tk], [ck])
            nsk = kb.buf(es, "s5_nsk", [128, 8], F32)
            V(lambda e: e.tensor_scalar(out=nsk[:, :], in0=sk[:, :], scalar1=-1.0, scalar2=None, op0=TT), [sk], [nsk])
            dsk = kb.buf(es, "s5_dsk", [128, 2], F32)
            glb = kb.buf(es, "s5_glb", [128, 2], F32)
            glw = kb.buf(es, "s5_glw", [128, 2, 256], BF16)
            kb.dma(dsk[:, :], self.w["s5_d"][l].rearrange("(c p) -> p c", p=128), dsk, W=[dsk], allow_slow_non_contiguous=True)
            kb.dma(glb[:, :], self.w["s5_glu_b"][l].rearrange("(c p) -> p c", p=128), glb, W=[glb], allow_slow_non_contiguous=True)
            kb.dma(glw[:, :, :], self.wb["s5_glu_w"][l].rearrange("(c p) o -> p c o", p=128), glw, W=[glw])
            uf = [kb.buf(es, "s5_uf%d" % i, [128, 2, 512], F32) for i in range(2)]
            ub = [kb.buf(es, "s5_ub%d" % i, [128, 2, 512], BF16) for i in range(2)]
            ini = kb.buf(es, "s5_ini", [128, 8, 2], F32)
            kb.op('pool', lambda e: e.memset(ini[:, :, :], 0.0), W=[ini])
            vr2 = [kb.buf(es, "s5_vr%d" % i, [128, 512], F32) for i in range(2)]
            vi2 = [kb.buf(es, "s5_vi%d" % i, [128, 512], F32) for i in range(2)]
            ta2 = [kb.buf(es, "s5_ta%d" % i, [128, 512], F32) for i in range(2)]
            tb2_ = [kb.buf(es, "s5_tb%d" % i, [128, 512], F32) for i in range(2)]
            tc2 = [kb.buf(es, "s5_tc%d" % i, [128, 512], F32) for i in range(2)]
            td2 = [kb.buf(es, "s5_td%d" % i, [128, 512], F32) for i in range(2)]
            gr2 = [kb.buf(es, "s5_gr%d" % i, [128, 512], F32) for i in range(2)]
            gi2 = [kb.buf(es, "s5_gi%d" % i, [128, 512], F32) for i in range(2)]
            hr = [kb.buf(es, "s5_hr%d" % i, [128, 512], BF16) for i in range(2)]
            hi = [kb.buf(es, "s5_hi%d" % i, [128, 512], BF16) for i in range(2)]
            yb = kb.buf(es, "s5_y", [128, 512], F32)
            tm = kb.buf(es, "s5_tm", [128, 512], F32)
            zf = kb.buf(es, "s5_zf", [128, 2, 512], F32)
            zb = kb.buf(es, "s5_zb", [128, 2, 512], BF16)
            sg = kb.buf(es, "s5_sg", [128, 512], F32)
            ob = [kb.buf(es, "s5_ob%d" % i, [128, 512], BF16) for i in range(2)]
            tcol = kb.buf(es, "s5_tcol", [128, 2], F32)
            n = 0
            for i in range(NT):
                t0 = i * 512
                uf_ = uf[i % 2]
                ub_ = ub[i % 2]
                kb.dma(uf_[:, :, :], self.ZS[s, :, t0:t0 + 512].rearrange("(c p) t -> p c t", p=128), uf_, W=[uf_])
                kb.op('act', lambda e: e.copy(out=ub_[:, :, :], in_=uf_[:, :, :]), R=[uf_], W=[ub_])
                for cc in range(2):
                    yps = pss[4 + cc]
                    for mm in range(4):
                        m = cc * 4 + mm
                        pr = pss[(2 * n) % 4]
                        pi = pss[(2 * n + 1) % 4]
                        hr_ = hr[n % 2]
                        hi_ = hi[n % 2]
                        vr, vi, ta, tbb, gr, gi = vr2[n % 2], vi2[n % 2], ta2[n % 2], tb2_[n % 2], gr2[n % 2], gi2[n % 2]
                        tc, td = tc2[n % 2], td2[n % 2]
                        n += 1
                        kb.op('pe', lambda e, m=m, pr=pr, cc=cc: e.matmul(pr[:, :], lhsT=wB[0][:, m, :], rhs=ub_[:, cc, :], start=True, stop=True), R=[wB[0], ub_], W=[pr])
                        kb.op('pe', lambda e, m=m, pi=pi, cc=cc: e.matmul(pi[:, :], lhsT=wB[1][:, m, :], rhs=ub_[:, cc, :], start=True, stop=True), R=[wB[1], ub_], W=[pi])
                        V(lambda e, m=m, pr=pr: e.tensor_tensor(out=ta[:, :], in0=pr[:, :], in1=CT[:, m, :], op=TT), [pr, CT], [ta])
                        V(lambda e, m=m, pi=pi: e.tensor_tensor(out=tbb[:, :], in0=pi[:, :], in1=ST[:, m, :], op=TT), [pi, ST], [tbb])
                        kb.op('pool', lambda e: e.tensor_tensor(out=vr[:, :], in0=ta[:, :], in1=tbb[:, :], op=ALU.add), R=[ta, tbb], W=[vr])
                        V(lambda e, m=m, pi=pi: e.tensor_tensor(out=ta[:, :], in0=pi[:, :], in1=CT[:, m, :], op=TT), [pi, CT], [ta])
                        V(lambda e, m=m, pr=pr: e.tensor_tensor(out=tbb[:, :], in0=pr[:, :], in1=ST[:, m, :], op=TT), [pr, ST], [tbb])
                        kb.op('pool', lambda e: e.tensor_tensor(out=vi[:, :], in0=ta[:, :], in1=tbb[:, :], op=ALU.subtract), R=[ta, tbb], W=[vi])
                        V(lambda e, m=m: e.tensor_tensor_scan(out=gr[:, :], data0=RH[:, m, :], data1=vr[:, :], initial=ini[:, m, 0:1], op0=ALU.mult, op1=ALU.add), [RH, vr, ini], [gr])
                        V(lambda e, m=m: e.tensor_tensor_scan(out=gi[:, :], data0=RH[:, m, :], data1=vi[:, :], initial=ini[:, m, 1:2], op0=ALU.mult, op1=ALU.add), [RH, vi, ini], [gi])
                        V(lambda e, m=m: e.tensor_scalar(out=tcol[:, 0:1], in0=gr[:, 511:512], scalar1=ck[:, m:m + 1], scalar2=None, op0=TT), [gr, ck], [tcol])
                        kb.op('dve', lambda e, m=m: e.scalar_tensor_tensor(out=ini[:, m, 0:1], in0=gi[:, 511:512], scalar=nsk[:, m:m + 1], in1=tcol[:, 0:1], op0=TT, op1=ALU.add), R=[gi, nsk, tcol], PW=[ini])
                        V(lambda e, m=m: e.tensor_scalar(out=tcol[:, 1:2], in0=gi[:, 511:512], scalar1=ck[:, m:m + 1], scalar2=None, op0=TT), [gi, ck], [tcol])
                        kb.op('dve', lambda e, m=m: e.scalar_tensor_tensor(out=ini[:, m, 1:2], in0=gr[:, 511:512], scalar=sk[:, m:m + 1], in1=tcol[:, 1:2], op0=TT, op1=ALU.add), R=[gr, sk, tcol], PW=[ini])
                        kb.op('pool', lambda e, m=m, gr=gr, tc=tc: e.tensor_tensor(out=tc[:, :], in0=gr[:, :], in1=CT[:, m, :], op=TT), R=[gr, CT], W=[tc])
                        kb.op('pool', lambda e, m=m, gi=gi, td=td: e.tensor_tensor(out=td[:, :], in0=gi[:, :], in1=ST[:, m, :], op=TT), R=[gi, ST], W=[td])
                        V(lambda e, hr_=hr_, tc=tc, td=td: e.tensor_tensor(out=hr_[:, :], in0=tc[:, :], in1=td[:, :], op=ALU.subtract), [tc, td], [hr_])
                        kb.op('pool', lambda e, m=m, gr=gr, tc=tc: e.tensor_tensor(out=tc[:, :], in0=gr[:, :], in1=ST[:, m, :], op=TT), R=[gr, ST], W=[tc])
                        kb.op('pool', lambda e, m=m, gi=gi, td=td: e.tensor_tensor(out=td[:, :], in0=gi[:, :], in1=CT[:, m, :], op=TT), R=[gi, CT], W=[td])
                        V(lambda e, hi_=hi_, tc=tc, td=td: e.tensor_tensor(out=hi_[:, :], in0=tc[:, :], in1=td[:, :], op=ALU.add), [tc, td], [hi_])
                        kb.op('pe', lambda e, m=m, hr_=hr_, mm=mm, yps=yps: e.matmul(yps[:, :], lhsT=wC[0][:, m, :], rhs=hr_[:, :], start=(mm == 0), stop=False), R=[wC[0], hr_], PW=[yps])
                        kb.op('pe', lambda e, m=m, hi_=hi_, mm=mm, yps=yps: e.matmul(yps[:, :], lhsT=wC[1][:, m, :], rhs=hi_[:, :], start=False, stop=(mm == 3)), R=[wC[1], hi_], PW=[yps])
                    kb.op('dve', lambda e, cc=cc, yps=yps: e.scalar_tensor_tensor(out=yb[:, :], in0=uf_[:, cc, :], scalar=dsk[:, cc:cc + 1], in1=yps[:, :], op0=TT, op1=ALU.add), R=[uf_, dsk, yps], W=[yb])
                    gelu_tanh(kb, yb, tm, zf[:, cc, :], zf, 512)
                    kb.op('act', lambda e, cc=cc: e.copy(out=zb[:, cc, :], in_=zf[:, cc, :]), R=[zf], PW=[zb])
                for oc in range(2):
                    ps = pss[6 + oc]
                    for kc in range(2):
                        kb.op('pe', lambda e, oc=oc, kc=kc, ps=ps: e.matmul(ps[:, :], lhsT=glw[:, kc, oc * 128:(oc + 1) * 128], rhs=zb[:, kc, :], start=(kc == 0), stop=(kc == 1)), R=[glw, zb], PW=[ps])
                    kb.op('act', lambda e, oc=oc, ps=ps: e.activation(out=sg[:, :], in_=ps[:, :], func=AF.Sigmoid, bias=glb[:, oc:oc + 1], scale=1.0), R=[ps, glb], W=[sg])
                    o_ = ob[oc]
                    kb.op('dve', lambda e, oc=oc, o_=o_: e.tensor_tensor(out=o_[:, :], in0=zf[:, oc, :], in1=sg[:, :], op=TT), R=[zf, sg], W=[o_])
                    kb.dma(self.MIXT[s, 768 + oc * 128:768 + (oc + 1) * 128, t0:t0 + 512], o_[:, :], o_, R=[o_])
            kb.barrier()

    def load_ln(self, es, l, j, tag):
        kb = self.kb
        g = kb.buf(es, "lng" + tag, [128, D], F32)
        b = kb.buf(es, "lnb" + tag, [128, D], F32)
        kb.dma(g[:, :], self.w["ln_g"][l, j:j + 1, :].partition_broadcast(128), g, W=[g])
        kb.dma(b[:, :], self.w["ln_b"][l, j:j + 1, :].partition_broadcast(128), b, W=[b])
        return g, b

    def phase_outproj(self, l, src):
        kb = self.kb
        with ExitStack() as es:
            wo = kb.buf(es, "op_w", [128, 8, D], BF16)
            kb.dma(wo[:, :, :], self.wb["w_out"][l].rearrange("(c p) o -> p c o", p=128), wo, W=[wo])
            g, b = self.load_ln(es, l, 0, "op")
            scr = self.ln_scratch(es, "op")
            mx = [kb.buf(es, "op_mx%d" % i, [128, 8, 512], BF16) for i in range(2)]
            xt = [kb.buf(es, "op_x%d" % i, [128, 4, D], F32) for i in range(2)]
            ot = [kb.buf(es, "op_o%d" % i, [128, 4, D], F32) for i in range(2)]
            tt = [kb.buf(es, "op_t%d" % i, [128, D], F32) for i in range(2)]
            pss = [kb.buf(es, "op_ps%d" % i, [128, 512], F32, psum=True) for i in range(4)]
            n = 0
            for s in range(self.n_seq):
                for i in range(NT):
                    t0 = i * 512
                    mx_, x_, o_ = mx[n % 2], xt[n % 2], ot[n % 2]
                    kb.dma(mx_[:, :, :], self.MIXT[s, :, t0:t0 + 512].rearrange("(c p) t -> p c t", p=128), mx_, W=[mx_])
                    kb.dma(x_[:, :, :], src[s, t0:t0 + 512, :].rearrange("(a p) d -> p a d", p=128), x_, W=[x_])
                    for sub in range(4):
                        tt_ = tt[sub % 2]
                        for half in range(2):
                            ps = pss[(sub * 2 + half) % 4]
                            for kc in range(8):
                                kb.op('pe', lambda e, kc=kc, sub=sub, half=half, ps=ps: e.matmul(ps[:, :], lhsT=mx_[:, kc, sub * 128:(sub + 1) * 128], rhs=wo[:, kc, half * 512:(half + 1) * 512],
                                                                                         start=(kc == 0), stop=(kc == 7)), R=[mx_, wo], PW=[ps])
                            kb.op('dve', lambda e, sub=sub, half=half, ps=ps, tt_=tt_: e.scalar_tensor_tensor(out=tt_[:, half * 512:(half + 1) * 512], in0=x_[:, sub, half * 512:(half + 1) * 512], scalar=ALPHA,
                                                                                                   in1=ps[:, :], op0=ALU.mult, op1=ALU.add), R=[x_, ps], PW=[tt_])
                        eng, fn, rd = layer_norm_tm(kb, tt_, g, b, o_[:, sub, :], scr)
                        kb.op(eng, fn, R=rd, PW=[o_])
                    kb.dma(self.XR[s, t0:t0 + 512, :].rearrange("(a p) d -> p a d", p=128), o_[:, :, :], o_, R=[o_])
                    n += 1
            kb.barrier()

    def phase_ffn(self, l, moe):
        kb = self.kb
        C = self.C
        j = l // 2
        if moe:
            FF, GS, experts = D_FFE, 4, NE
            W1, W3, W2 = self.wb["moe_w1"][j], self.wb["moe_w3"][j], self.wb["moe_w2"][j]
        else:
            FF, GS, experts = D_FF, 2, 1
            W1, W3, W2 = self.wb["ffn_w1"][j:j + 1], self.wb["ffn_w3"][j:j + 1], self.wb["ffn_w2"][j:j + 1]
        NFC = FF // 128
        NG = NFC // GS
        GW = GS * 128
        with ExitStack() as es:
            g, b = self.load_ln(es, l, 1, "ff")
            scr = self.ln_scratch(es, "ff")
            xt = [kb.buf(es, "ff_x%d" % i, [128, 4, D], F32) for i in range(1 if moe else 2)]
            ot = kb.buf(es, "ff_o", [128, 4, D], F32)
            xT = kb.buf(es, "ff_xT", [128, 8, 512], BF16)
            w1 = [kb.buf(es, "ff_w1%d" % i, [128, 8, GW], BF16) for i in range(2)]
            w3 = [kb.buf(es, "ff_w3%d" % i, [128, 8, GW], BF16) for i in range(2)]
            w2 = [kb.buf(es, "ff_w2%d" % i, [128, NFC, 128], BF16) for i in range(2)]
            gT = kb.buf(es, "ff_g", [128, NFC, 512], BF16)
            sl = [kb.buf(es, "ff_s%d" % i, [128, 512], F32) for i in range(2)]
            facc = kb.buf(es, "ff_acc", [128, 8, 512], F32)
            tt = [kb.buf(es, "ff_t%d" % i, [128, D], F32) for i in range(2)]
            ps_h1 = [kb.buf(es, "ff_ph1%d" % i, [128, 512], F32, psum=True) for i in range(2)]
            ps_h3 = [kb.buf(es, "ff_ph3%d" % i, [128, 512], F32, psum=True) for i in range(2)]
            ps_o = [kb.buf(es, "ff_po%d" % i, [128, 512], F32, psum=True) for i in range(2)]
            ps_t = [kb.buf(es, "ff_pt%d" % i, [128, 512], F32, psum=True) for i in range(2)]
            if moe:
                xTf = kb.buf(es, "ff_xTf", [128, 8, 512], F32)
                rt = kb.buf(es, "ff_rt", [128, 8, 128], F32)
                kb.op('pool', lambda e: e.memset(rt[:, :, :], 0.0), W=[rt])
                kb.dma(rt[:, :, 0:NE], self.w["moe_router"][j].rearrange("(c p) e -> p c e", p=128), rt, PW=[rt])
                lg = kb.buf(es, "ff_lg", [128, 4, 8], F32)
                t8 = kb.buf(es, "ff_t8", [128, 4, 8], F32)
                wv = kb.buf(es, "ff_wv", [128, 4, 2], F32)
                cmb = kb.buf(es, "ff_cmb", [128, 4, 8], F32)
                cm2 = kb.buf(es, "ff_cm2", [128, 8], F32)
                cmB = kb.buf(es, "ff_cmB", [128, 8, 512], BF16)
                cexp = [kb.buf(es, "ff_cexp%d" % i, [128, 128], F32) for i in range(2)]
            cnt = [0]
            nw = [0]
            n2 = [0]
            n = 0
            for s in range(self.n_seq):
                for i in range(NT):
                    t0 = i * 512
                    x_ = xt[n % len(xt)]
                    n += 1
                    kb.dma(x_[:, :, :], self.XR[s, t0:t0 + 512, :].rearrange("(a p) d -> p a d", p=128), x_, W=[x_])
                    if moe:
                        transpose_in(kb, x_, 4, 8, xTf, C['ident'], ps_t, cnt, dst2=xT)
                    else:
                        transpose_in(kb, x_, 4, 8, xT, C['ident'], ps_t, cnt)
                    if moe and os.environ.get('MOE_SKIP_ROUTE'):
                        kb.op('pool', lambda e: e.memset(cmB[:, :, :], 0.5), W=[cmB])
                    elif moe:
                        for sub in range(4):
                            ps = ps_o[sub % 2]
                            for kc in range(8):
                                kb.op('pe', lambda e, kc=kc, sub=sub, ps=ps: e.matmul(ps[:, 0:128], lhsT=xTf[:, kc, sub * 128:(sub + 1) * 128], rhs=rt[:, kc, :], start=(kc == 0), stop=(kc == 7)), R=[xTf, rt], PW=[ps])
                            kb.op('dve', lambda e, sub=sub, ps=ps: e.tensor_copy(out=lg[:, sub, :], in_=ps[:, 0:8]), R=[ps], PW=[lg])
                            kb.op('dve', lambda e, sub=sub: e.max(out=t8[:, sub, :], in_=lg[:, sub, :]), R=[lg], PW=[t8])
                            kb.op('dve', lambda e, sub=sub: e.tensor_tensor(out=wv[:, sub, 0:1], in0=t8[:, sub, 0:1], in1=t8[:, sub, 1:2], op=ALU.subtract), R=[t8], PW=[wv])
                            kb.op('act', lambda e, sub=sub: e.activation(out=wv[:, sub, 0:1], in_=wv[:, sub, 0:1], func=AF.Sigmoid), R=[wv], PW=[wv])
                            kb.op('dve', lambda e, sub=sub: e.tensor_scalar(out=wv[:, sub, 1:2], in0=wv[:, sub, 0:1], scalar1=-1.0, scalar2=1.0, op0=ALU.mult, op1=ALU.add), R=[wv], PW=[wv])
                            kb.op('dve', lambda e, sub=sub: e.tensor_scalar(out=cmb[:, sub, :], in0=lg[:, sub, :], scalar1=t8[:, sub, 0:1], scalar2=wv[:, sub, 0:1], op0=ALU.is_equal, op1=ALU.mult), R=[lg, t8, wv], PW=[cmb])
                            kb.op('dve', lambda e, sub=sub: e.tensor_scalar(out=cm2[:, :], in0=lg[:, sub, :], scalar1=t8[:, sub, 1:2], scalar2=wv[:, sub, 1:2], op0=ALU.is_equal, op1=ALU.mult), R=[lg, t8, wv], W=[cm2])
                            kb.op('dve', lambda e, sub=sub: e.tensor_tensor(out=cmb[:, sub, :], in0=cmb[:, sub, :], in1=cm2[:, :], op=ALU.add), R=[cmb, cm2], PW=[cmb])
                        for ex in range(NE):
                            ps = ps_o[ex % 2]
                            for sub in range(4):
                                ce = cexp[(ex * 4 + sub) % 2]
                                kb.op('dve', lambda e, ex=ex, sub=sub, ce=ce: e.tensor_copy(out=ce[:, :], in_=cmb[:, sub, ex:ex + 1].to_broadcast([128, 128])), R=[cmb], W=[ce])
                                kb.op('pe', lambda e, sub=sub, ps=ps, ce=ce: e.matmul(ps[:, sub * 128:(sub + 1) * 128], lhsT=ce[:, :], rhs=C['ident'][:, :], start=True, stop=True), R=[ce, C['ident']], PW=[ps])
                            kb.op('act', lambda e, ex=ex, ps=ps: e.copy(out=cmB[:, ex, :], in_=ps[:, :]), R=[ps], PW=[cmB])
                    for ex in range(int(os.environ.get('MOE_NE', experts)) if moe else experts):
                        for gi_ in range(0 if (moe and os.environ.get('MOE_MODE') == 's2') else NG):
                            w1_, w3_ = w1[nw[0] % 2], w3[nw[0] % 2]
                            nw[0] += 1
                            kb.dma(w1_[:, :, :], W1[ex, :, gi_ * GW:(gi_ + 1) * GW].rearrange("(c p) f -> p c f", p=128), w1_, W=[w1_])
                            kb.dma(w3_[:, :, :], W3[ex, :, gi_ * GW:(gi_ + 1) * GW].rearrange("(c p) f -> p c f", p=128), w3_, W=[w3_])
                            for fi in range(GS):
                                fc = gi_ * GS + fi
                                p1, p3 = ps_h1[fc % 2], ps_h3[fc % 2]
                                for kc in range(8):
                                    kb.op('pe', lambda e, kc=kc, fi=fi, p1=p1, w1_=w1_: e.matmul(p1[:, :], lhsT=w1_[:, kc, fi * 128:(fi + 1) * 128], rhs=xT[:, kc, :], start=(kc == 0), stop=(kc == 7)), R=[w1_, xT], PW=[p1])
                                for kc in range(8):
                                    kb.op('pe', lambda e, kc=kc, fi=fi, p3=p3, w3_=w3_: e.matmul(p3[:, :], lhsT=w3_[:, kc, fi * 128:(fi + 1) * 128], rhs=xT[:, kc, :], start=(kc == 0), stop=(kc == 7)), R=[w3_, xT], PW=[p3])
                                s_ = sl[fc % 2]
                                kb.op('act', lambda e, p1=p1, s_=s_: e.activation(out=s_[:, :], in_=p1[:, :], func=AF.Silu), R=[p1], W=[s_])
                                if moe:
                                    kb.op('dve', lambda e, p3=p3, s_=s_: e.tensor_tensor(out=s_[:, :], in0=s_[:, :], in1=p3[:, :], op=ALU.mult), R=[s_, p3], W=[s_])
                                    kb.op('dve', lambda e, fc=fc, s_=s_, ex=ex: e.tensor_tensor(out=gT[:, fc, :], in0=s_[:, :], in1=cmB[:, ex, :], op=ALU.mult), R=[s_, cmB], PW=[gT])
                                else:
                                    kb.op('dve', lambda e, fc=fc, p3=p3, s_=s_: e.tensor_tensor(out=gT[:, fc, :], in0=s_[:, :], in1=p3[:, :], op=ALU.mult), R=[s_, p3], PW=[gT])
                        for dc in range(0 if (moe and os.environ.get('MOE_MODE') == 's1') else 8):
                            w2_ = w2[n2[0] % 2]
                            n2[0] += 1
                            kb.dma(w2_[:, :, :], W2[ex, :, dc * 128:(dc + 1) * 128].rearrange("(c p) o -> p c o", p=128), w2_, W=[w2_])
                            po = ps_o[dc % 2]
                            for fc in range(NFC):
                                kb.op('pe', lambda e, fc=fc, po=po, w2_=w2_: e.matmul(po[:, :], lhsT=w2_[:, fc, :], rhs=gT[:, fc, :], start=(fc == 0), stop=(fc == NFC - 1)), R=[w2_, gT], PW=[po])
                            if ex == 0:
                                kb.op('act', lambda e, dc=dc, po=po: e.copy(out=facc[:, dc, :], in_=po[:, :]), R=[po], PW=[facc])
                            else:
                                kb.op('dve', lambda e, dc=dc, po=po: e.tensor_tensor(out=facc[:, dc, :], in0=facc[:, dc, :], in1=po[:, :], op=ALU.add), R=[facc, po], PW=[facc])
                    for sub in range(4):
                        tt_ = tt[sub % 2]
                        for half in range(2):
                            pst = ps_t[cnt[0] % 2]
                            cnt[0] += 1
                            for q in range(4):
                                dc = half * 4 + q
                                kb.op('pe', lambda e, dc=dc, q=q, sub=sub, pst=pst: e.transpose(pst[:, q * 128:(q + 1) * 128], facc[:, dc, sub * 128:(sub + 1) * 128], C['ident'][:, :]), R=[facc, C['ident']], PW=[pst])
                            kb.op('dve', lambda e, sub=sub, half=half, pst=pst, tt_=tt_: e.scalar_tensor_tensor(out=tt_[:, half * 512:(half + 1) * 512], in0=x_[:, sub, half * 512:(half + 1) * 512], scalar=ALPHA,
                                                                                                    in1=pst[:, :], op0=ALU.mult, op1=ALU.add), R=[x_, pst], PW=[tt_])
                        eng, fn, rd = layer_norm_tm(kb, tt_, g, b, ot[:, sub, :], scr)
                        kb.op(eng, fn, R=rd, PW=[ot])
                    kb.dma(self.XR[s, t0:t0 + 512, :].rearrange("(a p) d -> p a d", p=128), ot[:, :, :], ot, R=[ot])
            kb.barrier()

    def phase_ple(self, l, dst):
        kb = self.kb
        C = self.C
        with ExitStack() as es:
            wg = kb.buf(es, "pl_wg", [128, 8, D], BF16)
            wp = kb.buf(es, "pl_wp", [128, 2, D], BF16)
            bg = kb.buf(es, "pl_bg", [128, D], F32)
            kb.dma(wg[:, :, :], self.wb["ple_gate_w"][l].rearrange("(c p) o -> p c o", p=128), wg, W=[wg])
            kb.dma(wp[:, :, :], self.wb["ple_proj"][l].rearrange("(c p) o -> p c o", p=128), wp, W=[wp])
            kb.dma(bg[:, :], self.w["ple_gate_b"][l:l + 1, :].partition_broadcast(128), bg, W=[bg])
            g, b = self.load_ln(es, l, 2, "pl")
            scr = self.ln_scratch(es, "pl")
            xt = [kb.buf(es, "pl_x%d" % i, [128, 4, D], F32) for i in range(2)]
            pt = [kb.buf(es, "pl_p%d" % i, [128, 4, 256], F32) for i in range(2)]
            ot = [kb.buf(es, "pl_o%d" % i, [128, 4, D], F32) for i in range(2)]
            xT = kb.buf(es, "pl_xT", [128, 8, 512], BF16)
            pT = kb.buf(es, "pl_pT", [128, 2, 512], BF16)
            tt = [kb.buf(es, "pl_t%d" % i, [128, D], F32) for i in range(2)]
            uu = [kb.buf(es, "pl_u%d" % i, [128, 512], F32) for i in range(2)]
            ps_t = [kb.buf(es, "pl_pt%d" % i, [128, 512], F32, psum=True) for i in range(2)]
            ps_a = [kb.buf(es, "pl_pa%d" % i, [128, 512], F32, psum=True) for i in range(2)]
            ps_b = [kb.buf(es, "pl_pb%d" % i, [128, 512], F32, psum=True) for i in range(2)]
            cnt = [0]
            n = 0
            for s in range(self.n_seq):
                for i in range(NT):
                    t0 = i * 512
                    x_, p_, o_ = xt[n % 2], pt[n % 2], ot[n % 2]
                    n += 1
                    kb.dma(x_[:, :, :], self.XR[s, t0:t0 + 512, :].rearrange("(a p) d -> p a d", p=128), x_, W=[x_])
                    kb.dma(p_[:, :, :], self.p[l, s, t0:t0 + 512, :].rearrange("(a p) d -> p a d", p=128), p_, W=[p_])
                    transpose_in(kb, x_, 4, 8, xT, C['ident'], ps_t, cnt)
                    transpose_in(kb, p_, 4, 2, pT, C['ident'], ps_t, cnt)
                    for sub in range(4):
                        tt_ = tt[sub % 2]
                        for half in range(2):
                            pa, pb = ps_a[half], ps_b[half]
                            u_ = uu[half]
                            hs = slice(half * 512, (half + 1) * 512)
                            for kc in range(8):
                                kb.op('pe', lambda e, kc=kc, sub=sub, pa=pa, hs=hs: e.matmul(pa[:, :], lhsT=xT[:, kc, sub * 128:(sub + 1) * 128], rhs=wg[:, kc, hs], start=(kc == 0), stop=(kc == 7)), R=[xT, wg], PW=[pa])
                            for kc in range(2):
                                kb.op('pe', lambda e, kc=kc, sub=sub, pb=pb, hs=hs: e.matmul(pb[:, :], lhsT=pT[:, kc, sub * 128:(sub + 1) * 128], rhs=wp[:, kc, hs], start=(kc == 0), stop=(kc == 1)), R=[pT, wp], PW=[pb])
                            kb.op('dve', lambda e, pa=pa, u_=u_, hs=hs: e.tensor_tensor(out=u_[:, :], in0=pa[:, :], in1=bg[:, hs], op=ALU.add), R=[pa, bg], W=[u_])
                            kb.op('act', lambda e, u_=u_: e.activation(out=u_[:, :], in_=u_[:, :], func=AF.Sigmoid), R=[u_], W=[u_])
                            kb.op('dve', lambda e, pb=pb, u_=u_: e.tensor_tensor(out=u_[:, :], in0=u_[:, :], in1=pb[:, :], op=ALU.mult), R=[u_, pb], W=[u_])
                            kb.op('dve', lambda e, sub=sub, u_=u_, hs=hs, tt_=tt_: e.scalar_tensor_tensor(out=tt_[:, hs], in0=x_[:, sub, hs], scalar=ALPHA, in1=u_[:, :], op0=ALU.mult, op1=ALU.add), R=[x_, u_], PW=[tt_])
                        eng, fn, rd = layer_norm_tm(kb, tt_, g, b, o_[:, sub, :], scr)
                        kb.op(eng, fn, R=rd, PW=[o_])
                    kb.dma(dst[s, t0:t0 + 512, :].rearrange("(a p) d -> p a d", p=128), o_[:, :, :], o_, R=[o_])
            kb.barrier()


def win_perm():
    q = lambda h, half: [h * 64 + half * 32 + d for d in range(32)]
    kv = lambda br, h, half: [512 + (br * 2 + h) * 64 + half * 32 + d for d in range(32)]
    vfull = lambda br, h: [512 + (br * 2 + h) * 64 + d for d in range(64)]
    cols = []
    for hs in (range(0, 4), range(4, 8)):
        for half in range(2):
            for h in hs:
                cols += q(h, half)
    for half in range(2):
        cols += kv(0, 0, half) + kv(0, 1, half) + kv(2, 0, half) + kv(2, 1, half)
    for half in range(2):
        cols += kv(4, 0, half) + kv(4, 1, half)
    cols += vfull(1, 0) + vfull(1, 1)
    cols += list(range(512 + 768 + 24, 512 + 768 + 24 + 512))
    cols += list(range(512 + 768 + 24 + 512, 2072))
    cols += vfull(3, 0) + vfull(3, 1) + vfull(5, 0) + vfull(5, 1)
    cols += list(range(512 + 768, 512 + 768 + 24))
    assert len(cols) == INW and len(set(cols)) == INW
    return np.array(cols)


def make_consts():
    ident = np.eye(128, dtype=np.float32)
    inv_freq = (np.float32(10000.0) ** (-np.arange(0, 64, 2, dtype=np.float32) / np.float32(64))).astype(np.float32)
    invf = np.tile(inv_freq, 4).reshape(128, 1).astype(np.float32)
    j = np.arange(L)
    emat = (j[None, :] // 64 == np.arange(64)[:, None]).astype(np.float32).astype(ml_dtypes.bfloat16)
    n_cmp = 255
    c_start = np.arange(n_cmp)[:, None] * 16
    s_start = np.arange(64)[None, :] * 64
    ovl = np.zeros((256, 64), np.float32)
    ovl[:n_cmp] = ((c_start < s_start + 64) & (c_start + 32 > s_start)).astype(np.float32)
    return {"c_ident": ident, "c_invf": invf, "c_emat": emat, "c_ovl": ovl.astype(ml_dtypes.bfloat16)}


_PROG = {}


def kernel(**inputs):
    n_cores = 8
    if 'full' not in _PROG:
        _PROG['full'] = Prog()
    prog = _PROG['full']
    consts = make_consts()
    perm = win_perm()
    shared = {k: np.ascontiguousarray(np.asarray(v)) for k, v in inputs.items() if k not in ("x", "p", "positions")}
    shared["w_in"] = np.ascontiguousarray(shared["w_in"][:, :, perm])
    shared.update(consts)
    x = np.asarray(inputs["x"])
    p = np.asarray(inputs["p"])
    pos = np.asarray(inputs["positions"]).astype(np.int32)
    in_maps = []
    for c in range(n_cores):
        m = dict(shared)
        m["x"] = np.ascontiguousarray(x[2 * c:2 * c + 2])
        m["p"] = np.ascontiguousarray(p[:, 2 * c:2 * c + 2])
        m["positions"] = np.ascontiguousarray(pos[2 * c:2 * c + 2])
        in_maps.append(m)
    res = run_bass_kernel_spmd(prog.nc, in_maps, core_ids=list(range(n_cores)))
    out = np.concatenate([np.asarray(r["y"]) for r in res.results], axis=0)
    return out.astype(np.float32)
```

```python
import math
import os
from contextlib import ExitStack
import numpy as np
import ml_dtypes
import concourse.bass as bass
import concourse.mybir as mybir
from concourse.bass_utils import run_bass_kernel_spmd

F32 = mybir.dt.float32
BF16 = mybir.dt.bfloat16
I32 = mybir.dt.int32
AF = mybir.ActivationFunctionType
ALU = mybir.AluOpType

D = 1024
L = 4096
DEPTH = 4
NT = L // 512
ALPHA = (2 * DEPTH) ** 0.25
LN_EPS = 1e-5
INW = 2072
D_FF = 2816
D_FFE = 3584
NE = 8
BIG = 30000.0
TWO_PI = 2.0 * math.pi
NDSEM = 80


class Buf:
    def __init__(self, t):
        self.t = t
        self.w = {}
        self.wf = {}
        self.r = {}
        self.prev = {}
        self.dsem = None

    def __getitem__(self, idx):
        return self.t[idx]


def _merge(d, src):
    for s, v in src.items():
        if d.get(s, 0) < v:
            d[s] = v


class KB:
    def __init__(self, nc, es):
        self.nc = nc
        self.E = {'pe': nc.tensor, 'act': nc.scalar, 'dve': nc.vector, 'pool': nc.gpsimd, 'sp': nc.sync}
        self.sem = {}
        self.tot = {}
        for e in self.E:
            self._mksem('E_' + e, es)
        self.dfree = []
        for i in range(NDSEM):
            self._mksem('D%d' % i, es)
            self.dfree.append('D%d' % i)
        self.waited = {e: {} for e in self.E}
        ss = os.environ.get('SYNC_SAME', '111')
        self.sync_same = {'pe': False, 'act': ss[0] == '1', 'dve': ss[1] == '1', 'pool': ss[2] == '1', 'sp': False}
        self.phase_bufs = []
        self.rr = 0
        self.log = None

    def _mksem(self, name, es):
        self.sem[name] = es.enter_context(self.nc.semaphore(name))
        self.tot[name] = 0

    def buf(self, es, name, shape, dt, psum=False):
        self.nbuf = getattr(self, 'nbuf', 0) + 1
        name = "%s_%d" % (name, self.nbuf)
        if psum:
            t = es.enter_context(self.nc.psum_tensor(name, shape, dt))
        else:
            t = es.enter_context(self.nc.sbuf_tensor(name, shape, dt))
        b = Buf(t)
        self.phase_bufs.append(b)
        return b

    def _wait(self, eng, deps):
        own = 'E_' + eng
        for s, v in deps.items():
            if s == own and not self.sync_same[eng]:
                continue
            if self.waited[eng].get(s, 0) >= v:
                continue
            self.E[eng].wait_ge(self.sem[s], v)
            self.waited[eng][s] = v
            if self.log is not None:
                self.log.append((eng, 'w', s, v))

    def _deps(self, R, W, PW):
        deps = {}
        for b in R:
            _merge(deps, b.w)
        for b in W:
            _merge(deps, b.w)
            _merge(deps, b.r)
            _merge(deps, b.prev)
        for b in PW:
            if b.r:
                p = {}
                _merge(p, b.r)
                _merge(p, b.w)
                b.prev = p
                b.w = {}
                b.wf = {}
                b.r = {}
            _merge(deps, b.prev)
            _merge(deps, b.wf)
        return deps

    def _post(self, tok, R, W, PW):
        for b in R:
            _merge(b.r, tok)
        for b in W:
            b.w = dict(tok)
            b.wf = dict(tok)
            b.r = {}
            b.prev = {}
        for b in PW:
            _merge(b.w, tok)

    def op(self, eng, fn, R=(), W=(), PW=()):
        deps = self._deps(R, W, PW)
        self._wait(eng, deps)
        ins = fn(self.E[eng])
        s = 'E_' + eng
        self.tot[s] += 1
        ins.then_inc(self.sem[s], 1)
        if self.log is not None:
            self.log.append((eng, 'i', s, 1))
        self._post({s: self.tot[s]}, R, W, PW)

    def dma(self, out, in_, sb, R=(), W=(), PW=(), q='sp', **kw):
        deps = self._deps(R, W, PW)
        self._wait(q, deps)
        if sb.dsem is None:
            sb.dsem = self.dfree.pop()
        ins = self.E[q].dma_start(out=out, in_=in_, **kw)
        s = sb.dsem
        self.tot[s] += 16
        ins.then_inc(self.sem[s], 16)
        if self.log is not None:
            self.log.append((q, 'i', s, 16))
        self._post({s: self.tot[s]}, R, W, PW)

    def barrier(self):
        if getattr(self, 'pre_barrier', None) is not None and not os.environ.get('NO_PREBAR'):
            self.pre_barrier()
        allt = dict(self.tot)
        for e in self.E:
            self._wait(e, allt)
        for b in self.phase_bufs:
            if b.dsem is not None:
                self.dfree.append(b.dsem)
                b.dsem = None
        self.phase_bufs = []

    def ew(self):
        self.rr += 1
        return ('dve', 'pool')[self.rr % 2]


def layer_norm_tm(kb, tt, g_b, b_b, out_ap, scr):
    st, mv, rs = scr['st'], scr['mv'], scr['rs']
    kb.op('dve', lambda e: e.bn_stats(out=st[:, 0:6], in_=tt[:, 0:512]), R=[tt], PW=[st])
    kb.op('dve', lambda e: e.bn_stats(out=st[:, 6:12], in_=tt[:, 512:1024]), R=[tt], PW=[st])
    kb.op('dve', lambda e: e.bn_aggr(out=mv[:, 0:2], in_=st[:, 0:12]), R=[st], W=[mv])
    kb.op('act', lambda e: e.activation(out=rs[:, 0:1], in_=mv[:, 1:2], func=AF.Sqrt, bias=scr['eps'][:, 0:1], scale=1.0), R=[mv, scr['eps']], W=[rs])
    kb.op('dve', lambda e: e.reciprocal(out=rs[:, 1:2], in_=rs[:, 0:1]), R=[rs], PW=[rs])
    kb.op('dve', lambda e: e.tensor_scalar(out=tt[:, :], in0=tt[:, :], scalar1=mv[:, 0:1], scalar2=rs[:, 1:2], op0=ALU.subtract, op1=ALU.mult), R=[tt, mv, rs], W=[tt])
    kb.op('pool', lambda e: e.tensor_tensor(out=tt[:, :], in0=tt[:, :], in1=g_b[:, :], op=ALU.mult), R=[tt, g_b], W=[tt])
    return ('pool', lambda e: e.tensor_tensor(out=out_ap, in0=tt[:, :], in1=b_b[:, :], op=ALU.add), [tt, b_b])


def gelu_tanh(kb, u, tmp, out_ap, out_buf, n):
    kb.op('dve', lambda e: e.tensor_tensor(out=tmp[:, 0:n], in0=u[:, 0:n], in1=u[:, 0:n], op=ALU.mult), R=[u], W=[tmp])
    kb.op('dve', lambda e: e.tensor_scalar(out=tmp[:, 0:n], in0=tmp[:, 0:n], scalar1=0.044715, scalar2=1.0, op0=ALU.mult, op1=ALU.add), R=[tmp], W=[tmp])
    kb.op('dve', lambda e: e.tensor_tensor(out=tmp[:, 0:n], in0=tmp[:, 0:n], in1=u[:, 0:n], op=ALU.mult), R=[tmp, u], W=[tmp])
    kb.op('act', lambda e: e.activation(out=tmp[:, 0:n], in_=tmp[:, 0:n], func=AF.Sigmoid, scale=1.5957691216057308), R=[tmp], W=[tmp])
    kb.op('dve', lambda e: e.tensor_tensor(out=out_ap, in0=tmp[:, 0:n], in1=u[:, 0:n], op=ALU.mult), R=[tmp, u], PW=[out_buf])


def transpose_in(kb, src, nsub, nfc, dst, ident, pss, cnt, dst2=None):
    for fc in range(nfc):
        ps = pss[cnt[0] % len(pss)]
        cnt[0] += 1
        for sub in range(nsub):
            kb.op('pe', lambda e, fc=fc, sub=sub, ps=ps: e.transpose(ps[:, sub * 128:(sub + 1) * 128], src[:, sub, fc * 128:(fc + 1) * 128], ident[:, :]),
                  R=[src, ident], PW=[ps])
        eng = ('act', 'dve')[fc % 2]
        if eng == 'act':
            kb.op('act', lambda e, fc=fc, ps=ps: e.copy(out=dst[:, fc, 0:nsub * 128], in_=ps[:, 0:nsub * 128]), R=[ps], PW=[dst])
        else:
            kb.op('dve', lambda e, fc=fc, ps=ps: e.tensor_copy(out=dst[:, fc, 0:nsub * 128], in_=ps[:, 0:nsub * 128]), R=[ps], PW=[dst])
        if dst2 is not None:
            kb.op('pool', lambda e, fc=fc: e.tensor_copy(out=dst2[:, fc, 0:nsub * 128], in_=dst[:, fc, 0:nsub * 128]), R=[dst], PW=[dst2])


class Prog:
    def __init__(self, n_layers=DEPTH, n_seq=2, dbg=(), only=None):
        self.only = only
        self.n_layers = n_layers
        self.n_seq = n_seq
        self.dbg = set(dbg)
        nc = bass.Bass("TRN2", target_bir_lowering=False)
        self.nc = nc
        S = n_seq

        def din(name, shape, dt=F32):
            return nc.dram_tensor(name, list(shape), dt, kind="ExternalInput").ap()

        def dscr(name, shape, dt):
            kind = "ExternalOutput" if name in self.dbg else "Internal"
            return nc.dram_tensor(name, list(shape), dt, kind=kind).ap()

        self.x = din("x", [S, L, D])
        self.p = din("p", [DEPTH, S, L, 256])
        self.pos = din("positions", [S, L], I32)
        self.w = {}
        for name, shape in [("w_in", [DEPTH, D, INW]), ("w_out", [DEPTH, D, D]), ("cmp_pe", [DEPTH, 2, 32, 64]),
                            ("cmp_w1", [DEPTH, 2, 2048, 256]), ("cmp_b1", [DEPTH, 2, 256]), ("cmp_w2", [DEPTH, 2, 256, 64]),
                            ("cmp_b2", [DEPTH, 2, 64]), ("conv_w", [DEPTH, 31, 256]), ("conv_b", [DEPTH, 256]),
                            ("conv_ln_g", [DEPTH, 256]), ("conv_ln_b", [DEPTH, 256]), ("s5_a_re", [DEPTH, 16, 64]),
                            ("s5_a_im", [DEPTH, 16, 64]), ("s5_log_dt", [DEPTH, 16]), ("s5_b_re", [DEPTH, 16, 64, 16]),
                            ("s5_b_im", [DEPTH, 16, 64, 16]), ("s5_c_re", [DEPTH, 16, 16, 64]), ("s5_c_im", [DEPTH, 16, 16, 64]),
                            ("s5_d", [DEPTH, 256]), ("s5_glu_w", [DEPTH, 256, 256]), ("s5_glu_b", [DEPTH, 256]),
                            ("ffn_w1", [2, D, D_FF]), ("ffn_w3", [2, D, D_FF]), ("ffn_w2", [2, D_FF, D]),
                            ("moe_router", [2, D, NE]), ("moe_w1", [2, NE, D, D_FFE]), ("moe_w3", [2, NE, D, D_FFE]),
                            ("moe_w2", [2, NE, D_FFE, D]), ("ple_gate_w", [DEPTH, D, D]), ("ple_gate_b", [DEPTH, D]),
                            ("ple_proj", [DEPTH, 256, D]), ("ln_g", [DEPTH, 3, D]), ("ln_b", [DEPTH, 3, D])]:
            self.w[name] = din(name, shape)
        self.c_ident = din("c_ident", [128, 128])
        self.c_invf = din("c_invf", [128, 1])
        self.c_emat = din("c_emat", [64, L], BF16)
        self.c_ovl = din("c_ovl", [256, 64], BF16)
        self.y = nc.dram_tensor("y", [S, L, D], F32, kind="ExternalOutput").ap()
        self.wb = {}
        for name in ["w_in", "w_out", "cmp_w1", "cmp_w2", "s5_glu_w", "ffn_w1", "ffn_w3", "ffn_w2", "moe_w1", "moe_w3", "moe_w2",
                     "ple_gate_w", "ple_proj"]:
            shp = list(self.w[name].shape)
            if name.startswith("ffn") or name.startswith("moe"):
                nl = (n_layers + 1) // 2 if name.startswith("ffn") else n_layers // 2
                shp[0] = max(nl, 1)
            else:
                shp[0] = n_layers
            self.wb[name] = dscr("b_" + name, shp, BF16)
        self.XR = dscr("XR", [S, L, D], F32)
        self.ROPE = dscr("ROPE", [S, 2, 128, L], F32)
        self.QT = dscr("QT", [S, 8, 64, L], BF16)
        self.KCT = dscr("KCT", [S, 2, 64, L], BF16)
        self.VCT = dscr("VCT", [S, 2, 64, L], BF16)
        self.KST = dscr("KST", [S, 2, 64, L], BF16)
        self.KWT = dscr("KWT", [S, 2, 64, L], BF16)
        self.VSW = dscr("VSW", [S, L, 256], BF16)
        self.G = dscr("G", [S, L, 24], F32)
        self.ZC = dscr("ZC", [S, 512, L], F32)
        self.ZS = dscr("ZS", [S, 256, L], F32)
        self.KCC = dscr("KCC", [S, 2, 64, 256], BF16)
        self.VCA = dscr("VCA", [S, 2, 256, 129], BF16)
        self.MIXT = dscr("MIXT", [S, D, L], BF16)

        with ExitStack() as es:
            self.kb = KB(nc, es)
            if 'LOG' in self.dbg:
                self.kb.log = []
            self.build()

    def consts(self, es):
        kb = self.kb
        c = {}
        c['ident'] = kb.buf(es, "ident", [128, 128], F32)
        kb.dma(c['ident'][:, :], self.c_ident[:, :], c['ident'], W=[c['ident']])
        c['eps'] = kb.buf(es, "epsb", [128, 1], F32)
        kb.op('pool', lambda e: e.memset(c['eps'][:, :], LN_EPS), W=[c['eps']])
        c['dum'] = kb.buf(es, "dumb", [128, 2], F32)
        kb.op('pool', lambda e: e.memset(c['dum'][:, :], 0.0), W=[c['dum']])
        return c

    def act_reset(self):
        d = self.C['dum']
        self.kb.op('act', lambda e: e.activation(out=d[:, 1:2], in_=d[:, 0:1], func=AF.Sigmoid), R=[d], PW=[d])

    def ln_scratch(self, es, tag):
        kb = self.kb
        return {'st': kb.buf(es, "lnst" + tag, [128, 12], F32), 'mv': kb.buf(es, "lnmv" + tag, [128, 2], F32),
                'rs': kb.buf(es, "lnrs" + tag, [128, 2], F32), 'eps': self.C['eps']}

    def build(self):
        kb = self.kb
        with ExitStack() as ces:
            self.C = self.consts(ces)
            kb.pre_barrier = self.act_reset
            on = lambda nm: (self.only is None) or (nm in self.only)
            if on('cast'):
                self.phase_cast()
            if on('rope'):
                self.phase_rope()
            for l in range(self.n_layers):
                src = self.x if l == 0 else self.XR
                for s in range(self.n_seq):
                    if on('inproj'):
                        self.phase_inproj(l, s, src)
                    if on('compress'):
                        self.phase_compress(l, s)
                    if on('attn'):
                        self.phase_attn(l, s)
                    if on('conv'):
                        self.phase_conv(l, s)
                    if on('s5'):
                        self.phase_s5(l, s)
                if on('outproj'):
                    self.phase_outproj(l, src)
                if on('ffn'):
                    self.phase_ffn(l, moe=(l % 2 == 1))
                if on('ple'):
                    self.phase_ple(l, self.y if l == self.n_layers - 1 else self.XR)
            kb.barrier()

    def phase_cast(self):
        kb = self.kb
        CH = 3584
        with ExitStack() as es:
            fb = [kb.buf(es, "cf%d" % i, [128, CH], F32) for i in range(3)]
            bb = [kb.buf(es, "cb%d" % i, [128, CH], BF16) for i in range(3)]
            n = 0
            for name, dst in self.wb.items():
                src = self.w[name]
                nl = dst.shape[0]
                s2 = src[0:nl].flatten_outer_dims()
                d2 = dst.flatten_outer_dims()
                R, Cc = s2.shape
                assert R % 128 == 0
                rpp = R // 128
                s3 = s2.rearrange("(p r) c -> p (r c)", p=128)
                d3 = d2.rearrange("(p r) c -> p (r c)", p=128)
                tot = rpp * Cc
                for c0 in range(0, tot, CH):
                    cw = min(CH, tot - c0)
                    f = fb[n % 3]
                    b = bb[n % 3]
                    kb.dma(f[:, 0:cw], s3[:, c0:c0 + cw], f, W=[f])
                    eng = ('act', 'dve', 'pool')[n % 3]
                    if eng == 'act':
                        kb.op('act', lambda e, f=f, b=b, cw=cw: e.copy(out=b[:, 0:cw], in_=f[:, 0:cw]), R=[f], W=[b])
                    else:
                        kb.op(eng, lambda e, f=f, b=b, cw=cw: e.tensor_copy(out=b[:, 0:cw], in_=f[:, 0:cw]), R=[f], W=[b])
                    kb.dma(d3[:, c0:c0 + cw], b[:, 0:cw], b, R=[b], q='act')
                    n += 1
            kb.barrier()

    def phase_rope(self):
        kb = self.kb
        with ExitStack() as es:
            pi_ = kb.buf(es, "rp_i", [128, L], I32)
            ang = kb.buf(es, "rp_a", [128, L], F32)
            kf = kb.buf(es, "rp_k", [128, L], F32)
            ki = kb.buf(es, "rp_ki", [128, L], I32)
            r = kb.buf(es, "rp_r", [128, L], F32)
            m = kb.buf(es, "rp_m", [128, L], F32)
            o = kb.buf(es, "rp_o", [128, L], F32)
            invf = kb.buf(es, "rp_f", [128, 1], F32)
            kb.dma(invf[:, :], self.c_invf[:, :], invf, W=[invf])
            HI = 6.28125
            LO = TWO_PI - HI
            for s in range(self.n_seq):
                kb.dma(pi_[:, :], self.pos[s:s + 1, :].partition_broadcast(128), pi_, W=[pi_])
                kb.op('dve', lambda e: e.tensor_copy(out=ang[:, :], in_=pi_[:, :]), R=[pi_], W=[ang])
                kb.op('dve', lambda e: e.tensor_scalar(out=ang[:, :], in0=ang[:, :], scalar1=invf[:, 0:1], scalar2=None, op0=ALU.mult), R=[ang, invf], W=[ang])
                kb.op('dve', lambda e: e.tensor_scalar(out=kf[:, :], in0=ang[:, :], scalar1=1.0 / TWO_PI, scalar2=None, op0=ALU.mult), R=[ang], W=[kf])
                kb.op('dve', lambda e: e.tensor_copy(out=ki[:, :], in_=kf[:, :]), R=[kf], W=[ki])
                kb.op('dve', lambda e: e.tensor_copy(out=kf[:, :], in_=ki[:, :]), R=[ki], W=[kf])
                kb.op('dve', lambda e: e.scalar_tensor_tensor(out=r[:, :], in0=kf[:, :], scalar=-HI, in1=ang[:, :], op0=ALU.mult, op1=ALU.add), R=[kf, ang], W=[r])
                kb.op('dve', lambda e: e.scalar_tensor_tensor(out=r[:, :], in0=kf[:, :], scalar=-LO, in1=r[:, :], op0=ALU.mult, op1=ALU.add), R=[kf, r], W=[r])
                for which, shift in ((1, 0.0), (0, math.pi / 2)):
                    kb.op('dve', lambda e, shift=shift: e.tensor_scalar(out=o[:, :], in0=r[:, :], scalar1=shift, scalar2=None, op0=ALU.add), R=[r], W=[o])
                    for _ in range(2):
                        kb.op('dve', lambda e: e.tensor_scalar(out=m[:, :], in0=o[:, :], scalar1=math.pi, scalar2=-TWO_PI, op0=ALU.is_gt, op1=ALU.mult), R=[o], W=[m])
                        kb.op('dve', lambda e: e.tensor_tensor(out=o[:, :], in0=o[:, :], in1=m[:, :], op=ALU.add), R=[o, m], W=[o])
                        kb.op('dve', lambda e: e.tensor_scalar(out=m[:, :], in0=o[:, :], scalar1=-math.pi, scalar2=TWO_PI, op0=ALU.is_lt, op1=ALU.mult), R=[o], W=[m])
                        kb.op('dve', lambda e: e.tensor_tensor(out=o[:, :], in0=o[:, :], in1=m[:, :], op=ALU.add), R=[o, m], W=[o])
                    kb.op('dve', lambda e: e.tensor_scalar(out=o[:, :], in0=o[:, :], scalar1=math.pi, scalar2=-math.pi, op0=ALU.min, op1=ALU.max), R=[o], W=[o])
                    kb.op('act', lambda e: e.activation(out=o[:, :], in_=o[:, :], func=(AF.Identity if os.environ.get('NOSIN') else AF.Sin)), R=[o], W=[o])
                    kb.dma(self.ROPE[s, which, :, :], o[:, :], o, R=[o])
            self.act_reset()
            kb.barrier()

    def phase_inproj(self, l, s, src):
        kb = self.kb
        C = self.C
        with ExitStack() as es:
            win = kb.buf(es, "ip_w", [128, 8, INW], BF16)
            for kc in range(8):
                kb.dma(win[:, kc, :], self.wb["w_in"][l, kc * 128:(kc + 1) * 128, :], win, PW=[win])
            xt = [kb.buf(es, "ip_x%d" % i, [128, 4, D], F32) for i in range(2)]
            xT = [kb.buf(es, "ip_xT%d" % i, [128, 8, 512], BF16) for i in range(2)]
            cs = [kb.buf(es, "ip_c%d" % i, [128, 2, 512], F32) for i in range(2)]
            pss = [kb.buf(es, "ip_ps%d" % i, [128, 512], F32, psum=True) for i in range(8)]
            fa = [kb.buf(es, "ip_fa%d" % i, [128, 512], F32) for i in range(2)]
            fb = [kb.buf(es, "ip_fb%d" % i, [128, 512], F32) for i in range(2)]
            t1 = [kb.buf(es, "ip_t1%d" % i, [128, 512], F32) for i in range(2)]
            t2 = [kb.buf(es, "ip_t2%d" % i, [128, 512], F32) for i in range(2)]
            oa = [kb.buf(es, "ip_oa%d" % i, [128, 512], BF16) for i in range(2)]
            ob = [kb.buf(es, "ip_ob%d" % i, [128, 512], BF16) for i in range(2)]
            of = [kb.buf(es, "ip_of%d" % i, [128, 512], F32) for i in range(3)]
            ov = [kb.buf(es, "ip_ov%d" % i, [128, 512], BF16) for i in range(2)]
            tv = [kb.buf(es, "ip_tv%d" % i, [128, 4, 256], BF16) for i in range(2)]
            tg = [kb.buf(es, "ip_tg%d" % i, [128, 4, 24], F32) for i in range(2)]
            cnt = [0]
            pc = [0]
            rc = [0]

            def nextps():
                pc[0] += 1
                return pss[2 + pc[0] % 6]

            for i in range(NT):
                t0 = i * 512
                x_ = xt[i % 2]
                xT_ = xT[i % 2]
                cs_ = cs[i % 2]
                kb.dma(x_[:, :, :], src[s, t0:t0 + 512, :].rearrange("(a p) d -> p a d", p=128), x_, W=[x_])
                kb.dma(cs_[:, 0, :], self.ROPE[s, 0, :, t0:t0 + 512], cs_, PW=[cs_])
                kb.dma(cs_[:, 1, :], self.ROPE[s, 1, :, t0:t0 + 512], cs_, PW=[cs_])
                transpose_in(kb, x_, 4, 8, xT_, C['ident'], pss[0:2], cnt)

                def fm(c0, m, ps):
                    for kc in range(8):
                        kb.op('pe', lambda e, kc=kc: e.matmul(ps[0:m, :], lhsT=win[:, kc, c0:c0 + m], rhs=xT_[:, kc, :], start=(kc == 0), stop=(kc == 7)),
                              R=[win, xT_], PW=[ps])

                pairs = [(0, 128, 128, [(32 * h, self.QT[s, h]) for h in range(4)]),
                         (256, 384, 128, [(32 * h, self.QT[s, 4 + h]) for h in range(4)]),
                         (512, 640, 128, [(0, self.KCT[s, 0]), (32, self.KCT[s, 1]), (64, self.KST[s, 0]), (96, self.KST[s, 1])]),
                         (768, 832, 64, [(0, self.KWT[s, 0]), (32, self.KWT[s, 1])])]
                for (ca, cb, m, dests) in pairs:
                    psa = nextps()
                    psb = nextps()
                    fm(ca, m, psa)
                    fm(cb, m, psb)
                    j = rc[0] % 2
                    rc[0] += 1
                    A, B, T1, T2, OA, OB = fa[j], fb[j], t1[j], t2[j], oa[j], ob[j]
                    kb.op('act', lambda e: e.copy(out=A[0:m, :], in_=psa[0:m, :]), R=[psa], W=[A])
                    kb.op('act', lambda e: e.copy(out=B[0:m, :], in_=psb[0:m, :]), R=[psb], W=[B])
                    kb.op('dve', lambda e: e.tensor_tensor(out=T1[0:m, :], in0=A[0:m, :], in1=cs_[0:m, 0, :], op=ALU.mult), R=[A, cs_], W=[T1])
                    kb.op('pool', lambda e: e.tensor_tensor(out=T2[0:m, :], in0=B[0:m, :], in1=cs_[0:m, 1, :], op=ALU.mult), R=[B, cs_], W=[T2])
                    kb.op('dve', lambda e: e.tensor_tensor(out=OA[0:m, :], in0=T1[0:m, :], in1=T2[0:m, :], op=ALU.subtract), R=[T1, T2], W=[OA])
                    kb.op('pool', lambda e: e.tensor_tensor(out=T1[0:m, :], in0=B[0:m, :], in1=cs_[0:m, 0, :], op=ALU.mult), R=[B, cs_], W=[T1])
                    kb.op('dve', lambda e: e.tensor_tensor(out=T2[0:m, :], in0=A[0:m, :], in1=cs_[0:m, 1, :], op=ALU.mult), R=[A, cs_], W=[T2])
                    kb.op('pool', lambda e: e.tensor_tensor(out=OB[0:m, :], in0=T1[0:m, :], in1=T2[0:m, :], op=ALU.add), R=[T1, T2], W=[OB])
                    for (ro, dst) in dests:
                        kb.dma(dst[0:32, t0:t0 + 512], OA[ro:ro + 32, :], OA, R=[OA])
                        kb.dma(dst[32:64, t0:t0 + 512], OB[ro:ro + 32, :], OB, R=[OB])
                ps = nextps()
                fm(896, 128, ps)
                o_ = ov[i % 2]
                kb.op('act', lambda e: e.copy(out=o_[:, :], in_=ps[:, :]), R=[ps], W=[o_])
                kb.dma(self.VCT[s, 0, :, t0:t0 + 512], o_[0:64, :], o_, R=[o_])
                kb.dma(self.VCT[s, 1, :, t0:t0 + 512], o_[64:128, :], o_, R=[o_])
                for ci in range(6):
                    ps = nextps()
                    fm(1024 + ci * 128, 128, ps)
                    o_ = of[ci % 3]
                    if ci % 2 == 0:
                        kb.op('act', lambda e, o_=o_, ps=ps: e.copy(out=o_[:, :], in_=ps[:, :]), R=[ps], W=[o_])
                    else:
                        kb.op('dve', lambda e, o_=o_, ps=ps: e.tensor_copy(out=o_[:, :], in_=ps[:, :]), R=[ps], W=[o_])
                    if ci < 4:
                        kb.dma(self.ZC[s, ci * 128:(ci + 1) * 128, t0:t0 + 512], o_[:, :], o_, R=[o_])
                    else:
                        kb.dma(self.ZS[s, (ci - 4) * 128:(ci - 3) * 128, t0:t0 + 512], o_[:, :], o_, R=[o_])
                tv_ = tv[i % 2]
                tg_ = tg[i % 2]
                for sub in range(4):
                    ps = nextps()
                    for kc in range(8):
                        kb.op('pe', lambda e, kc=kc, sub=sub, ps=ps: e.matmul(ps[:, 0:280], lhsT=xT_[:, kc, sub * 128:(sub + 1) * 128], rhs=win[:, kc, 1792:2072], start=(kc == 0), stop=(kc == 7)),
                              R=[win, xT_], PW=[ps])
                    kb.op('dve', lambda e, sub=sub, ps=ps: e.tensor_copy(out=tv_[:, sub, :], in_=ps[:, 0:256]), R=[ps], PW=[tv_])
                    kb.op('act', lambda e, sub=sub, ps=ps: e.activation(out=tg_[:, sub, :], in_=ps[:, 256:280], func=AF.Sigmoid), R=[ps], PW=[tg_])
                kb.dma(self.VSW[s, t0:t0 + 512, :].rearrange("(a p) d -> p a d", p=128), tv_[:, :, :], tv_, R=[tv_])
                kb.dma(self.G[s, t0:t0 + 512, :].rearrange("(a p) d -> p a d", p=128), tg_[:, :, :], tg_, R=[tg_])
            kb.barrier()

    def phase_compress(self, l, s):
        kb = self.kb
        with ExitStack() as es:
            w1 = [kb.buf(es, "cp_w1%d" % j, [64, 32, 256], BF16) for j in range(2)]
            w2 = [kb.buf(es, "cp_w2%d" % j, [128, 2, 64], BF16) for j in range(2)]
            b1 = [kb.buf(es, "cp_b1%d" % j, [128, 2], F32) for j in range(2)]
            peT = [kb.buf(es, "cp_pe%d" % j, [64, 32], F32) for j in range(2)]
            b2k = kb.buf(es, "cp_b2k", [64, 1], F32)
            b2v = kb.buf(es, "cp_b2v", [128, 64], F32)
            for j in range(2):
                kb.dma(w1[j][:, :, :], self.wb["cmp_w1"][l, j].rearrange("(t d) h -> d t h", d=64), w1[j], W=[w1[j]])
                kb.dma(w2[j][:, :, :], self.wb["cmp_w2"][l, j].rearrange("(c p) o -> p c o", p=128), w2[j], W=[w2[j]])
                kb.dma(b1[j][:, :], self.w["cmp_b1"][l, j].rearrange("(c p) -> p c", p=128), b1[j], W=[b1[j]], allow_slow_non_contiguous=True)
                kb.dma(peT[j][:, :], self.w["cmp_pe"][l, j].rearrange("t d -> d t"), peT[j], W=[peT[j]], allow_slow_non_contiguous=True)
            kb.dma(b2k[:, :], self.w["cmp_b2"][l, 0].rearrange("(p o) -> p o", o=1), b2k, W=[b2k], allow_slow_non_contiguous=True)
            kb.dma(b2v[:, :], self.w["cmp_b2"][l, 1:2, :].partition_broadcast(128), b2v, W=[b2v])
            kt = [kb.buf(es, "cp_kt%d" % i, [64, L], BF16) for i in range(2)]
            blk = [kb.buf(es, "cp_blk%d" % i, [64, 32, 256], BF16) for i in range(2)]
            u = kb.buf(es, "cp_u", [128, 256], F32)
            tmp = kb.buf(es, "cp_tmp", [128, 256], F32)
            gl = kb.buf(es, "cp_gl", [128, 2, 256], BF16)
            okc = kb.buf(es, "cp_okc", [64, 256], BF16)
            ovc = kb.buf(es, "cp_ovc", [128, 2, 129], BF16)
            pss = [kb.buf(es, "cp_ps%d" % i, [128, 512], F32, psum=True) for i in range(4)]
            n = 0
            for h in range(2):
                for j in range(2):
                    srcT = (self.KCT, self.VCT)[j][s, h]
                    kt_ = kt[n % 2]
                    blk_ = blk[n % 2]
                    kb.dma(kt_[:, :], srcT[:, :], kt_, W=[kt_])
                    ktv = kt_[:, :].rearrange("d (n t) -> d t n", t=16)
                    for tb in range(32):
                        eng = kb.ew()
                        if tb < 16:
                            src_ap = ktv[:, tb, 0:255]
                        else:
                            src_ap = ktv[:, tb - 16, 1:256]
                        kb.op(eng, lambda e, tb=tb, src_ap=src_ap: e.tensor_scalar(out=blk_[:, tb, 0:255], in0=src_ap, scalar1=peT[j][:, tb:tb + 1], scalar2=None, op0=ALU.add),
                              R=[kt_, peT[j]], PW=[blk_])
                    for hc in range(2):
                        ps = pss[hc]
                        for tb in range(32):
                            kb.op('pe', lambda e, tb=tb, hc=hc, ps=ps: e.matmul(ps[:, 0:255], lhsT=w1[j][:, tb, hc * 128:(hc + 1) * 128], rhs=blk_[:, tb, 0:255], start=(tb == 0), stop=(tb == 31)),
                                  R=[w1[j], blk_], PW=[ps])
                        kb.op('act', lambda e, hc=hc, ps=ps: e.activation(out=u[:, 0:255], in_=ps[:, 0:255], func=AF.Identity, bias=b1[j][:, hc:hc + 1], scale=1.0), R=[ps, b1[j]], W=[u])
                        gelu_tanh(kb, u, tmp, gl[:, hc, 0:255], gl, 255)
                    if j == 0:
                        ps = pss[2]
                        for hc in range(2):
                            kb.op('pe', lambda e, hc=hc: e.matmul(ps[0:64, 0:255], lhsT=w2[0][:, hc, :], rhs=gl[:, hc, 0:255], start=(hc == 0), stop=(hc == 1)), R=[w2[0], gl], PW=[ps])
                        kb.op('pool', lambda e: e.memset(okc[:, :], 0.0), W=[okc])
                        kb.op('act', lambda e: e.activation(out=okc[:, 0:255], in_=ps[0:64, 0:255], func=AF.Identity, bias=b2k[:, 0:1], scale=1.0), R=[ps, b2k], PW=[okc])
                        kb.dma(self.KCC[s, h, :, :], okc[:, :], okc, R=[okc])
                    else:
                        kb.op('pool', lambda e: e.memset(ovc[:, :, :], 0.0), W=[ovc])
                        kb.dma(ovc[:, :, 65:129], self.c_ovl.rearrange("(c p) o -> p c o", p=128), ovc, PW=[ovc])
                        kb.op('pool', lambda e: e.memset(ovc[:, :, 64:65], 1.0), PW=[ovc])
                        for c in range(2):
                            rows = 128 if c == 0 else 127
                            ps = pss[2 + c]
                            for hc in range(2):
                                kb.op('pe', lambda e, hc=hc, c=c, rows=rows, ps=ps: e.matmul(ps[0:rows, 0:64], lhsT=gl[:, hc, c * 128:c * 128 + rows], rhs=w2[1][:, hc, :], start=(hc == 0), stop=(hc == 1)),
                                      R=[w2[1], gl], PW=[ps])
                            kb.op('dve', lambda e, c=c, rows=rows, ps=ps: e.tensor_tensor(out=ovc[0:rows, c, 0:64], in0=ps[0:rows, 0:64], in1=b2v[0:rows, :], op=ALU.add), R=[ps, b2v], PW=[ovc])
                        kb.dma(self.VCA[s, h].rearrange("(c p) o -> p c o", p=128), ovc[:, :, :], ovc, R=[ovc])
                    n += 1
            kb.barrier()

    def phase_attn(self, l, s):
        kb = self.kb
        C = self.C
        with ExitStack() as es:
            kcT = kb.buf(es, "at_kc", [64, 256], BF16)
            vca = kb.buf(es, "at_vca", [128, 2, 129], BF16)
            kaug = kb.buf(es, "at_kaug", [128, L], BF16)
            kw = kb.buf(es, "at_kw", [64, L], BF16)
            vs = kb.buf(es, "at_vs", [128, 32, 65], BF16)
            vw = kb.buf(es, "at_vw", [128, 32, 65], BF16)
            qa = [[kb.buf(es, "at_q%d_%d" % (g, i), [128, 512], BF16) for i in range(2)] for g in range(4)]
            gt = [kb.buf(es, "at_g%d" % i, [128, 4, 24], F32) for i in range(2)]
            oacc = kb.buf(es, "at_oacc", [128, 4, 256], F32)
            imp = kb.buf(es, "at_imp", [128, 4, 64], F32)
            sc2 = kb.buf(es, "at_sc2", [128, 64], F32)
            selm = kb.buf(es, "at_selm", [128, 64], F32)
            t8 = kb.buf(es, "at_t8", [128, 16], F32)
            rv = kb.buf(es, "at_rv", [128, 8], F32)
            pt = [kb.buf(es, "at_pt%d" % i, [128, 512], BF16) for i in range(3)]
            mixo = [kb.buf(es, "at_mo%d" % i, [128, 2, 512], BF16) for i in range(2)]
            ps_s = [kb.buf(es, "at_pss%d" % i, [128, 512], F32, psum=True) for i in range(2)]
            acc = kb.buf(es, "at_acc", [128, 4, 512], F32, psum=True)
            ftmp = kb.buf(es, "at_ftmp", [128, 4, 64], F32)
            ps_t = [kb.buf(es, "at_pst%d" % i, [128, 512], F32, psum=True) for i in range(2)]
            kb.dma(kaug[64:128, :], self.c_emat[:, :], kaug, PW=[kaug])
            kb.op('pool', lambda e: e.memset(vs[:, :, 64:65], 1.0), PW=[vs])
            kb.op('pool', lambda e: e.memset(vw[:, :, 64:65], 1.0), PW=[vw])
            sc = [0]
            pcnt = [0]
            tcnt = [0]
            for k in range(2):
                kb.dma(kcT[:, :], self.KCC[s, k], kcT, W=[kcT])
                kb.dma(vca[:, :, :], self.VCA[s, k].rearrange("(c p) o -> p c o", p=128), vca, W=[vca])
                kb.dma(kaug[0:64, :], self.KST[s, k], kaug, PW=[kaug])
                kb.dma(kw[:, :], self.KWT[s, k], kw, W=[kw])
                kb.dma(vs[:, :, 0:64], self.VSW[s, :, k * 64:(k + 1) * 64].rearrange("(c p) d -> p c d", p=128), vs, PW=[vs])
                kb.dma(vw[:, :, 0:64], self.VSW[s, :, 128 + k * 64:128 + (k + 1) * 64].rearrange("(c p) d -> p c d", p=128), vw, PW=[vw])
                for i in range(NT):
                    q0 = i * 512
                    Q = [qa[g][i % 2] for g in range(4)]
                    gt_ = gt[i % 2]
                    for g in range(4):
                        kb.dma(Q[g][0:64, :], self.QT[s, 4 * k + g, :, q0:q0 + 512], Q[g], PW=[Q[g]])
                    kb.dma(gt_[:, :, :], self.G[s, q0:q0 + 512, :].rearrange("(a p) d -> p a d", p=128), gt_, W=[gt_])

                    def score(lhsT_ap, lbufs, g, krows, c0, c1, rows=128):
                        ps = ps_s[sc[0] % 2]
                        sc[0] += 1
                        kb.op('pe', lambda e: e.matmul(ps[0:rows, c0:c1], lhsT=lhsT_ap, rhs=Q[g][0:krows, c0:c1], start=True, stop=True), R=lbufs + [Q[g]], W=[ps])
                        p_ = pt[pcnt[0] % 3]
                        pcnt[0] += 1
                        kb.op('act', lambda e: e.activation(out=p_[0:rows, c0:c1], in_=ps[0:rows, c0:c1], func=AF.Exp, scale=0.125), R=[ps], W=[p_])
                        return p_

                    def run_jobs(jobs):
                        st = [None] * len(jobs)
                        if jobs:
                            st[0] = jobs[0][0]()
                        for j in range(len(jobs)):
                            if j + 1 < len(jobs):
                                st[j + 1] = jobs[j + 1][0]()
                            jobs[j][1](st[j])

                    def finish(g, br, first, with_imp=False):
                        col = 3 * (4 * k + g) + br
                        kb.op('dve', lambda e: e.tensor_scalar(out=rv[:, 0:4], in0=acc[:, :, 64], scalar1=1e-30, scalar2=None, op0=ALU.max), R=[acc], W=[rv])
                        kb.op('dve', lambda e: e.reciprocal(out=rv[:, 0:4], in_=rv[:, 0:4]), R=[rv], W=[rv])
                        if with_imp:
                            rb = rv[:, 0:4].unsqueeze(2).to_broadcast([128, 4, 64])
                            if g == 0:
                                kb.op('dve', lambda e: e.tensor_tensor(out=imp[:, :, :], in0=acc[:, :, 65:129], in1=rb, op=ALU.mult), R=[acc, rv], W=[imp])
                            else:
                                kb.op('dve', lambda e: e.tensor_tensor(out=ftmp[:, :, :], in0=acc[:, :, 65:129], in1=rb, op=ALU.mult), R=[acc, rv], W=[ftmp])
                                kb.op('pool', lambda e: e.tensor_tensor(out=imp[:, :, :], in0=imp[:, :, :], in1=ftmp[:, :, :], op=ALU.add), R=[imp, ftmp], W=[imp])
                        kb.op('dve', lambda e, col=col: e.tensor_tensor(out=rv[:, 4:8], in0=rv[:, 0:4], in1=gt_[:, :, col], op=ALU.mult), R=[rv, gt_], PW=[rv])
                        wb = rv[:, 4:8].unsqueeze(2).to_broadcast([128, 4, 64])
                        if first:
                            kb.op('dve', lambda e: e.tensor_tensor(out=oacc[:, :, g * 64:(g + 1) * 64], in0=acc[:, :, 0:64], in1=wb, op=ALU.mult), R=[acc, rv], PW=[oacc])
                        else:
                            kb.op('dve', lambda e: e.tensor_tensor(out=ftmp[:, :, :], in0=acc[:, :, 0:64], in1=wb, op=ALU.mult), R=[acc, rv], W=[ftmp])
                            kb.op('pool', lambda e: e.tensor_tensor(out=oacc[:, :, g * 64:(g + 1) * 64], in0=oacc[:, :, g * 64:(g + 1) * 64], in1=ftmp[:, :, :], op=ALU.add), R=[oacc, ftmp], PW=[oacc])

                    ncnt = min(255, 32 * i + 31)
                    chunks = [(0, min(128, ncnt))] + ([(1, ncnt - 128)] if ncnt > 128 else [])
                    jobs = []
                    for g in range(4):
                        for (c, rows) in chunks:
                            def sfn(g=g, c=c, rows=rows):
                                p_ = score(kcT[0:64, c * 128:c * 128 + rows], [kcT], g, 64, 0, 512, rows)
                                last_end = 16 * (c * 128 + rows - 1) + 31
                                if last_end > q0:
                                    kb.op('pool', lambda e: e.affine_select(out=p_[0:rows, :], in_=p_[0:rows, :], pattern=[[1, 512]], compare_op=ALU.is_ge, fill=0.0,
                                                                             base=q0 - 31 - 16 * 128 * c, channel_multiplier=-16), R=[p_], W=[p_])
                                return p_

                            def pfn(p_, g=g, c=c, rows=rows):
                                for sub in range(4):
                                    kb.op('pe', lambda e, sub=sub: e.matmul(acc[:, sub, 0:129], lhsT=p_[0:rows, sub * 128:(sub + 1) * 128], rhs=vca[0:rows, c, :],
                                                                              start=(c == 0), stop=(c == chunks[-1][0])), R=[p_, vca], PW=[acc])
                                if c == chunks[-1][0]:
                                    finish(g, 0, True, with_imp=True)
                            jobs.append((sfn, pfn))
                    run_jobs(jobs)
                    for sub in range(4):
                        b0 = 8 * i + 2 * sub
                        for hh in range(2):
                            b = b0 + hh
                            r0 = 64 * hh
                            if b + 1 < 64:
                                kb.op('pool', lambda e, sub=sub, r0=r0, b=b: e.memset(imp[r0:r0 + 64, sub, b + 1:64], -1.0), PW=[imp])
                            kb.op('pool', lambda e, sub=sub, r0=r0, b=b: e.memset(imp[r0:r0 + 64, sub, max(b - 1, 0):b + 1], 1e9), PW=[imp])
                            kb.op('pool', lambda e, sub=sub, r0=r0: e.memset(imp[r0:r0 + 64, sub, 0:1], 1e9), PW=[imp])
                        kb.op('dve', lambda e, sub=sub: e.max(out=t8[:, 0:8], in_=imp[:, sub, :]), R=[imp], PW=[t8])
                        kb.op('dve', lambda e, sub=sub: e.match_replace(out=sc2[:, :], in_to_replace=t8[:, 0:8], in_values=imp[:, sub, :], imm_value=-1e30), R=[imp, t8], W=[sc2])
                        kb.op('dve', lambda e: e.max(out=t8[:, 8:16], in_=sc2[:, :]), R=[sc2], PW=[t8])
                        kb.op('dve', lambda e, sub=sub: e.tensor_scalar(out=selm[:, :], in0=imp[:, sub, :], scalar1=t8[:, 15:16], scalar2=None, op0=ALU.is_ge), R=[imp, t8], W=[selm])
                        pst = ps_t[tcnt[0] % 2]
                        tcnt[0] += 1
                        kb.op('pe', lambda e, pst=pst: e.transpose(pst[0:64, 0:128], selm[:, :], C['ident'][:, :]), R=[selm, C['ident']], W=[pst])
                        for g in range(4):
                            kb.op('act', lambda e, g=g, sub=sub, pst=pst: e.activation(out=Q[g][64:128, sub * 128:(sub + 1) * 128], in_=pst[0:64, 0:128], func=AF.Identity, bias=-BIG, scale=BIG),
                                  R=[pst], PW=[Q[g]])
                    jobs = []
                    for g in range(4):
                        nch = 4 * i + 4
                        for c in range(nch):
                            def sfn(g=g, c=c):
                                off = 128 * c - q0
                                c0 = max(off, 0)
                                p_ = score(kaug[:, c * 128:(c + 1) * 128], [kaug], g, 128, c0, 512)
                                if off >= 0:
                                    kb.op('pool', lambda e: e.affine_select(out=p_[:, c0:c0 + 128], in_=p_[:, c0:c0 + 128], pattern=[[1, 128]], compare_op=ALU.is_ge, fill=0.0,
                                                                             base=0, channel_multiplier=-1), R=[p_], W=[p_])
                                return p_

                            def pfn(p_, g=g, c=c, nch=nch):
                                c0 = max(128 * c - q0, 0)
                                for sub in range(4):
                                    if sub * 128 < c0:
                                        continue
                                    kb.op('pe', lambda e, sub=sub: e.matmul(acc[:, sub, 0:65], lhsT=p_[:, sub * 128:(sub + 1) * 128], rhs=vs[:, c, :],
                                                                              start=(c == 0), stop=(c == 4 * i + sub)), R=[p_, vs], PW=[acc])
                                if c == nch - 1:
                                    finish(g, 1, False)
                            jobs.append((sfn, pfn))
                    run_jobs(jobs)
                    jobs = []
                    for g in range(4):
                        clist = list(range(max(0, 4 * i - 4), 4 * i + 4))
                        for c in clist:
                            off = 128 * c - q0
                            if off >= 0:
                                c0, c1 = off, 512
                            else:
                                m = (off + 512) // 128
                                c0, c1 = 0, 128 * (m + 1)

                            def sfn(g=g, c=c, off=off, c0=c0, c1=c1):
                                p_ = score(kw[0:64, c * 128:(c + 1) * 128], [kw], g, 64, c0, c1)
                                if off >= 0:
                                    kb.op('pool', lambda e: e.affine_select(out=p_[:, c0:c0 + 128], in_=p_[:, c0:c0 + 128], pattern=[[1, 128]], compare_op=ALU.is_ge, fill=0.0,
                                                                             base=0, channel_multiplier=-1), R=[p_], W=[p_])
                                else:
                                    kb.op('pool', lambda e: e.affine_select(out=p_[:, c1 - 128:c1], in_=p_[:, c1 - 128:c1], pattern=[[-1, 128]], compare_op=ALU.is_ge, fill=0.0,
                                                                             base=-1, channel_multiplier=1), R=[p_], W=[p_])
                                return p_

                            def pfn(p_, g=g, c=c, c0=c0, c1=c1, last=(c == clist[-1])):
                                for sub in range(4):
                                    if not (c0 <= sub * 128 < c1):
                                        continue
                                    kb.op('pe', lambda e, sub=sub: e.matmul(acc[:, sub, 0:65], lhsT=p_[:, sub * 128:(sub + 1) * 128], rhs=vw[:, c, :],
                                                                              start=(c == max(0, 4 * i + sub - 4)), stop=(c == 4 * i + sub)), R=[p_, vw], PW=[acc])
                                if last:
                                    finish(g, 2, False)
                            jobs.append((sfn, pfn))
                    run_jobs(jobs)
                    mo = mixo[i % 2]
                    for fc in range(2):
                        pst = ps_t[tcnt[0] % 2]
                        tcnt[0] += 1
                        for sub in range(4):
                            kb.op('pe', lambda e, fc=fc, sub=sub, pst=pst: e.transpose(pst[:, sub * 128:(sub + 1) * 128], oacc[:, sub, fc * 128:(fc + 1) * 128], C['ident'][:, :]),
                                  R=[oacc, C['ident']], PW=[pst])
                        kb.op('act', lambda e, fc=fc, pst=pst: e.copy(out=mo[:, fc, :], in_=pst[:, :]), R=[pst], PW=[mo])
                    kb.dma(self.MIXT[s, k * 256:(k + 1) * 256, q0:q0 + 512].rearrange("(c p) t -> p c t", p=128), mo[:, :, :], mo, R=[mo])
            kb.barrier()

    def phase_conv(self, l, s):
        kb = self.kb
        with ExitStack() as es:
            cw = kb.buf(es, "cv_w", [128, 2, 31], F32)
            cb = kb.buf(es, "cv_b", [128, 2], F32)
            lg = kb.buf(es, "cv_g", [128, 2], F32)
            lb = kb.buf(es, "cv_lb", [128, 2], F32)
            ones = kb.buf(es, "cv_ones", [128, 128], F32)
            for c in range(2):
                kb.dma(cw[:, c, :], self.w["conv_w"][l][:, c * 128:(c + 1) * 128].rearrange("k p -> p k"), cw, PW=[cw], allow_slow_non_contiguous=True)
            for (t_, nm) in ((cb, "conv_b"), (lg, "conv_ln_g"), (lb, "conv_ln_b")):
                kb.dma(t_[:, :], self.w[nm][l].rearrange("(c p) -> p c", p=128), t_, W=[t_], allow_slow_non_contiguous=True)
            kb.op('pool', lambda e: e.memset(ones[:, :], 1.0 / 256.0), W=[ones])
            a = [kb.buf(es, "cv_a%d" % c, [128, L], F32) for c in range(2)]
            u = [kb.buf(es, "cv_u%d" % c, [128, L + 32], F32) for c in range(2)]
            y = [kb.buf(es, "cv_y%d" % c, [128, L], F32) for c in range(2)]
            sq = [kb.buf(es, "cv_sq%d" % c, [128, 512], F32) for c in range(2)]
            mean = kb.buf(es, "cv_mean", [128, 512], F32)
            rstd = kb.buf(es, "cv_rstd", [128, 512], F32)
            tmp = kb.buf(es, "cv_tmp", [128, 512], F32)
            ob = [kb.buf(es, "cv_ob%d" % c, [128, 512], BF16) for c in range(2)]
            ps1 = kb.buf(es, "cv_ps1", [128, 512], F32, psum=True)
            ps2 = kb.buf(es, "cv_ps2", [128, 512], F32, psum=True)
            eps = self.C['eps']
            for c in range(2):
                kb.dma(a[c][:, :], self.ZC[s, c * 128:(c + 1) * 128, :], a[c], W=[a[c]])
                kb.dma(y[c][:, :], self.ZC[s, 256 + c * 128:256 + (c + 1) * 128, :], y[c], W=[y[c]])
                kb.op('act', lambda e, c=c: e.activation(out=y[c][:, :], in_=y[c][:, :], func=AF.Sigmoid), R=[y[c]], W=[y[c]])
                kb.op('pool', lambda e, c=c: e.memset(u[c][:, 0:32], 0.0), PW=[u[c]])
                kb.op('pool', lambda e, c=c: e.tensor_tensor(out=u[c][:, 32:L + 32], in0=a[c][:, :], in1=y[c][:, :], op=ALU.mult), R=[a[c], y[c]], PW=[u[c]])
                kb.op('dve', lambda e, c=c: e.tensor_scalar(out=y[c][:, :], in0=u[c][:, 2:L + 2], scalar1=cw[:, c, 0:1], scalar2=cb[:, c:c + 1], op0=ALU.mult, op1=ALU.add), R=[u[c], cw, cb], W=[y[c]])
                for k in range(1, 31):
                    kb.op('dve', lambda e, c=c, k=k: e.scalar_tensor_tensor(out=y[c][:, :], in0=u[c][:, 2 + k:L + 2 + k], scalar=cw[:, c, k:k + 1], in1=y[c][:, :], op0=ALU.mult, op1=ALU.add),
                          R=[u[c], cw, y[c]], W=[y[c]])
            for i in range(NT):
                t0 = i * 512
                for c in range(2):
                    kb.op('act', lambda e, c=c: e.activation(out=sq[c][:, :], in_=y[c][:, t0:t0 + 512], func=AF.Square), R=[y[c]], W=[sq[c]])
                for c in range(2):
                    kb.op('pe', lambda e, c=c: e.matmul(ps1[:, :], lhsT=ones[:, :], rhs=y[c][:, t0:t0 + 512], start=(c == 0), stop=(c == 1)), R=[ones, y[c]], PW=[ps1])
                for c in range(2):
                    kb.op('pe', lambda e, c=c: e.matmul(ps2[:, :], lhsT=ones[:, :], rhs=sq[c][:, :], start=(c == 0), stop=(c == 1)), R=[ones, sq[c]], PW=[ps2])
                kb.op('act', lambda e: e.copy(out=mean[:, :], in_=ps1[:, :]), R=[ps1], W=[mean])
                kb.op('dve', lambda e: e.tensor_tensor(out=tmp[:, :], in0=mean[:, :], in1=mean[:, :], op=ALU.mult), R=[mean], W=[tmp])
                kb.op('dve', lambda e: e.tensor_tensor(out=rstd[:, :], in0=ps2[:, :], in1=tmp[:, :], op=ALU.subtract), R=[ps2, tmp], W=[rstd])
                kb.op('act', lambda e: e.activation(out=rstd[:, :], in_=rstd[:, :], func=AF.Sqrt, bias=eps[:, 0:1], scale=1.0), R=[rstd, eps], W=[rstd])
                kb.op('dve', lambda e: e.reciprocal(out=rstd[:, :], in_=rstd[:, :]), R=[rstd], W=[rstd])
                for c in range(2):
                    kb.op('dve', lambda e, c=c: e.tensor_tensor(out=tmp[:, :], in0=y[c][:, t0:t0 + 512], in1=mean[:, :], op=ALU.subtract), R=[y[c], mean], W=[tmp])
                    kb.op('pool', lambda e: e.tensor_tensor(out=tmp[:, :], in0=tmp[:, :], in1=rstd[:, :], op=ALU.mult), R=[tmp, rstd], W=[tmp])
                    o_ = ob[c]
                    kb.op('act', lambda e, c=c, o_=o_: e.activation(out=o_[:, :], in_=tmp[:, :], func=AF.Silu, bias=lb[:, c:c + 1], scale=lg[:, c:c + 1]), R=[tmp, lb, lg], W=[o_])
                    kb.dma(self.MIXT[s, 512 + c * 128:512 + (c + 1) * 128, t0:t0 + 512], o_[:, :], o_, R=[o_])
            kb.barrier()

    def sincos_small(self, es, th, n, cs_out, sn_out, tag):
        kb = self.kb
        kf = kb.buf(es, "sc_kf" + tag, [128, n], F32)
        ki = kb.buf(es, "sc_ki" + tag, [128, n], I32)
        r = kb.buf(es, "sc_r" + tag, [128, n], F32)
        m = kb.buf(es, "sc_m" + tag, [128, n], F32)
        HI = 6.28125
        LO = TWO_PI - HI
        kb.op('dve', lambda e: e.tensor_scalar(out=kf[:, :], in0=th[:, :], scalar1=1.0 / TWO_PI, scalar2=None, op0=ALU.mult), R=[th], W=[kf])
        kb.op('dve', lambda e: e.tensor_copy(out=ki[:, :], in_=kf[:, :]), R=[kf], W=[ki])
        kb.op('dve', lambda e: e.tensor_copy(out=kf[:, :], in_=ki[:, :]), R=[ki], W=[kf])
        kb.op('dve', lambda e: e.scalar_tensor_tensor(out=r[:, :], in0=kf[:, :], scalar=-HI, in1=th[:, :], op0=ALU.mult, op1=ALU.add), R=[kf, th], W=[r])
        kb.op('dve', lambda e: e.scalar_tensor_tensor(out=r[:, :], in0=kf[:, :], scalar=-LO, in1=r[:, :], op0=ALU.mult, op1=ALU.add), R=[kf, r], W=[r])
        for o, shift in ((sn_out, 0.0), (cs_out, math.pi / 2)):
            kb.op('dve', lambda e, o=o, shift=shift: e.tensor_scalar(out=o[:, :], in0=r[:, :], scalar1=shift, scalar2=None, op0=ALU.add), R=[r], W=[o])
            for _ in range(2):
                kb.op('dve', lambda e, o=o: e.tensor_scalar(out=m[:, :], in0=o[:, :], scalar1=math.pi, scalar2=-TWO_PI, op0=ALU.is_gt, op1=ALU.mult), R=[o], W=[m])
                kb.op('dve', lambda e, o=o: e.tensor_tensor(out=o[:, :], in0=o[:, :], in1=m[:, :], op=ALU.add), R=[o, m], W=[o])
                kb.op('dve', lambda e, o=o: e.tensor_scalar(out=m[:, :], in0=o[:, :], scalar1=-math.pi, scalar2=TWO_PI, op0=ALU.is_lt, op1=ALU.mult), R=[o], W=[m])
                kb.op('dve', lambda e, o=o: e.tensor_tensor(out=o[:, :], in0=o[:, :], in1=m[:, :], op=ALU.add), R=[o, m], W=[o])
            kb.op('dve', lambda e, o=o: e.tensor_scalar(out=o[:, :], in0=o[:, :], scalar1=math.pi, scalar2=-math.pi, op0=ALU.min, op1=ALU.max), R=[o], W=[o])
            kb.op('act', lambda e, o=o: e.activation(out=o[:, :], in_=o[:, :], func=AF.Sin), R=[o], W=[o])
        self.act_reset()

    def phase_s5(self, l, s):
        kb = self.kb
        C = self.C
        TT = ALU.mult
        with ExitStack() as es:
            are = kb.buf(es, "s5_are", [128, 8], F32)
            aim = kb.buf(es, "s5_aim", [128, 8], F32)
            dt = kb.buf(es, "s5_dt", [128, 8], F32)
            kb.dma(are[:, :], self.w["s5_a_re"][l].rearrange("g n -> (g n)").rearrange("(m p) -> p m", p=128), are, W=[are], allow_slow_non_contiguous=True)
            kb.dma(aim[:, :], self.w["s5_a_im"][l].rearrange("g n -> (g n)").rearrange("(m p) -> p m", p=128), aim, W=[aim], allow_slow_non_contiguous=True)
            ldt2 = self.w["s5_log_dt"][l:l + 1, :].rearrange("o (m h) -> o h m", h=2)
            kb.dma(dt[0:64, :], ldt2[:, 0, :].partition_broadcast(64), dt, PW=[dt], allow_slow_non_contiguous=True)
            kb.dma(dt[64:128, :], ldt2[:, 1, :].partition_broadcast(64), dt, PW=[dt], allow_slow_non_contiguous=True)
            kb.op('act', lambda e: e.activation(out=dt[:, :], in_=dt[:, :], func=AF.Exp), R=[dt], W=[dt])
            rho = kb.buf(es, "s5_rho", [128, 8], F32)
            th = kb.buf(es, "s5_th", [128, 8], F32)
            kb.op('dve', lambda e: e.tensor_tensor(out=rho[:, :], in0=are[:, :], in1=dt[:, :], op=TT), R=[are, dt], W=[rho])
            kb.op('act', lambda e: e.activation(out=rho[:, :], in_=rho[:, :], func=AF.Exp), R=[rho], W=[rho])
            kb.op('dve', lambda e: e.tensor_tensor(out=th[:, :], in0=aim[:, :], in1=dt[:, :], op=TT), R=[aim, dt], W=[th])
            c1 = kb.buf(es, "s5_c1", [128, 8], F32)
            s1 = kb.buf(es, "s5_s1", [128, 8], F32)
            self.sincos_small(es, th, 8, c1, s1, "a")
            abr = kb.buf(es, "s5_abr", [128, 8], F32)
            abi = kb.buf(es, "s5_abi", [128, 8], F32)
            den = kb.buf(es, "s5_den", [128, 8], F32)
            t1 = kb.buf(es, "s5_t1", [128, 8], F32)
            cfr = kb.buf(es, "s5_cfr", [128, 8], F32)
            cfi = kb.buf(es, "s5_cfi", [128, 8], F32)
            V = lambda fn, R, W: kb.op('dve', fn, R=R, W=W)
            V(lambda e: e.tensor_tensor(out=abr[:, :], in0=rho[:, :], in1=c1[:, :], op=TT), [rho, c1], [abr])
            V(lambda e: e.tensor_tensor(out=abi[:, :], in0=rho[:, :], in1=s1[:, :], op=TT), [rho, s1], [abi])
            V(lambda e: e.tensor_tensor(out=den[:, :], in0=are[:, :], in1=are[:, :], op=TT), [are], [den])
            V(lambda e: e.tensor_tensor(out=t1[:, :], in0=aim[:, :], in1=aim[:, :], op=TT), [aim], [t1])
            V(lambda e: e.tensor_tensor(out=den[:, :], in0=den[:, :], in1=t1[:, :], op=ALU.add), [den, t1], [den])
            V(lambda e: e.reciprocal(out=den[:, :], in_=den[:, :]), [den], [den])
            V(lambda e: e.tensor_scalar(out=abr[:, :], in0=abr[:, :], scalar1=-1.0, scalar2=None, op0=ALU.add), [abr], [abr])
            V(lambda e: e.tensor_tensor(out=cfr[:, :], in0=abr[:, :], in1=are[:, :], op=TT), [abr, are], [cfr])
            V(lambda e: e.tensor_tensor(out=t1[:, :], in0=abi[:, :], in1=aim[:, :], op=TT), [abi, aim], [t1])
            V(lambda e: e.tensor_tensor(out=cfr[:, :], in0=cfr[:, :], in1=t1[:, :], op=ALU.add), [cfr, t1], [cfr])
            V(lambda e: e.tensor_tensor(out=cfr[:, :], in0=cfr[:, :], in1=den[:, :], op=TT), [cfr, den], [cfr])
            V(lambda e: e.tensor_tensor(out=cfi[:, :], in0=abi[:, :], in1=are[:, :], op=TT), [abi, are], [cfi])
            V(lambda e: e.tensor_tensor(out=t1[:, :], in0=abr[:, :], in1=aim[:, :], op=TT), [abr, aim], [t1])
            V(lambda e: e.tensor_tensor(out=cfi[:, :], in0=cfi[:, :], in1=t1[:, :], op=ALU.subtract), [cfi, t1], [cfi])
            V(lambda e: e.tensor_tensor(out=cfi[:, :], in0=cfi[:, :], in1=den[:, :], op=TT), [cfi, den], [cfi])
            bre = kb.buf(es, "s5_bre", [128, 8, 16], F32)
            bim = kb.buf(es, "s5_bim", [128, 8, 16], F32)
            kb.dma(bre[:, :, :], self.w["s5_b_re"][l].rearrange("g n c -> (g n) c").rearrange("(m p) c -> p m c", p=128), bre, W=[bre])
            kb.dma(bim[:, :, :], self.w["s5_b_im"][l].rearrange("g n c -> (g n) c").rearrange("(m p) c -> p m c", p=128), bim, W=[bim])
            bpad = [kb.buf(es, "s5_bpad%d" % j, [128, 8, 128], F32) for j in range(2)]
            t16 = kb.buf(es, "s5_t16", [128, 16], F32)
            for j in range(2):
                kb.op('pool', lambda e, j=j: e.memset(bpad[j][:, :, :], 0.0), W=[bpad[j]])
            for m in range(8):
                for hh in range(2):
                    r0 = 64 * hh
                    co = ((2 * m + hh) * 16) % 128
                    rs_ = slice(r0, r0 + 64)
                    V(lambda e, m=m, rs_=rs_: e.tensor_scalar(out=t16[rs_, :], in0=bim[rs_, m, :], scalar1=cfi[rs_, m:m + 1], scalar2=None, op0=TT), [bim, cfi], [t16])
                    kb.op('dve', lambda e, m=m, rs_=rs_, co=co: e.scalar_tensor_tensor(out=bpad[0][rs_, m, co:co + 16], in0=bre[rs_, m, :], scalar=cfr[rs_, m:m + 1], in1=t16[rs_, :], op0=TT, op1=ALU.subtract),
                          R=[bre, cfr, t16], PW=[bpad[0]])
                    V(lambda e, m=m, rs_=rs_: e.tensor_scalar(out=t16[rs_, :], in0=bre[rs_, m, :], scalar1=cfi[rs_, m:m + 1], scalar2=None, op0=TT), [bre, cfi], [t16])
                    kb.op('dve', lambda e, m=m, rs_=rs_, co=co: e.scalar_tensor_tensor(out=bpad[1][rs_, m, co:co + 16], in0=bim[rs_, m, :], scalar=cfr[rs_, m:m + 1], in1=t16[rs_, :], op0=TT, op1=ALU.add),
                          R=[bim, cfr, t16], PW=[bpad[1]])
            pss = [kb.buf(es, "s5_ps%d" % i, [128, 512], F32, psum=True) for i in range(8)]
            wB = [kb.buf(es, "s5_wB%d" % j, [128, 8, 128], BF16) for j in range(2)]
            for j in range(2):
                for m in range(8):
                    ps = pss[m % 2]
                    kb.op('pe', lambda e, j=j, m=m, ps=ps: e.transpose(ps[:, 0:128], bpad[j][:, m, :], C['ident'][:, :]), R=[bpad[j], C['ident']], W=[ps])
                    kb.op('act', lambda e, j=j, m=m, ps=ps: e.copy(out=wB[j][:, m, :], in_=ps[:, 0:128]), R=[ps], PW=[wB[j]])
            craw = [kb.buf(es, "s5_craw%d" % j, [128, 2, 64], F32) for j in range(2)]
            cT = [kb.buf(es, "s5_cT%d" % j, [64, 256], F32) for j in range(2)]
            wC = [kb.buf(es, "s5_wC%d" % j, [128, 8, 128], BF16) for j in range(2)]
            for j, nm in enumerate(("s5_c_re", "s5_c_im")):
                kb.dma(craw[j][:, :, :], self.w[nm][l].rearrange("g c n -> (g c) n").rearrange("(a p) n -> p a n", p=128), craw[j], W=[craw[j]])
                kb.op('pool', lambda e, j=j: e.memset(wC[j][:, :, :], 0.0), W=[wC[j]])
                for a_ in range(2):
                    ps = pss[2 + a_]
                    kb.op('pe', lambda e, j=j, a_=a_, ps=ps: e.transpose(ps[0:64, 0:128], craw[j][:, a_, :], C['ident'][:, :]), R=[craw[j], C['ident']], W=[ps])
                    kb.op('act', lambda e, j=j, a_=a_, ps=ps: e.copy(out=cT[j][:, a_ * 128:(a_ + 1) * 128], in_=ps[0:64, 0:128]), R=[ps], PW=[cT[j]])
                sgn = 1.0 if j == 0 else -1.0
                for m in range(8):
                    for hh in range(2):
                        g_ = 2 * m + hh
                        co = (g_ * 16) % 128
                        kb.op('dve', lambda e, j=j, m=m, hh=hh, g_=g_, co=co, sgn=sgn: e.tensor_scalar(out=wC[j][64 * hh:64 * hh + 64, m, co:co + 16], in0=cT[j][:, g_ * 16:(g_ + 1) * 16],
                                                                                                  scalar1=sgn, scalar2=None, op0=TT), R=[cT[j]], PW=[wC[j]])
            CT = kb.buf(es, "s5_CT", [128, 8, 512], F32)
            ST = kb.buf(es, "s5_ST", [128, 8, 512], F32)
            RH = kb.buf(es, "s5_RH", [128, 8, 512], F32)
            ck = kb.buf(es, "s5_ck", [128, 8], F32)
            sk = kb.buf(es, "s5_sk", [128, 8], F32)
            tk = kb.buf(es, "s5_tk", [128, 8], F32)
            tk2 = kb.buf(es, "s5_tk2", [128, 8], F32)
            tb1 = kb.buf(es, "s5_tb1", [128, 8, 256], F32)
            tb2 = kb.buf(es, "s5_tb2", [128, 8, 256], F32)
            V(lambda e: e.tensor_copy(out=ck[:, :], in_=c1[:, :]), [c1], [ck])
            V(lambda e: e.tensor_copy(out=sk[:, :], in_=s1[:, :]), [s1], [sk])
            kb.op('pool', lambda e: e.memset(CT[:, :, 0:1], 1.0), PW=[CT])
            kb.op('pool', lambda e: e.memset(ST[:, :, 0:1], 0.0), PW=[ST])
            for m in range(8):
                V(lambda e, m=m: e.tensor_copy(out=RH[:, m, :], in_=rho[:, m:m + 1].to_broadcast([128, 512])), [rho], [RH])
            w_ = 1
            for kk in range(10):
                if kk < 9:
                    cb_ = ck[:, :].unsqueeze(2).to_broadcast([128, 8, w_])
                    sb_ = sk[:, :].unsqueeze(2).to_broadcast([128, 8, w_])
                    V(lambda e, w_=w_, cb_=cb_: e.tensor_tensor(out=tb1[:, :, 0:w_], in0=CT[:, :, 0:w_], in1=cb_, op=TT), [CT, ck], [tb1])
                    V(lambda e, w_=w_, sb_=sb_: e.tensor_tensor(out=tb2[:, :, 0:w_], in0=ST[:, :, 0:w_], in1=sb_, op=TT), [ST, sk], [tb2])
                    kb.op('dve', lambda e, w_=w_: e.tensor_tensor(out=CT[:, :, w_:2 * w_], in0=tb1[:, :, 0:w_], in1=tb2[:, :, 0:w_], op=ALU.subtract), R=[tb1, tb2], PW=[CT])
                    V(lambda e, w_=w_, sb_=sb_: e.tensor_tensor(out=tb1[:, :, 0:w_], in0=CT[:, :, 0:w_], in1=sb_, op=TT), [CT, sk], [tb1])
                    V(lambda e, w_=w_, cb_=cb_: e.tensor_tensor(out=tb2[:, :, 0:w_], in0=ST[:, :, 0:w_], in1=cb_, op=TT), [ST, ck], [tb2])
                    kb.op('dve', lambda e, w_=w_: e.tensor_tensor(out=ST[:, :, w_:2 * w_], in0=tb1[:, :, 0:w_], in1=tb2[:, :, 0:w_], op=ALU.add), R=[tb1, tb2], PW=[ST])
                    w_ *= 2
                    V(lambda e: e.tensor_tensor(out=tk[:, :], in0=ck[:, :], in1=ck[:, :], op=TT), [ck], [tk])
                    V(lambda e: e.tensor_tensor(out=tk2[:, :], in0=sk[:, :], in1=sk[:, :], op=TT), [sk], [tk2])
                    V(lambda e: e.tensor_tensor(out=tk[:, :], in0=tk[:, :], in1=tk2[:, :], op=ALU.subtract), [tk, tk2], [tk])
                    V(lambda e: e.tensor_tensor(out=tk2[:, :], in0=ck[:, :], in1=sk[:, :], op=TT), [ck, sk], [tk2])
                    V(lambda e: e.tensor_scalar(out=sk[:, :], in0=tk2[:, :], scalar1=2.0, scalar2=None, op0=TT), [tk2], [sk])
                    V(lambda e: e.tensor_copy(out=ck[:, :], in_=tk[:, :]), [tk], [ck])
            nsk = kb.buf(es, "s5_nsk", [128, 8], F32)
            V(lambda e: e.tensor_scalar(out=nsk[:, :], in0=sk[:, :], scalar1=-1.0, scalar2=None, op0=TT), [sk], [nsk])
            dsk = kb.buf(es, "s5_dsk", [128, 2], F32)
            glb = kb.buf(es, "s5_glb", [128, 2], F32)
            glw = kb.buf(es, "s5_glw", [128, 2, 256], BF16)
            kb.dma(dsk[:, :], self.w["s5_d"][l].rearrange("(c p) -> p c", p=128), dsk, W=[dsk], allow_slow_non_contiguous=True)
            kb.dma(glb[:, :], self.w["s5_glu_b"][l].rearrange("(c p) -> p c", p=128), glb, W=[glb], allow_slow_non_contiguous=True)
            kb.dma(glw[:, :, :], self.wb["s5_glu_w"][l].rearrange("(c p) o -> p c o", p=128), glw, W=[glw])
            uf = [kb.buf(es, "s5_uf%d" % i, [128, 2, 512], F32) for i in range(2)]
            ub = [kb.buf(es, "s5_ub%d" % i, [128, 2, 512], BF16) for i in range(2)]
            ini = kb.buf(es, "s5_ini", [128, 8, 2], F32)
            kb.op('pool', lambda e: e.memset(ini[:, :, :], 0.0), W=[ini])
            vr2 = [kb.buf(es, "s5_vr%d" % i, [128, 512], F32) for i in range(2)]
            vi2 = [kb.buf(es, "s5_vi%d" % i, [128, 512], F32) for i in range(2)]
            ta2 = [kb.buf(es, "s5_ta%d" % i, [128, 512], F32) for i in range(2)]
            tb2_ = [kb.buf(es, "s5_tb%d" % i, [128, 512], F32) for i in range(2)]
            tc2 = [kb.buf(es, "s5_tc%d" % i, [128, 512], F32) for i in range(2)]
            td2 = [kb.buf(es, "s5_td%d" % i, [128, 512], F32) for i in range(2)]
            gr2 = [kb.buf(es, "s5_gr%d" % i, [128, 512], F32) for i in range(2)]
            gi2 = [kb.buf(es, "s5_gi%d" % i, [128, 512], F32) for i in range(2)]
            hr = [kb.buf(es, "s5_hr%d" % i, [128, 512], BF16) for i in range(2)]
            hi = [kb.buf(es, "s5_hi%d" % i, [128, 512], BF16) for i in range(2)]
            yb = kb.buf(es, "s5_y", [128, 512], F32)
            tm = kb.buf(es, "s5_tm", [128, 512], F32)
            zf = kb.buf(es, "s5_zf", [128, 2, 512], F32)
            zb = kb.buf(es, "s5_zb", [128, 2, 512], BF16)
            sg = kb.buf(es, "s5_sg", [128, 512], F32)
            ob = [kb.buf(es, "s5_ob%d" % i, [128, 512], BF16) for i in range(2)]
            tcol = kb.buf(es, "s5_tcol", [128, 2], F32)
            n = 0
            for i in range(NT):
                t0 = i * 512
                uf_ = uf[i % 2]
                ub_ = ub[i % 2]
                kb.dma(uf_[:, :, :], self.ZS[s, :, t0:t0 + 512].rearrange("(c p) t -> p c t", p=128), uf_, W=[uf_])
                kb.op('act', lambda e: e.copy(out=ub_[:, :, :], in_=uf_[:, :, :]), R=[uf_], W=[ub_])
                for cc in range(2):
                    yps = pss[4 + cc]
                    for mm in range(4):
                        m = cc * 4 + mm
                        pr = pss[(2 * n) % 4]
                        pi = pss[(2 * n + 1) % 4]
                        hr_ = hr[n % 2]
                        hi_ = hi[n % 2]
                        vr, vi, ta, tbb, gr, gi = vr2[n % 2], vi2[n % 2], ta2[n % 2], tb2_[n % 2], gr2[n % 2], gi2[n % 2]
                        tc, td = tc2[n % 2], td2[n % 2]
                        n += 1
                        kb.op('pe', lambda e, m=m, pr=pr, cc=cc: e.matmul(pr[:, :], lhsT=wB[0][:, m, :], rhs=ub_[:, cc, :], start=True, stop=True), R=[wB[0], ub_], W=[pr])
                        kb.op('pe', lambda e, m=m, pi=pi, cc=cc: e.matmul(pi[:, :], lhsT=wB[1][:, m, :], rhs=ub_[:, cc, :], start=True, stop=True), R=[wB[1], ub_], W=[pi])
                        V(lambda e, m=m, pr=pr: e.tensor_tensor(out=ta[:, :], in0=pr[:, :], in1=CT[:, m, :], op=TT), [pr, CT], [ta])
                        V(lambda e, m=m, pi=pi: e.tensor_tensor(out=tbb[:, :], in0=pi[:, :], in1=ST[:, m, :], op=TT), [pi, ST], [tbb])
                        kb.op('pool', lambda e: e.tensor_tensor(out=vr[:, :], in0=ta[:, :], in1=tbb[:, :], op=ALU.add), R=[ta, tbb], W=[vr])
                        V(lambda e, m=m, pi=pi: e.tensor_tensor(out=ta[:, :], in0=pi[:, :], in1=CT[:, m, :], op=TT), [pi, CT], [ta])
                        V(lambda e, m=m, pr=pr: e.tensor_tensor(out=tbb[:, :], in0=pr[:, :], in1=ST[:, m, :], op=TT), [pr, ST], [tbb])
                        kb.op('pool', lambda e: e.tensor_tensor(out=vi[:, :], in0=ta[:, :], in1=tbb[:, :], op=ALU.subtract), R=[ta, tbb], W=[vi])
                        V(lambda e, m=m: e.tensor_tensor_scan(out=gr[:, :], data0=RH[:, m, :], data1=vr[:, :], initial=ini[:, m, 0:1], op0=ALU.mult, op1=ALU.add), [RH, vr, ini], [gr])
                        V(lambda e, m=m: e.tensor_tensor_scan(out=gi[:, :], data0=RH[:, m, :], data1=vi[:, :], initial=ini[:, m, 1:2], op0=ALU.mult, op1=ALU.add), [RH, vi, ini], [gi])
                        V(lambda e, m=m: e.tensor_scalar(out=tcol[:, 0:1], in0=gr[:, 511:512], scalar1=ck[:, m:m + 1], scalar2=None, op0=TT), [gr, ck], [tcol])
                        kb.op('dve', lambda e, m=m: e.scalar_tensor_tensor(out=ini[:, m, 0:1], in0=gi[:, 511:512], scalar=nsk[:, m:m + 1], in1=tcol[:, 0:1], op0=TT, op1=ALU.add), R=[gi, nsk, tcol], PW=[ini])
                        V(lambda e, m=m: e.tensor_scalar(out=tcol[:, 1:2], in0=gi[:, 511:512], scalar1=ck[:, m:m + 1], scalar2=None, op0=TT), [gi, ck], [tcol])
                        kb.op('dve', lambda e, m=m: e.scalar_tensor_tensor(out=ini[:, m, 1:2], in0=gr[:, 511:512], scalar=sk[:, m:m + 1], in1=tcol[:, 1:2], op0=TT, op1=ALU.add), R=[gr, sk, tcol], PW=[ini])
                        kb.op('pool', lambda e, m=m, gr=gr, tc=tc: e.tensor_tensor(out=tc[:, :], in0=gr[:, :], in1=CT[:, m, :], op=TT), R=[gr, CT], W=[tc])
                        kb.op('pool', lambda e, m=m, gi=gi, td=td: e.tensor_tensor(out=td[:, :], in0=gi[:, :], in1=ST[:, m, :], op=TT), R=[gi, ST], W=[td])
                        V(lambda e, hr_=hr_, tc=tc, td=td: e.tensor_tensor(out=hr_[:, :], in0=tc[:, :], in1=td[:, :], op=ALU.subtract), [tc, td], [hr_])
                        kb.op('pool', lambda e, m=m, gr=gr, tc=tc: e.tensor_tensor(out=tc[:, :], in0=gr[:, :], in1=ST[:, m, :], op=TT), R=[gr, ST], W=[tc])
                        kb.op('pool', lambda e, m=m, gi=gi, td=td: e.tensor_tensor(out=td[:, :], in0=gi[:, :], in1=CT[:, m, :], op=TT), R=[gi, CT], W=[td])
                        V(lambda e, hi_=hi_, tc=tc, td=td: e.tensor_tensor(out=hi_[:, :], in0=tc[:, :], in1=td[:, :], op=ALU.add), [tc, td], [hi_])
                        kb.op('pe', lambda e, m=m, hr_=hr_, mm=mm, yps=yps: e.matmul(yps[:, :], lhsT=wC[0][:, m, :], rhs=hr_[:, :], start=(mm == 0), stop=False), R=[wC[0], hr_], PW=[yps])
                        kb.op('pe', lambda e, m=m, hi_=hi_, mm=mm, yps=yps: e.matmul(yps[:, :], lhsT=wC[1][:, m, :], rhs=hi_[:, :], start=False, stop=(mm == 3)), R=[wC[1], hi_], PW=[yps])
                    kb.op('dve', lambda e, cc=cc, yps=yps: e.scalar_tensor_tensor(out=yb[:, :], in0=uf_[:, cc, :], scalar=dsk[:, cc:cc + 1], in1=yps[:, :], op0=TT, op1=ALU.add), R=[uf_, dsk, yps], W=[yb])
                    gelu_tanh(kb, yb, tm, zf[:, cc, :], zf, 512)
                    kb.op('act', lambda e, cc=cc: e.copy(out=zb[:, cc, :], in_=zf[:, cc, :]), R=[zf], PW=[zb])
                for oc in range(2):
                    ps = pss[6 + oc]
                    for kc in range(2):
                        kb.op('pe', lambda e, oc=oc, kc=kc, ps=ps: e.matmul(ps[:, :], lhsT=glw[:, kc, oc * 128:(oc + 1) * 128], rhs=zb[:, kc, :], start=(kc == 0), stop=(kc == 1)), R=[glw, zb], PW=[ps])
                    kb.op('act', lambda e, oc=oc, ps=ps: e.activation(out=sg[:, :], in_=ps[:, :], func=AF.Sigmoid, bias=glb[:, oc:oc + 1], scale=1.0), R=[ps, glb], W=[sg])
                    o_ = ob[oc]
                    kb.op('dve', lambda e, oc=oc, o_=o_: e.tensor_tensor(out=o_[:, :], in0=zf[:, oc, :], in1=sg[:, :], op=TT), R=[zf, sg], W=[o_])
                    kb.dma(self.MIXT[s, 768 + oc * 128:768 + (oc + 1) * 128, t0:t0 + 512], o_[:, :], o_, R=[o_])
            kb.barrier()

    def load_ln(self, es, l, j, tag):
        kb = self.kb
        g = kb.buf(es, "lng" + tag, [128, D], F32)
        b = kb.buf(es, "lnb" + tag, [128, D], F32)
        kb.dma(g[:, :], self.w["ln_g"][l, j:j + 1, :].partition_broadcast(128), g, W=[g])
        kb.dma(b[:, :], self.w["ln_b"][l, j:j + 1, :].partition_broadcast(128), b, W=[b])
        return g, b

    def phase_outproj(self, l, src):
        kb = self.kb
        with ExitStack() as es:
            wo = kb.buf(es, "op_w", [128, 8, D], BF16)
            kb.dma(wo[:, :, :], self.wb["w_out"][l].rearrange("(c p) o -> p c o", p=128), wo, W=[wo])
            g, b = self.load_ln(es, l, 0, "op")
            scr = self.ln_scratch(es, "op")
            mx = [kb.buf(es, "op_mx%d" % i, [128, 8, 512], BF16) for i in range(2)]
            xt = [kb.buf(es, "op_x%d" % i, [128, 4, D], F32) for i in range(2)]
            ot = [kb.buf(es, "op_o%d" % i, [128, 4, D], F32) for i in range(2)]
            tt = [kb.buf(es, "op_t%d" % i, [128, D], F32) for i in range(2)]
            pss = [kb.buf(es, "op_ps%d" % i, [128, 512], F32, psum=True) for i in range(4)]
            n = 0
            for s in range(self.n_seq):
                for i in range(NT):
                    t0 = i * 512
                    mx_, x_, o_ = mx[n % 2], xt[n % 2], ot[n % 2]
                    kb.dma(mx_[:, :, :], self.MIXT[s, :, t0:t0 + 512].rearrange("(c p) t -> p c t", p=128), mx_, W=[mx_])
                    kb.dma(x_[:, :, :], src[s, t0:t0 + 512, :].rearrange("(a p) d -> p a d", p=128), x_, W=[x_])
                    for sub in range(4):
                        tt_ = tt[sub % 2]
                        for half in range(2):
                            ps = pss[(sub * 2 + half) % 4]
                            for kc in range(8):
                                kb.op('pe', lambda e, kc=kc, sub=sub, half=half, ps=ps: e.matmul(ps[:, :], lhsT=mx_[:, kc, sub * 128:(sub + 1) * 128], rhs=wo[:, kc, half * 512:(half + 1) * 512],
                                                                                         start=(kc == 0), stop=(kc == 7)), R=[mx_, wo], PW=[ps])
                            kb.op('dve', lambda e, sub=sub, half=half, ps=ps, tt_=tt_: e.scalar_tensor_tensor(out=tt_[:, half * 512:(half + 1) * 512], in0=x_[:, sub, half * 512:(half + 1) * 512], scalar=ALPHA,
                                                                                                   in1=ps[:, :], op0=ALU.mult, op1=ALU.add), R=[x_, ps], PW=[tt_])
                        eng, fn, rd = layer_norm_tm(kb, tt_, g, b, o_[:, sub, :], scr)
                        kb.op(eng, fn, R=rd, PW=[o_])
                    kb.dma(self.XR[s, t0:t0 + 512, :].rearrange("(a p) d -> p a d", p=128), o_[:, :, :], o_, R=[o_], q='pool')
                    n += 1
            kb.barrier()

    def phase_ffn(self, l, moe):
        kb = self.kb
        C = self.C
        j = l // 2
        if moe:
            FF, GS, experts = D_FFE, 4, NE
            W1, W3, W2 = self.wb["moe_w1"][j], self.wb["moe_w3"][j], self.wb["moe_w2"][j]
        else:
            FF, GS, experts = D_FF, 2, 1
            W1, W3, W2 = self.wb["ffn_w1"][j:j + 1], self.wb["ffn_w3"][j:j + 1], self.wb["ffn_w2"][j:j + 1]
        NFC = FF // 128
        NG = NFC // GS
        GW = GS * 128
        with ExitStack() as es:
            g, b = self.load_ln(es, l, 1, "ff")
            scr = self.ln_scratch(es, "ff")
            xt = [kb.buf(es, "ff_x%d" % i, [128, 4, D], F32) for i in range(1 if moe else 2)]
            ot = kb.buf(es, "ff_o", [128, 4, D], F32)
            xT = kb.buf(es, "ff_xT", [128, 8, 512], BF16)
            w1 = [kb.buf(es, "ff_w1%d" % i, [128, 8, GW], BF16) for i in range(2)]
            w3 = [kb.buf(es, "ff_w3%d" % i, [128, 8, GW], BF16) for i in range(2)]
            w2 = [kb.buf(es, "ff_w2%d" % i, [128, NFC, 128], BF16) for i in range(2)]
            gT = kb.buf(es, "ff_g", [128, NFC, 512], BF16)
            sl = [kb.buf(es, "ff_s%d" % i, [128, 512], F32) for i in range(2)]
            facc = kb.buf(es, "ff_acc", [128, 8, 512], F32)
            tt = [kb.buf(es, "ff_t%d" % i, [128, D], F32) for i in range(2)]
            ps_h1 = [kb.buf(es, "ff_ph1%d" % i, [128, 512], F32, psum=True) for i in range(2)]
            ps_h3 = [kb.buf(es, "ff_ph3%d" % i, [128, 512], F32, psum=True) for i in range(2)]
            ps_o = [kb.buf(es, "ff_po%d" % i, [128, 512], F32, psum=True) for i in range(2)]
            ps_t = [kb.buf(es, "ff_pt%d" % i, [128, 512], F32, psum=True) for i in range(2)]
            if moe:
                xTf = kb.buf(es, "ff_xTf", [128, 8, 512], F32)
                rt = kb.buf(es, "ff_rt", [128, 8, 128], F32)
                kb.op('pool', lambda e: e.memset(rt[:, :, :], 0.0), W=[rt])
                kb.dma(rt[:, :, 0:NE], self.w["moe_router"][j].rearrange("(c p) e -> p c e", p=128), rt, PW=[rt])
                lg = kb.buf(es, "ff_lg", [128, 4, 8], F32)
                t8 = kb.buf(es, "ff_t8", [128, 4, 8], F32)
                wv = kb.buf(es, "ff_wv", [128, 4, 2], F32)
                cmb = kb.buf(es, "ff_cmb", [128, 4, 8], F32)
                cm2 = kb.buf(es, "ff_cm2", [128, 8], F32)
                cmB = kb.buf(es, "ff_cmB", [128, 8, 512], BF16)
                cexp = [kb.buf(es, "ff_cexp%d" % i, [128, 128], F32) for i in range(2)]
            cnt = [0]
            nw = [0]
            n2 = [0]
            n = 0
            for s in range(self.n_seq):
                for i in range(NT):
                    t0 = i * 512
                    x_ = xt[n % len(xt)]
                    n += 1
                    kb.dma(x_[:, :, :], self.XR[s, t0:t0 + 512, :].rearrange("(a p) d -> p a d", p=128), x_, W=[x_])
                    if moe:
                        transpose_in(kb, x_, 4, 8, xTf, C['ident'], ps_t, cnt, dst2=xT)
                    else:
                        transpose_in(kb, x_, 4, 8, xT, C['ident'], ps_t, cnt)
                    if moe and os.environ.get('MOE_SKIP_ROUTE'):
                        kb.op('pool', lambda e: e.memset(cmB[:, :, :], 0.5), W=[cmB])
                    elif moe:
                        for sub in range(4):
                            ps = ps_o[sub % 2]
                            for kc in range(8):
                                kb.op('pe', lambda e, kc=kc, sub=sub, ps=ps: e.matmul(ps[:, 0:128], lhsT=xTf[:, kc, sub * 128:(sub + 1) * 128], rhs=rt[:, kc, :], start=(kc == 0), stop=(kc == 7)), R=[xTf, rt], PW=[ps])
                            kb.op('dve', lambda e, sub=sub, ps=ps: e.tensor_copy(out=lg[:, sub, :], in_=ps[:, 0:8]), R=[ps], PW=[lg])
                            kb.op('dve', lambda e, sub=sub: e.max(out=t8[:, sub, :], in_=lg[:, sub, :]), R=[lg], PW=[t8])
                            kb.op('dve', lambda e, sub=sub: e.tensor_tensor(out=wv[:, sub, 0:1], in0=t8[:, sub, 0:1], in1=t8[:, sub, 1:2], op=ALU.subtract), R=[t8], PW=[wv])
                            kb.op('act', lambda e, sub=sub: e.activation(out=wv[:, sub, 0:1], in_=wv[:, sub, 0:1], func=AF.Sigmoid), R=[wv], PW=[wv])
                            kb.op('dve', lambda e, sub=sub: e.tensor_scalar(out=wv[:, sub, 1:2], in0=wv[:, sub, 0:1], scalar1=-1.0, scalar2=1.0, op0=ALU.mult, op1=ALU.add), R=[wv], PW=[wv])
                            kb.op('dve', lambda e, sub=sub: e.tensor_scalar(out=cmb[:, sub, :], in0=lg[:, sub, :], scalar1=t8[:, sub, 0:1], scalar2=wv[:, sub, 0:1], op0=ALU.is_equal, op1=ALU.mult), R=[lg, t8, wv], PW=[cmb])
                            kb.op('dve', lambda e, sub=sub: e.tensor_scalar(out=cm2[:, :], in0=lg[:, sub, :], scalar1=t8[:, sub, 1:2], scalar2=wv[:, sub, 1:2], op0=ALU.is_equal, op1=ALU.mult), R=[lg, t8, wv], W=[cm2])
                            kb.op('dve', lambda e, sub=sub: e.tensor_tensor(out=cmb[:, sub, :], in0=cmb[:, sub, :], in1=cm2[:, :], op=ALU.add), R=[cmb, cm2], PW=[cmb])
                        for ex in range(NE):
                            ps = ps_o[ex % 2]
                            for sub in range(4):
                                ce = cexp[(ex * 4 + sub) % 2]
                                kb.op('dve', lambda e, ex=ex, sub=sub, ce=ce: e.tensor_copy(out=ce[:, :], in_=cmb[:, sub, ex:ex + 1].to_broadcast([128, 128])), R=[cmb], W=[ce])
                                kb.op('pe', lambda e, sub=sub, ps=ps, ce=ce: e.matmul(ps[:, sub * 128:(sub + 1) * 128], lhsT=ce[:, :], rhs=C['ident'][:, :], start=True, stop=True), R=[ce, C['ident']], PW=[ps])
                            kb.op('act', lambda e, ex=ex, ps=ps: e.copy(out=cmB[:, ex, :], in_=ps[:, :]), R=[ps], PW=[cmB])
                    for ex in range(int(os.environ.get('MOE_NE', experts)) if moe else experts):
                        for gi_ in range(0 if (moe and os.environ.get('MOE_MODE') == 's2') else NG):
                            w1_, w3_ = w1[nw[0] % 2], w3[nw[0] % 2]
                            nw[0] += 1
                            kb.dma(w1_[:, :, :], W1[ex, :, gi_ * GW:(gi_ + 1) * GW].rearrange("(c p) f -> p c f", p=128), w1_, W=[w1_])
                            kb.dma(w3_[:, :, :], W3[ex, :, gi_ * GW:(gi_ + 1) * GW].rearrange("(c p) f -> p c f", p=128), w3_, W=[w3_])
                            for fi in range(GS):
                                fc = gi_ * GS + fi
                                p1, p3 = ps_h1[fc % 2], ps_h3[fc % 2]
                                for kc in range(8):
                                    kb.op('pe', lambda e, kc=kc, fi=fi, p1=p1, w1_=w1_: e.matmul(p1[:, :], lhsT=w1_[:, kc, fi * 128:(fi + 1) * 128], rhs=xT[:, kc, :], start=(kc == 0), stop=(kc == 7)), R=[w1_, xT], PW=[p1])
                                for kc in range(8):
                                    kb.op('pe', lambda e, kc=kc, fi=fi, p3=p3, w3_=w3_: e.matmul(p3[:, :], lhsT=w3_[:, kc, fi * 128:(fi + 1) * 128], rhs=xT[:, kc, :], start=(kc == 0), stop=(kc == 7)), R=[w3_, xT], PW=[p3])
                                s_ = sl[fc % 2]
                                kb.op('act', lambda e, p1=p1, s_=s_: e.activation(out=s_[:, :], in_=p1[:, :], func=AF.Silu), R=[p1], W=[s_])
                                if moe:
                                    kb.op('dve', lambda e, p3=p3, s_=s_: e.tensor_tensor(out=s_[:, :], in0=s_[:, :], in1=p3[:, :], op=ALU.mult), R=[s_, p3], W=[s_])
                                    kb.op('dve', lambda e, fc=fc, s_=s_, ex=ex: e.tensor_tensor(out=gT[:, fc, :], in0=s_[:, :], in1=cmB[:, ex, :], op=ALU.mult), R=[s_, cmB], PW=[gT])
                                else:
                                    kb.op('dve', lambda e, fc=fc, p3=p3, s_=s_: e.tensor_tensor(out=gT[:, fc, :], in0=s_[:, :], in1=p3[:, :], op=ALU.mult), R=[s_, p3], PW=[gT])
                        for dc in range(0 if (moe and os.environ.get('MOE_MODE') == 's1') else 8):
                            w2_ = w2[n2[0] % 2]
                            n2[0] += 1
                            kb.dma(w2_[:, :, :], W2[ex, :, dc * 128:(dc + 1) * 128].rearrange("(c p) o -> p c o", p=128), w2_, W=[w2_])
                            po = ps_o[dc % 2]
                            for fc in range(NFC):
                                kb.op('pe', lambda e, fc=fc, po=po, w2_=w2_: e.matmul(po[:, :], lhsT=w2_[:, fc, :], rhs=gT[:, fc, :], start=(fc == 0), stop=(fc == NFC - 1)), R=[w2_, gT], PW=[po])
                            if ex == 0:
                                kb.op('act', lambda e, dc=dc, po=po: e.copy(out=facc[:, dc, :], in_=po[:, :]), R=[po], PW=[facc])
                            else:
                                kb.op('dve', lambda e, dc=dc, po=po: e.tensor_tensor(out=facc[:, dc, :], in0=facc[:, dc, :], in1=po[:, :], op=ALU.add), R=[facc, po], PW=[facc])
                    for sub in range(4):
                        tt_ = tt[sub % 2]
                        for half in range(2):
                            pst = ps_t[cnt[0] % 2]
                            cnt[0] += 1
                            for q in range(4):
                                dc = half * 4 + q
                                kb.op('pe', lambda e, dc=dc, q=q, sub=sub, pst=pst: e.transpose(pst[:, q * 128:(q + 1) * 128], facc[:, dc, sub * 128:(sub + 1) * 128], C['ident'][:, :]), R=[facc, C['ident']], PW=[pst])
                            kb.op('dve', lambda e, sub=sub, half=half, pst=pst, tt_=tt_: e.scalar_tensor_tensor(out=tt_[:, half * 512:(half + 1) * 512], in0=x_[:, sub, half * 512:(half + 1) * 512], scalar=ALPHA,
                                                                                                    in1=pst[:, :], op0=ALU.mult, op1=ALU.add), R=[x_, pst], PW=[tt_])
                        eng, fn, rd = layer_norm_tm(kb, tt_, g, b, ot[:, sub, :], scr)
                        kb.op(eng, fn, R=rd, PW=[ot])
                    kb.dma(self.XR[s, t0:t0 + 512, :].rearrange("(a p) d -> p a d", p=128), ot[:, :, :], ot, R=[ot], q='pool')
            kb.barrier()

    def phase_ple(self, l, dst):
        kb = self.kb
        C = self.C
        with ExitStack() as es:
            wg = kb.buf(es, "pl_wg", [128, 8, D], BF16)
            wp = kb.buf(es, "pl_wp", [128, 2, D], BF16)
            bg = kb.buf(es, "pl_bg", [128, D], F32)
            kb.dma(wg[:, :, :], self.wb["ple_gate_w"][l].rearrange("(c p) o -> p c o", p=128), wg, W=[wg])
            kb.dma(wp[:, :, :], self.wb["ple_proj"][l].rearrange("(c p) o -> p c o", p=128), wp, W=[wp])
            kb.dma(bg[:, :], self.w["ple_gate_b"][l:l + 1, :].partition_broadcast(128), bg, W=[bg])
            g, b = self.load_ln(es, l, 2, "pl")
            scr = self.ln_scratch(es, "pl")
            xt = [kb.buf(es, "pl_x%d" % i, [128, 4, D], F32) for i in range(2)]
            pt = [kb.buf(es, "pl_p%d" % i, [128, 4, 256], F32) for i in range(2)]
            ot = [kb.buf(es, "pl_o%d" % i, [128, 4, D], F32) for i in range(2)]
            xT = kb.buf(es, "pl_xT", [128, 8, 512], BF16)
            pT = kb.buf(es, "pl_pT", [128, 2, 512], BF16)
            tt = [kb.buf(es, "pl_t%d" % i, [128, D], F32) for i in range(2)]
            uu = [kb.buf(es, "pl_u%d" % i, [128, 512], F32) for i in range(2)]
            ps_t = [kb.buf(es, "pl_pt%d" % i, [128, 512], F32, psum=True) for i in range(2)]
            ps_a = [kb.buf(es, "pl_pa%d" % i, [128, 512], F32, psum=True) for i in range(2)]
            ps_b = [kb.buf(es, "pl_pb%d" % i, [128, 512], F32, psum=True) for i in range(2)]
            cnt = [0]
            n = 0
            for s in range(self.n_seq):
                for i in range(NT):
                    t0 = i * 512
                    x_, p_, o_ = xt[n % 2], pt[n % 2], ot[n % 2]
                    n += 1
                    kb.dma(x_[:, :, :], self.XR[s, t0:t0 + 512, :].rearrange("(a p) d -> p a d", p=128), x_, W=[x_])
                    kb.dma(p_[:, :, :], self.p[l, s, t0:t0 + 512, :].rearrange("(a p) d -> p a d", p=128), p_, W=[p_])
                    transpose_in(kb, x_, 4, 8, xT, C['ident'], ps_t, cnt)
                    transpose_in(kb, p_, 4, 2, pT, C['ident'], ps_t, cnt)
                    for sub in range(4):
                        tt_ = tt[sub % 2]
                        for half in range(2):
                            pa, pb = ps_a[half], ps_b[half]
                            u_ = uu[half]
                            hs = slice(half * 512, (half + 1) * 512)
                            for kc in range(8):
                                kb.op('pe', lambda e, kc=kc, sub=sub, pa=pa, hs=hs: e.matmul(pa[:, :], lhsT=xT[:, kc, sub * 128:(sub + 1) * 128], rhs=wg[:, kc, hs], start=(kc == 0), stop=(kc == 7)), R=[xT, wg], PW=[pa])
                            for kc in range(2):
                                kb.op('pe', lambda e, kc=kc, sub=sub, pb=pb, hs=hs: e.matmul(pb[:, :], lhsT=pT[:, kc, sub * 128:(sub + 1) * 128], rhs=wp[:, kc, hs], start=(kc == 0), stop=(kc == 1)), R=[pT, wp], PW=[pb])
                            kb.op('dve', lambda e, pa=pa, u_=u_, hs=hs: e.tensor_tensor(out=u_[:, :], in0=pa[:, :], in1=bg[:, hs], op=ALU.add), R=[pa, bg], W=[u_])
                            kb.op('act', lambda e, u_=u_: e.activation(out=u_[:, :], in_=u_[:, :], func=AF.Sigmoid), R=[u_], W=[u_])
                            kb.op('dve', lambda e, pb=pb, u_=u_: e.tensor_tensor(out=u_[:, :], in0=u_[:, :], in1=pb[:, :], op=ALU.mult), R=[u_, pb], W=[u_])
                            kb.op('dve', lambda e, sub=sub, u_=u_, hs=hs, tt_=tt_: e.scalar_tensor_tensor(out=tt_[:, hs], in0=x_[:, sub, hs], scalar=ALPHA, in1=u_[:, :], op0=ALU.mult, op1=ALU.add), R=[x_, u_], PW=[tt_])
                        eng, fn, rd = layer_norm_tm(kb, tt_, g, b, o_[:, sub, :], scr)
                        kb.op(eng, fn, R=rd, PW=[o_])
                    kb.dma(dst[s, t0:t0 + 512, :].rearrange("(a p) d -> p a d", p=128), o_[:, :, :], o_, R=[o_], q='pool')
            kb.barrier()


def win_perm():
    q = lambda h, half: [h * 64 + half * 32 + d for d in range(32)]
    kv = lambda br, h, half: [512 + (br * 2 + h) * 64 + half * 32 + d for d in range(32)]
    vfull = lambda br, h: [512 + (br * 2 + h) * 64 + d for d in range(64)]
    cols = []
    for hs in (range(0, 4), range(4, 8)):
        for half in range(2):
            for h in hs:
                cols += q(h, half)
    for half in range(2):
        cols += kv(0, 0, half) + kv(0, 1, half) + kv(2, 0, half) + kv(2, 1, half)
    for half in range(2):
        cols += kv(4, 0, half) + kv(4, 1, half)
    cols += vfull(1, 0) + vfull(1, 1)
    cols += list(range(512 + 768 + 24, 512 + 768 + 24 + 512))
    cols += list(range(512 + 768 + 24 + 512, 2072))
    cols += vfull(3, 0) + vfull(3, 1) + vfull(5, 0) + vfull(5, 1)
    cols += list(range(512 + 768, 512 + 768 + 24))
    assert len(cols) == INW and len(set(cols)) == INW
    return np.array(cols)


def make_consts():
    ident = np.eye(128, dtype=np.float32)
    inv_freq = (np.float32(10000.0) ** (-np.arange(0, 64, 2, dtype=np.float32) / np.float32(64))).astype(np.float32)
    invf = np.tile(inv_freq, 4).reshape(128, 1).astype(np.float32)
    j = np.arange(L)
    emat = (j[None, :] // 64 == np.arange(64)[:, None]).astype(np.float32).astype(ml_dtypes.bfloat16)
    n_cmp = 255
    c_start = np.arange(n_cmp)[:, None] * 16
    s_start = np.arange(64)[None, :] * 64
    ovl = np.zeros((256, 64), np.float32)
    ovl[:n_cmp] = ((c_start < s_start + 64) & (c_start + 32 > s_start)).astype(np.float32)
    return {"c_ident": ident, "c_invf": invf, "c_emat": emat, "c_ovl": ovl.astype(ml_dtypes.bfloat16)}


_PROG = {}


def kernel(**inputs):
    n_cores = 8
    if 'full' not in _PROG:
        _PROG['full'] = Prog()
    prog = _PROG['full']
    consts = make_consts()
    perm = win_perm()
    shared = {k: np.ascontiguousarray(np.asarray(v)) for k, v in inputs.items() if k not in ("x", "p", "positions")}
    shared["w_in"] = np.ascontiguousarray(shared["w_in"][:, :, perm])
    shared.update(consts)
    x = np.asarray(inputs["x"])
    p = np.asarray(inputs["p"])
    pos = np.asarray(inputs["positions"]).astype(np.int32)
    in_maps = []
    for c in range(n_cores):
        m = dict(shared)
        m["x"] = np.ascontiguousarray(x[2 * c:2 * c + 2])
        m["p"] = np.ascontiguousarray(p[:, 2 * c:2 * c + 2])
        m["positions"] = np.ascontiguousarray(pos[2 * c:2 * c + 2])
        in_maps.append(m)
    res = run_bass_kernel_spmd(prog.nc, in_maps, core_ids=list(range(n_cores)))
    out = np.concatenate([np.asarray(r["y"]) for r in res.results], axis=0)
    return out.astype(np.float32)
```
